# Optimizing a Trainium2 kernel written in Bass

```python
import jax, jax.numpy as jnp
from jax import lax
import numpy as np

D_MODEL = 2048
BATCH = 4
SEQ = 4096
DEPTH = 4

CHUNK = 64
N_A_LAYERS = DEPTH // 2
N_B_LAYERS = DEPTH - N_A_LAYERS
RMS_EPS = 1e-6

GLA_HEADS = 4
GLA_DK = D_MODEL // 2 // GLA_HEADS
GLA_DV = D_MODEL // GLA_HEADS
GLA_GATE_RANK = 16
GLA_GATE_TEMP = 16.0
GLA_IN_COLS = 2 * GLA_HEADS * GLA_DK + 2 * GLA_HEADS * GLA_DV + GLA_GATE_RANK

FOX_HEADS = 16
FOX_DH = D_MODEL // FOX_HEADS
FOX_QBLOCK = 128
FOX_FORGET_BIAS_MEAN = 3.0

PEER_HEADS = 8
PEER_QDIM = 256
PEER_HALF = PEER_QDIM // 2
PEER_NKEYS = 128
PEER_N_EXPERTS = PEER_NKEYS * PEER_NKEYS
PEER_TOPK = 16
PEER_TOKEN_BLOCK = 128

kernel_name = 'hybrid_gla_fox_peer_adaln_trunk'


def _rmsnorm(x, g):
    xf = x.astype(jnp.float32)
    xf = xf * lax.rsqrt(jnp.mean(xf * xf, axis=-1, keepdims=True) + RMS_EPS)
    return xf.astype(x.dtype) * g


def _modulate(h, shift, scale):
    return h * (1 + scale[:, None, :]) + shift[:, None, :]


def _gla(h, w_in, w_gate2, b_gate2, g_norm, w_out):
    B, S, _ = h.shape
    nc = S // CHUNK
    hk = GLA_HEADS * GLA_DK
    hv = GLA_HEADS * GLA_DV
    proj = h @ w_in
    q, k, v, g, gl = jnp.split(proj, [hk, 2 * hk, 2 * hk + hv, 2 * hk + 2 * hv], axis=-1)
    log_a = jax.nn.log_sigmoid((gl @ w_gate2 + b_gate2).astype(jnp.float32)) / GLA_GATE_TEMP

    def to_chunks(t, dh):
        return t.astype(jnp.float32).reshape(B, nc, CHUNK, GLA_HEADS, dh).transpose(1, 0, 3, 2, 4)

    qc = to_chunks(q, GLA_DK) * (GLA_DK ** -0.5)
    kc = to_chunks(k, GLA_DK)
    vc = to_chunks(v, GLA_DV)
    bc = jnp.cumsum(to_chunks(log_a, GLA_DK), axis=3)

    def step(state, xs):
        qt, kt, vt, bt = xs
        inter = jnp.einsum('bhtk,bhkv->bhtv', qt * jnp.exp(bt), state)
        decay = jnp.exp(-jnp.abs(bt[:, :, :, None, :] - bt[:, :, None, :, :]))
        scores = jnp.einsum('bhtk,bhsk,bhtsk->bhts', qt, kt, decay)
        out = inter + jnp.einsum('bhts,bhsv->bhtv', scores, vt)
        b_end = bt[:, :, -1:, :]
        state = (jnp.exp(b_end[:, :, 0, :])[..., None] * state
                 + jnp.einsum('bhsk,bhsv->bhkv', kt * jnp.exp(b_end - bt), vt))
        return state, out

    s0 = jnp.zeros((B, GLA_HEADS, GLA_DK, GLA_DV), jnp.float32)
    _, o = lax.scan(step, s0, (qc, kc, vc, bc))
    o = o.transpose(1, 0, 3, 2, 4).reshape(B, S, GLA_HEADS, GLA_DV)
    o = _rmsnorm(o, g_norm).astype(h.dtype).reshape(B, S, hv)
    return (o * jax.nn.silu(g)) @ w_out


def _shared_kv(x, c_act, kv_norm, kv_ada_w, kv_ada_b, w_kv, b_f):
    B, S, _ = x.shape
    shift, scale = jnp.split(c_act @ kv_ada_w + kv_ada_b, 2, axis=-1)
    hk = _modulate(_rmsnorm(x, kv_norm), shift, scale)
    k, v, fl = jnp.split(hk @ w_kv, [D_MODEL, 2 * D_MODEL], axis=-1)
    log_f = jax.nn.log_sigmoid((fl + b_f).astype(jnp.float32))
    F = jnp.cumsum(log_f, axis=1).transpose(0, 2, 1)
    k = k.reshape(B, S, FOX_HEADS, FOX_DH).transpose(0, 2, 1, 3)
    v = v.reshape(B, S, FOX_HEADS, FOX_DH).transpose(0, 2, 1, 3)
    return k, v, F


def _fox(h, k, v, F, w_q, w_out):
    B, S, _ = h.shape
    q, g = jnp.split(h @ w_q, 2, axis=-1)
    q = q.reshape(B, S, FOX_HEADS, FOX_DH).transpose(0, 2, 1, 3) * (FOX_DH ** -0.5)
    outs = []
    for i in range(S // FOX_QBLOCK):
        lo, hi = i * FOX_QBLOCK, (i + 1) * FOX_QBLOCK
        s = jnp.einsum('bhqd,bhkd->bhqk', q[:, :, lo:hi], k[:, :, :hi]).astype(jnp.float32)
        s = s + F[:, :, lo:hi, None] - F[:, :, None, :hi]
        mask = jnp.arange(lo, hi)[:, None] >= jnp.arange(hi)[None, :]
        p = jax.nn.softmax(jnp.where(mask, s, -jnp.inf), axis=-1).astype(v.dtype)
        outs.append(jnp.einsum('bhqk,bhkd->bhqd', p, v[:, :, :hi]))
    o = jnp.concatenate(outs, axis=2).transpose(0, 2, 1, 3).reshape(B, S, D_MODEL)
    return (o * jax.nn.sigmoid(g)) @ w_out


def _peer(h, w_q, subkeys, u, v):
    B, S, D = h.shape
    T = B * S
    hf = h.reshape(T, D)
    q = (hf @ w_q).reshape(T, PEER_HEADS, 2, PEER_HALF)
    s = jnp.einsum('thpd,hpnd->thpn', q, subkeys)
    vals, idx = lax.top_k(s, PEER_TOPK)
    cand = (vals[:, :, 0, :, None] + vals[:, :, 1, None, :]).reshape(T, PEER_HEADS, PEER_TOPK * PEER_TOPK)
    cand_id = (idx[:, :, 0, :, None] * PEER_NKEYS + idx[:, :, 1, None, :]).reshape(T, PEER_HEADS, PEER_TOPK * PEER_TOPK)
    top_v, top_i = lax.top_k(cand, PEER_TOPK)
    expert = jnp.take_along_axis(cand_id, top_i, axis=-1)
    w = jax.nn.softmax(top_v.astype(jnp.float32), axis=-1).astype(h.dtype)
    nb = T // PEER_TOKEN_BLOCK
    kk = PEER_HEADS * PEER_TOPK
    ids = expert.reshape(nb, PEER_TOKEN_BLOCK, kk)
    ws = w.reshape(nb, PEER_TOKEN_BLOCK, kk)
    xb = hf.reshape(nb, PEER_TOKEN_BLOCK, D)

    def block(args):
        xt, it, wt = args
        act = jax.nn.gelu(jnp.einsum('tkd,td->tk', u[it], xt))
        return jnp.einsum('tk,tkd->td', wt * act, v[it])

    return lax.map(block, (xb, ids, ws)).reshape(B, S, D)


def setup_inputs(seed: int = 0) -> dict:
    key = jax.random.key(seed)
    ks = jax.random.split(key, 24)
    D = D_MODEL

    def nrm(k, shape, std):
        return jax.random.normal(k, shape, jnp.float32) * std

    def gain(k, shape):
        return 1.0 + nrm(k, shape, 0.01)

    return {
        'x': nrm(ks[0], (BATCH, SEQ, D), 1.0),
        'c': nrm(ks[1], (BATCH, D), 1.0),
        'ada_w': nrm(ks[2], (DEPTH, D, 6 * D), 0.5 * D ** -0.5),
        'ada_b': nrm(ks[3], (DEPTH, 6 * D), 0.02),
        'norm_mix': gain(ks[4], (DEPTH, D)),
        'norm_ffn': gain(ks[5], (DEPTH, D)),
        'gla_w_in': nrm(ks[6], (N_A_LAYERS, D, GLA_IN_COLS), D ** -0.5),
        'gla_w_gate2': nrm(ks[7], (N_A_LAYERS, GLA_GATE_RANK, GLA_HEADS * GLA_DK), GLA_GATE_RANK ** -0.5),
        'gla_b_gate2': nrm(ks[8], (N_A_LAYERS, GLA_HEADS * GLA_DK), 0.02),
        'gla_norm': gain(ks[9], (N_A_LAYERS, GLA_DV)),
        'gla_w_out': nrm(ks[10], (N_A_LAYERS, GLA_HEADS * GLA_DV, D), (GLA_HEADS * GLA_DV) ** -0.5),
        'kv_norm': gain(ks[11], (D,)),
        'kv_ada_w': nrm(ks[12], (D, 2 * D), 0.5 * D ** -0.5),
        'kv_ada_b': nrm(ks[13], (2 * D,), 0.02),
        'fox_w_kv': nrm(ks[14], (D, 2 * D + FOX_HEADS), D ** -0.5),
        'fox_b_f': FOX_FORGET_BIAS_MEAN + nrm(ks[15], (FOX_HEADS,), 0.5),
        'fox_w_q': nrm(ks[16], (N_B_LAYERS, D, 2 * D), D ** -0.5),
        'fox_w_out': nrm(ks[17], (N_B_LAYERS, D, D), D ** -0.5),
        'peer_w_q': nrm(ks[18], (DEPTH, D, PEER_HEADS * PEER_QDIM), D ** -0.5),
        'peer_subkeys': nrm(ks[19], (DEPTH, PEER_HEADS, 2, PEER_NKEYS, PEER_HALF), PEER_HALF ** -0.5),
        'peer_u': nrm(ks[20], (DEPTH, PEER_N_EXPERTS, D), D ** -0.5),
        'peer_v': nrm(ks[21], (DEPTH, PEER_N_EXPERTS, D), PEER_HEADS ** -0.5),
        'final_norm': gain(ks[22], (D,)),
    }


def reference(x, c, ada_w, ada_b, norm_mix, norm_ffn, gla_w_in, gla_w_gate2, gla_b_gate2,
              gla_norm, gla_w_out, kv_norm, kv_ada_w, kv_ada_b, fox_w_kv, fox_b_f,
              fox_w_q, fox_w_out, peer_w_q, peer_subkeys, peer_u, peer_v, final_norm):
    c_act = jax.nn.silu(c)
    kv = None
    for l in range(DEPTH):
        mod = c_act @ ada_w[l] + ada_b[l]
        sh_m, sc_m, gt_m, sh_f, sc_f, gt_f = jnp.split(mod, 6, axis=-1)
        h = _modulate(_rmsnorm(x, norm_mix[l]), sh_m, sc_m)
        if l < N_A_LAYERS:
            a = l
            y = _gla(h, gla_w_in[a], gla_w_gate2[a], gla_b_gate2[a], gla_norm[a], gla_w_out[a])
        else:
            b = l - N_A_LAYERS
            y = _fox(h, kv[0], kv[1], kv[2], fox_w_q[b], fox_w_out[b])
        x = x + gt_m[:, None, :] * y
        h = _modulate(_rmsnorm(x, norm_ffn[l]), sh_f, sc_f)
        x = x + gt_f[:, None, :] * _peer(h, peer_w_q[l], peer_subkeys[l], peer_u[l], peer_v[l])
        if l == N_A_LAYERS - 1:
            kv = _shared_kv(x, c_act, kv_norm, kv_ada_w, kv_ada_b, fox_w_kv, fox_b_f)
    return _rmsnorm(x, final_norm)
```

```python
import contextlib
import numpy as np
import concourse.bass as bass
import concourse.mybir as mybir

F32 = mybir.dt.float32
BF16 = mybir.dt.bfloat16
AF = mybir.ActivationFunctionType
ALU = mybir.AluOpType
AX = mybir.AxisListType

ENGS = ['pe', 'act', 'dve', 'pool', 'sp']
DMA_SLOTS = {'sp': 12, 'act': 4, 'pool': 6}


class Buf:
    def __init__(self, ap_full, name=""):
        self.t = ap_full
        self.name = name
        self.last_w = None
        self.readers = []

    def __getitem__(self, k):
        return self.t[k]


class Prog:
    def __init__(self, nc):
        self.nc = nc
        self.es = contextlib.ExitStack()
        self.ops = {e: [] for e in ENGS}
        self.cnt = {e: 0 for e in ENGS}
        self.seen = {e: {f: 0 for f in ENGS} for e in ENGS}
        self.sem = {}
        for e in ENGS:
            self.sem[e] = self.es.enter_context(nc.semaphore("s_" + e))
        self.dsem = {}
        self.dval = {}
        self.dseen = {e: {} for e in ENGS}
        self.drr = {q: 0 for q in DMA_SLOTS}
        for q, n in DMA_SLOTS.items():
            for i in range(n):
                key = (q, i)
                self.dsem[key] = self.es.enter_context(nc.semaphore("d_%s%d" % (q, i)))
                self.dval[key] = 0
        self.n_alloc = 0

    def sbuf(self, shape, dtype=F32, name=None, stack=None):
        self.n_alloc += 1
        name = name or ("sb%d" % self.n_alloc)
        t = (stack or self.es).enter_context(self.nc.sbuf_tensor(name + "_%d" % self.n_alloc, list(shape), dtype))
        return Buf(t, name)

    def psum(self, shape, dtype=F32, name=None, stack=None):
        self.n_alloc += 1
        name = name or ("ps%d" % self.n_alloc)
        t = (stack or self.es).enter_context(self.nc.psum_tensor(name + "_%d" % self.n_alloc, list(shape), dtype))
        return Buf(t, name)

    def dram(self, name, shape, dtype=F32, kind="Internal"):
        t = self.nc.dram_tensor(name, list(shape), dtype, kind=kind)
        return Buf(t.ap(), name)

    def _collect(self, eng, reads, writes):
        deps = []
        for b in reads:
            if b.last_w is not None:
                deps.append(b.last_w)
        for b in writes:
            if b.last_w is not None:
                deps.append(b.last_w)
            deps.extend(b.readers)
        waits = []
        for d in deps:
            if d[0] == 'eng':
                _, f, k = d
                if f == eng and eng == 'pe':
                    continue
                if self.seen[eng][f] < k:
                    self.seen[eng][f] = k
                    waits.append((self.sem[f], k))
            else:
                _, key, val = d
                if self.dseen[eng].get(key, 0) < val:
                    self.dseen[eng][key] = val
                    waits.append((self.dsem[key], val))
        best = {}
        for s, v in waits:
            if id(s) not in best or best[id(s)][1] < v:
                best[id(s)] = (s, v)
        return list(best.values())

    def _mark(self, tok, reads, writes):
        for b in writes:
            b.last_w = tok
            b.readers = []
        for b in reads:
            if b not in writes:
                b.readers.append(tok)
                if len(b.readers) > 64:
                    b.readers = b.readers[-64:]

    def op(self, eng, fn, reads=(), writes=()):
        waits = self._collect(eng, reads, writes)
        self.cnt[eng] += 1
        k = self.cnt[eng]
        sem = self.sem[eng]

        def run(e, waits=waits, fn=fn, sem=sem):
            for s, v in waits:
                e.wait_ge(s, v)
            fn(e).then_inc(sem, 1)
        self.ops[eng].append(run)
        self._mark(('eng', eng, k), reads, writes)

    def I(self, eng, meth, reads=(), writes=(), **kw):
        self.op(eng, lambda e, meth=meth, kw=kw: getattr(e, meth)(**kw), reads, writes)

    def dma(self, q, out, in_, reads=(), writes=(), **kw):
        waits = self._collect(q, reads, writes)
        n = DMA_SLOTS[q]
        i = self.drr[q]
        self.drr[q] = (i + 1) % n
        key = (q, i)
        prev = self.dval[key]
        if prev > 0 and self.dseen[q].get(key, 0) < prev:
            self.dseen[q][key] = prev
            waits.append((self.dsem[key], prev))
        self.dval[key] = prev + 16
        val = prev + 16
        sem = self.dsem[key]

        def run(e, waits=waits, out=out, in_=in_, sem=sem, kw=kw):
            for s, v in waits:
                e.wait_ge(s, v)
            e.dma_start(out=out, in_=in_, **kw).then_inc(sem, 16)
        self.ops[q].append(run)
        self._mark(('dma', key, val), reads, writes)

    def barrier(self):
        for e in ENGS:
            waits = []
            for f in ENGS:
                if f != e and self.seen[e][f] < self.cnt[f]:
                    self.seen[e][f] = self.cnt[f]
                    waits.append((self.sem[f], self.cnt[f]))
            for key, val in self.dval.items():
                if val > 0 and self.dseen[e].get(key, 0) < val:
                    self.dseen[e][key] = val
                    waits.append((self.dsem[key], val))

            def run(en, waits=waits):
                for s, v in waits:
                    en.wait_ge(s, v)
            self.ops[e].append(run)

    def finish(self):
        self.barrier()
        nc = self.nc
        with nc.Block() as block:
            @block.tensor
            def _(e):
                for f in self.ops['pe']:
                    f(e)

            @block.scalar
            def _(e):
                for f in self.ops['act']:
                    f(e)

            @block.vector
            def _(e):
                for f in self.ops['dve']:
                    f(e)

            @block.gpsimd
            def _(e):
                for f in self.ops['pool']:
                    f(e)

            @block.sync
            def _(e):
                for f in self.ops['sp']:
                    f(e)
        self.es.close()

from concourse.bass_utils import run_bass_kernel_spmd

D = 2048
NT = 16
TOK = 2048
EPS = 1e-6
MODN = 4 * 12288 + 4096

C_ID, C_M1, C_M2, C_LINC, C_USUF, C_ONES, C_MA, C_MB, C_P = 0, 128, 256, 384, 512, 640, 768, 896, 1024
NCST = 1025


def make_consts(p):
    c = np.zeros((128, NCST), np.float32)
    s = np.arange(128)[:, None]
    t = np.arange(128)[None, :]
    c[:, C_ID:C_ID + 128] = (s == t)
    m1 = (s <= t).astype(np.float32)
    c[:, C_M1:C_M1 + 128] = m1
    c[:, C_M2:C_M2 + 128] = ((s > t) & ((s // 64) == (t // 64)))
    c[:, C_LINC:C_LINC + 128] = m1 * (-1.0 / 16.0)
    c[:, C_USUF:C_USUF + 128] = (s > t) * (-1.0 / 16.0)
    c[:, C_ONES:C_ONES + 128] = 1.0
    if p == 0:
        c[:, C_MA:C_MA + 128] = m1
        c[:, C_MB:C_MB + 128] = 0.0
    else:
        c[:, C_MA:C_MA + 128] = 1.0
        c[:, C_MB:C_MB + 128] = m1
    c[:, C_P] = float(p)
    return c


class Ctx:
    def __init__(self, P, cst_d):
        self.P = P
        self.bank = [P.psum([128, 512], F32, name="bank%d" % i) for i in range(8)]
        self.cst = P.sbuf([128, NCST], F32, name="cst")
        P.dma('sp', self.cst[:], cst_d[:, :], reads=[cst_d], writes=[self.cst])
        self.small = {}

    def c(self, off, n=128):
        return self.cst[:, off:off + n]


def bcast_row(P, dst, dst_ap, src_buf, src_ap_row, q='sp'):
    P.dma(q, dst_ap, src_ap_row.partition_broadcast(128), reads=[src_buf], writes=[dst])


def rstd_from_ss(P, ss, rstd, n, inv_n):
    P.I('dve', 'tensor_scalar', reads=[ss], writes=[rstd], out=rstd[:, 0:n], in0=ss[:, 0:n],
        scalar1=inv_n, scalar2=EPS, op0=ALU.mult, op1=ALU.add)
    P.I('act', 'activation', reads=[rstd], writes=[rstd], out=rstd[:, 0:n], in_=rstd[:, 0:n], func=AF.Sqrt)
    P.I('dve', 'reciprocal', reads=[rstd], writes=[rstd], out=rstd[:, 0:n], in_=rstd[:, 0:n])


def normmod_T(P, cx, xt, A, B, h, junk, ss, rstd, hT, tcol, banks):
    P.I('act', 'activation', reads=[xt], writes=[junk, ss], out=junk[:], in_=xt[:], func=AF.Square,
        accum_out=ss[:, 0:1])
    rstd_from_ss(P, ss, rstd, 1, 1.0 / D)
    P.I('dve', 'scalar_tensor_tensor', reads=[xt, rstd, A], writes=[h], out=h[:], in0=xt[:],
        scalar=rstd[:, 0:1], in1=A[:], op0=ALU.mult, op1=ALU.mult)
    if B is not None:
        P.I('pool', 'tensor_tensor', reads=[h, B], writes=[h], out=h[:], in0=h[:], in1=B[:], op=ALU.add)
    transpose_T(P, cx, h, hT, tcol, banks)


def transpose_T(P, cx, h, hT, tcol, banks, nch=16):
    for g in range(nch // 4):
        bk = banks[g % len(banks)]
        for k in range(4):
            c = g * 4 + k
            P.I('pe', 'transpose', reads=[h, cx.cst], writes=[bk], out=bk[:, k * 128:(k + 1) * 128],
                in_=h[:, c * 128:(c + 1) * 128], identity=cx.c(C_ID))
        eng = 'act' if g % 2 == 0 else 'dve'
        src = bk[:].rearrange("p (k t) -> p k t", k=4)
        dst = hT[:, g * 4:(g + 1) * 4, tcol:tcol + 128]
        if eng == 'act':
            P.I('act', 'activation', reads=[bk], writes=[hT], out=dst, in_=src, func=AF.Copy)
        else:
            P.I('dve', 'tensor_copy', reads=[bk], writes=[hT], out=dst, in_=src)


def make_AB(P, A, B, gain_d, gain_row, modv_d, off_shift, off_scale, tmp):
    bcast_row(P, A, A[:], gain_d, gain_row)
    if modv_d is not None:
        bcast_row(P, tmp, tmp[:], modv_d, modv_d[0:1, off_scale:off_scale + D])
        bcast_row(P, B, B[:], modv_d, modv_d[0:1, off_shift:off_shift + D])
        P.I('dve', 'scalar_tensor_tensor', reads=[tmp, A], writes=[A], out=A[:], in0=tmp[:], scalar=1.0,
            in1=A[:], op0=ALU.add, op1=ALU.mult)


def build_mod():
    nc = bass.Bass("TRN2", target_bir_lowering=False)
    P = Prog(nc)
    c_d = P.dram("c", [1, D], F32, kind="ExternalInput")
    w_d = P.dram("w", [D, MODN], F32, kind="ExternalInput")
    b_d = P.dram("b", [1, MODN], F32, kind="ExternalInput")
    o_d = P.dram("modv", [1, MODN], F32, kind="ExternalOutput")
    ca = P.sbuf([128, 16], F32, name="ca")
    P.dma('sp', ca[:], c_d[0:1, :].rearrange("o (c p) -> p (o c)", p=128), reads=[c_d], writes=[ca],
          allow_slow_non_contiguous=True)
    P.I('act', 'activation', reads=[ca], writes=[ca], out=ca[:], in_=ca[:], func=AF.Silu)
    wt = [P.sbuf([128, 16, 512], F32, name="wt%d" % i) for i in range(2)]
    bt = [P.sbuf([1, 512], F32, name="bt%d" % i) for i in range(2)]
    ot = [P.sbuf([1, 512], F32, name="ot%d" % i) for i in range(2)]
    ps = [P.psum([128, 512], F32, name="ps%d" % i) for i in range(2)]
    for n in range(MODN // 512):
        w, b_, o, p_ = wt[n % 2], bt[n % 2], ot[n % 2], ps[n % 2]
        P.dma('sp' if n % 2 == 0 else 'act', w[:], w_d[:, n * 512:(n + 1) * 512].rearrange("(c p) n -> p c n", p=128),
              reads=[w_d], writes=[w])
        P.dma('pool', b_[:], b_d[0:1, n * 512:(n + 1) * 512], reads=[b_d], writes=[b_])
        for c in range(16):
            P.I('pe', 'matmul', reads=[ca, w], writes=[p_], out=p_[0:1, :], lhsT=ca[:, c:c + 1], rhs=w[:, c, :],
                start=(c == 0), stop=(c == 15))
        P.I('dve', 'tensor_tensor', reads=[p_, b_], writes=[o], out=o[:], in0=p_[0:1, :], in1=b_[:], op=ALU.add)
        P.dma('sp', o_d[0:1, n * 512:(n + 1) * 512], o[:], reads=[o], writes=[o_d])
    P.finish()
    return nc


def gla_a(P, cx, l, x_d, modv_d, nmix_d, win_d, wg2_d, bg2_d, oloc_d, gs_d, qc_d, send_d):
    bk = cx.bank
    A = P.sbuf([128, D], F32, name="A"); B = P.sbuf([128, D], F32, name="B");
    off = l * 12288
    PW = 256
    TP = 2
    hT = P.sbuf([128, 16, PW], BF16, name="hT")
    xt = [P.sbuf([128, D], F32, name="xt0")] * 2
    h = [P.sbuf([128, D], F32, name="h0")] * 2
    make_AB(P, A, B, nmix_d, nmix_d[l:l + 1, :], modv_d, off, off + D, h[0])
    junk = P.sbuf([128, D], BF16, name="junk")
    ss = P.sbuf([128, 4], F32, name="ss"); rstd = P.sbuf([128, 4], F32, name="rstd")
    wblk = [P.sbuf([128, 16, 512], BF16, name="wblk%d" % i) for i in range(2)]
    wgl = P.sbuf([128, 16, 16], BF16, name="wgl")
    P.dma('pool', wgl[:], win_d[l, :, 6144:6160].rearrange("(c p) n -> p c n", p=128), reads=[win_d], writes=[wgl])
    wg2 = P.sbuf([17, 1024], F32, name="wg2")
    P.dma('sp', wg2[0:16, :], wg2_d[l, :, :], reads=[wg2_d], writes=[wg2])
    P.dma('sp', wg2[16:17, :], bg2_d[l:l + 1, :], reads=[bg2_d], writes=[wg2])
    glT = P.sbuf([17, PW], F32, name="glT")
    P.I('pool', 'memset', writes=[glT], ap=glT[:], constant=1.0)
    qT = P.sbuf([128, 8, PW], BF16, name="qT"); kT = P.sbuf([128, 8, PW], BF16, name="kT")
    ktm = [P.sbuf([128, 1024], F32, name="ktm%d" % i) for i in range(TP)]
    vbf = [P.sbuf([128, 2048], BF16, name="vbf%d" % i) for i in range(TP)]
    gst = [P.sbuf([128, 512], F32, name="gst%d" % i) for i in range(2)]
    ez = P.sbuf([128, 1024], F32, name="ez"); la = P.sbuf([128, 1024], F32, name="la")
    EA = P.sbuf([128, 8, 128], F32, name="EA"); EB = P.sbuf([128, 8, 128], F32, name="EB")
    ER = P.sbuf([128, 1024], F32, name="ER")
    QA = P.sbuf([128, 8, 128], BF16, name="QA"); QB = P.sbuf([128, 8, 128], BF16, name="QB")
    KA = P.sbuf([128, 8, 128], BF16, name="KA"); KB = P.sbuf([128, 8, 128], BF16, name="KB")
    KE = P.sbuf([128, 1024], BF16, name="KE")
    QC = P.sbuf([128, 8, 128], F32, name="QC")
    cumP = P.sbuf([128, 8], F32, name="cumP"); expP = P.sbuf([128, 8], F32, name="expP")
    P.I('pool', 'memset', writes=[cumP], ap=cumP[:], constant=0.0)
    t1 = P.sbuf([128, 4, 128], F32, name="t1"); t2 = P.sbuf([128, 4, 128], F32, name="t2")
    PT = P.sbuf([128, 4, 128], BF16, name="PT")
    S32 = [P.sbuf([128, 512], F32, name="S32_%d" % i) for i in range(8)]
    Sbf = [P.sbuf([128, 512], BF16, name="Sbf_%d" % i) for i in range(8)]
    for i in range(8):
        P.I('pool', 'memset', writes=[S32[i]], ap=S32[i][:], constant=0.0)
        P.I('pool', 'memset', writes=[Sbf[i]], ap=Sbf[i][:], constant=0.0)
    ot = [P.sbuf([128, 2048], F32, name="ot0")] * 2
    m1b = cx.c(C_M1).unsqueeze(1).to_broadcast([128, 4, 128])
    m2b = cx.c(C_M2).unsqueeze(1).to_broadcast([128, 4, 128])
    nb = 0
    for ps_ in range(NT // TP):
        for t in range(TP):
            g = ps_ * TP + t
            P.dma('sp', xt[t % 2][:], x_d[g * 128:(g + 1) * 128, :], reads=[x_d], writes=[xt[t % 2]])
            normmod_T(P, cx, xt[t % 2], A, B, h[t % 2], junk, ss, rstd, hT, t * 128, [bk[0], bk[1]])
        for cb in range(12):
            w = wblk[nb % 2]; nb += 1
            P.dma('pool', w[:], win_d[l, :, cb * 512:(cb + 1) * 512].rearrange("(c p) n -> p c n", p=128),
                  reads=[win_d], writes=[w])
            if cb < 4:
                dstT = qT if cb < 2 else kT
                for m in range(4):
                    b_ = bk[(cb * 4 + m) % 2]
                    for c in range(16):
                        P.I('pe', 'matmul', reads=[w, hT], writes=[b_], out=b_[:, 0:PW], lhsT=w[:, c, m * 128:(m + 1) * 128],
                            rhs=hT[:, c, :], start=(c == 0), stop=(c == 15))
                    P.I('act', 'activation', reads=[b_], writes=[dstT], out=dstT[:, (cb % 2) * 4 + m, :], in_=b_[:, 0:PW],
                        func=AF.Copy)
            if cb >= 2:
                for t in range(TP):
                    g = ps_ * TP + t
                    b_ = bk[2 + (t % 2)]
                    for c in range(16):
                        P.I('pe', 'matmul', reads=[w, hT], writes=[b_], out=b_[:], lhsT=hT[:, c, t * 128:(t + 1) * 128],
                            rhs=w[:, c, :], start=(c == 0), stop=(c == 15))
                    if cb < 4:
                        P.I('dve', 'tensor_copy', reads=[b_], writes=[ktm[t]], out=ktm[t][:, (cb - 2) * 512:(cb - 1) * 512],
                            in_=b_[:])
                    elif cb < 8:
                        P.I('dve', 'tensor_copy', reads=[b_], writes=[vbf[t]], out=vbf[t][:, (cb - 4) * 512:(cb - 3) * 512],
                            in_=b_[:])
                    else:
                        go = gst[(cb * 4 + t) % 2]
                        P.I('act', 'activation', reads=[b_], writes=[go], out=go[:], in_=b_[:], func=AF.Silu)
                        P.dma('sp', gs_d[g * 128:(g + 1) * 128, (cb - 8) * 512:(cb - 7) * 512], go[:], reads=[go],
                              writes=[gs_d])
        for c in range(16):
            P.I('pe', 'matmul', reads=[wgl, hT], writes=[bk[0]], out=bk[0][0:16, 0:PW], lhsT=wgl[:, c, :], rhs=hT[:, c, :],
                start=(c == 0), stop=(c == 15))
        P.I('act', 'activation', reads=[bk[0]], writes=[glT], out=glT[0:16, :], in_=bk[0][0:16, 0:PW], func=AF.Copy)
        for t in range(TP):
            g = ps_ * TP + t
            for n in range(2):
                P.I('pe', 'matmul', reads=[glT, wg2], writes=[bk[4 + n]], out=bk[4 + n][:], lhsT=glT[:, t * 128:(t + 1) * 128],
                    rhs=wg2[:, n * 512:(n + 1) * 512], start=True, stop=True)
                P.I('act', 'activation', reads=[bk[4 + n]], writes=[ez], out=ez[:, n * 512:(n + 1) * 512], in_=bk[4 + n][:],
                    func=AF.Exp, scale=-1.0)
            P.I('act', 'activation', reads=[ez], writes=[la], out=la[:], in_=ez[:], func=AF.Ln, bias=1.0)
            for m in range(8):
                b_ = bk[4 + m // 4]
                P.I('pe', 'matmul', reads=[la, cx.cst], writes=[b_], out=b_[:, (m % 4) * 128:(m % 4 + 1) * 128],
                    lhsT=la[:, m * 128:(m + 1) * 128], rhs=cx.c(C_LINC), start=True, stop=True)
            for n in range(2):
                P.I('pe', 'matmul', reads=[la, cx.cst], writes=[bk[6 + n]], out=bk[6 + n][:], lhsT=cx.c(C_USUF),
                    rhs=la[:, n * 512:(n + 1) * 512], start=True, stop=True)
            for n in range(2):
                src = bk[4 + n][:].rearrange("p (k t) -> p k t", k=4)
                P.I('act', 'activation', reads=[bk[4 + n]], writes=[EA], out=EA[:, n * 4:(n + 1) * 4, :], in_=src, func=AF.Exp)
                P.I('act', 'activation', reads=[bk[4 + n]], writes=[EB], out=EB[:, n * 4:(n + 1) * 4, :], in_=src, func=AF.Exp,
                    scale=-1.0)
                P.I('act', 'activation', reads=[bk[6 + n]], writes=[ER], out=ER[:, n * 512:(n + 1) * 512], in_=bk[6 + n][:],
                    func=AF.Exp)
            P.I('act', 'activation', reads=[cumP], writes=[expP], out=expP[:], in_=cumP[:], func=AF.Exp)
            for n in range(2):
                P.I('dve', 'tensor_tensor', reads=[cumP, bk[4 + n]], writes=[cumP], out=cumP[:, n * 4:(n + 1) * 4],
                    in0=cumP[:, n * 4:(n + 1) * 4],
                    in1=bk[4 + n][:].rearrange("p (k t) -> p k t", k=4)[:, :, 127], op=ALU.add)
            qs = qT[:, :, t * 128:(t + 1) * 128]
            ks = kT[:, :, t * 128:(t + 1) * 128]
            P.I('dve', 'scalar_tensor_tensor', reads=[qT, EA], writes=[QA], out=QA[:], in0=qs, scalar=0.0625, in1=EA[:],
                op0=ALU.mult, op1=ALU.mult)
            P.I('dve', 'scalar_tensor_tensor', reads=[qT, EB], writes=[QB], out=QB[:], in0=qs, scalar=0.0625, in1=EB[:],
                op0=ALU.mult, op1=ALU.mult)
            P.I('pool', 'tensor_tensor', reads=[kT, EA], writes=[KA], out=KA[:], in0=ks, in1=EA[:], op=ALU.mult)
            P.I('pool', 'tensor_tensor', reads=[kT, EB], writes=[KB], out=KB[:], in0=ks, in1=EB[:], op=ALU.mult)
            P.I('pool', 'tensor_tensor', reads=[ktm[t], ER], writes=[KE], out=KE[:], in0=ktm[t][:], in1=ER[:], op=ALU.mult)
            P.I('dve', 'scalar_tensor_tensor', reads=[qT, EA, expP], writes=[QC], out=QC[:], in0=qs, scalar=0.0625, in1=EA[:],
                op0=ALU.mult, op1=ALU.mult)
            P.I('dve', 'tensor_tensor', reads=[QC, expP], writes=[QC], out=QC[:], in0=QC[:],
                in1=expP[:].unsqueeze(2).to_broadcast([128, 8, 128]), op=ALU.mult)
            P.dma('sp', qc_d[:, g * 128:(g + 1) * 128].rearrange("(m p) t -> p m t", p=128), QC[:], reads=[QC], writes=[qc_d])
            for hd in range(4):
                for j in range(2):
                    m = 2 * hd + j
                    P.I('pe', 'matmul', reads=[KB, QA], writes=[bk[0]], out=bk[0][:, hd * 128:(hd + 1) * 128], lhsT=KB[:, m, :],
                        rhs=QA[:, m, :], start=(j == 0), stop=(j == 1))
                for j in range(2):
                    m = 2 * hd + j
                    P.I('pe', 'matmul', reads=[KA, QB], writes=[bk[1]], out=bk[1][:, hd * 128:(hd + 1) * 128], lhsT=KA[:, m, :],
                        rhs=QB[:, m, :], start=(j == 0), stop=(j == 1))
            P.I('dve', 'tensor_tensor', reads=[bk[0], cx.cst], writes=[t1], out=t1[:],
                in0=bk[0][:].rearrange("p (k t) -> p k t", k=4), in1=m1b, op=ALU.mult)
            P.I('dve', 'tensor_tensor', reads=[bk[1], cx.cst], writes=[t2], out=t2[:],
                in0=bk[1][:].rearrange("p (k t) -> p k t", k=4), in1=m2b, op=ALU.mult)
            P.I('pool', 'tensor_tensor', reads=[t1, t2], writes=[PT], out=PT[:], in0=t1[:], in1=t2[:], op=ALU.add)
            o_ = ot[t % 2]
            for hd in range(4):
                b_ = bk[4 + hd]
                for j in range(2):
                    m = 2 * hd + j
                    P.I('pe', 'matmul', reads=[QA, Sbf[m]], writes=[b_], out=b_[:], lhsT=QA[:, m, :], rhs=Sbf[m][:],
                        start=(j == 0), stop=False)
                P.I('pe', 'matmul', reads=[PT, vbf[t]], writes=[b_], out=b_[:], lhsT=PT[:, hd, :],
                    rhs=vbf[t][:, hd * 512:(hd + 1) * 512], start=False, stop=True)
                P.I('act', 'activation', reads=[b_], writes=[o_], out=o_[:, hd * 512:(hd + 1) * 512], in_=b_[:], func=AF.Copy)
            P.dma('sp', oloc_d[g * 128:(g + 1) * 128, :], o_[:], reads=[o_], writes=[oloc_d])
            for hd in range(4):
                for j in range(2):
                    m = 2 * hd + j
                    b_ = bk[2 + (m % 2)]
                    P.I('pe', 'matmul', reads=[KE, vbf[t]], writes=[b_], out=b_[:], lhsT=KE[:, m * 128:(m + 1) * 128],
                        rhs=vbf[t][:, hd * 512:(hd + 1) * 512], start=True, stop=True)
                    P.I('dve', 'scalar_tensor_tensor', reads=[S32[m], EA, b_], writes=[S32[m]], out=S32[m][:], in0=S32[m][:],
                        scalar=EA[:, m, 127:128], in1=b_[:], op0=ALU.mult, op1=ALU.add)
                    P.I('act', 'activation', reads=[S32[m]], writes=[Sbf[m]], out=Sbf[m][:], in_=S32[m][:], func=AF.Copy)
    for m in range(8):
        P.dma('sp', send_d[m * 128:(m + 1) * 128, :], S32[m][:], reads=[S32[m]], writes=[send_d])


def gla_b(P, cx, l, x_d, xo_d, modv_d, gnorm_d, wout_d, oloc_d, gs_d, qc_d, sprev_d):
    bk = cx.bank
    off = l * 12288
    gate = P.sbuf([128, D], F32, name="gate")
    bcast_row(P, gate, gate[:], modv_d, modv_d[0:1, off + 2 * D:off + 3 * D])
    gn = P.sbuf([128, 512], F32, name="gn")
    bcast_row(P, gn, gn[:], gnorm_d, gnorm_d[l:l + 1, :])
    wout = P.sbuf([128, 16, D], BF16, name="wout")
    for n in range(4):
        P.dma('pool', wout[:, :, n * 512:(n + 1) * 512], wout_d[l, :, n * 512:(n + 1) * 512].rearrange("(c p) n -> p c n", p=128),
              reads=[wout_d], writes=[wout])
    sp = P.sbuf([128, 8, 512], BF16, name="sprev")
    P.dma('pool', sp[:], sprev_d[:, :].rearrange("(m p) n -> p m n", p=128), reads=[sprev_d], writes=[sp])
    qc = P.sbuf([128, 8, 128], BF16, name="qcb")
    o = P.sbuf([128, D], F32, name="o"); gs = P.sbuf([128, D], F32, name="gs"); xt = P.sbuf([128, D], F32, name="xtb")
    on = P.sbuf([128, D], F32, name="on"); junk = P.sbuf([128, 512], BF16, name="junkb")
    onT = P.sbuf([128, 16, 128], BF16, name="onT")
    ss = P.sbuf([128, 4], F32, name="ssb"); rstd = P.sbuf([128, 4], F32, name="rstdb")
    for g in range(NT):
        rows = slice(g * 128, (g + 1) * 128)
        P.dma('sp', o[:], oloc_d[rows, :], reads=[oloc_d], writes=[o])
        P.dma('act', gs[:], gs_d[rows, :], reads=[gs_d], writes=[gs])
        P.dma('sp', xt[:], x_d[rows, :], reads=[x_d], writes=[xt])
        P.dma('pool', qc[:], qc_d[:, rows].rearrange("(m p) t -> p m t", p=128), reads=[qc_d], writes=[qc])
        for hd in range(4):
            b_ = bk[hd]
            for j in range(2):
                m = 2 * hd + j
                P.I('pe', 'matmul', reads=[qc, sp], writes=[b_], out=b_[:], lhsT=qc[:, m, :], rhs=sp[:, m, :],
                    start=(j == 0), stop=(j == 1))
            osl = o[:, hd * 512:(hd + 1) * 512]
            P.I('dve', 'tensor_tensor', reads=[o, b_], writes=[o], out=osl, in0=osl, in1=b_[:], op=ALU.add)
            P.I('act', 'activation', reads=[o], writes=[junk, ss], out=junk[:], in_=osl, func=AF.Square,
                accum_out=ss[:, hd:hd + 1])
        rstd_from_ss(P, ss, rstd, 4, 1.0 / 512)
        for hd in range(4):
            P.I('dve', 'scalar_tensor_tensor', reads=[o, rstd, gn], writes=[on], out=on[:, hd * 512:(hd + 1) * 512],
                in0=o[:, hd * 512:(hd + 1) * 512], scalar=rstd[:, hd:hd + 1], in1=gn[:], op0=ALU.mult, op1=ALU.mult)
        P.I('pool', 'tensor_tensor', reads=[on, gs], writes=[on], out=on[:], in0=on[:], in1=gs[:], op=ALU.mult)
        transpose_T(P, cx, on, onT, 0, [bk[4], bk[5]])
        for n in range(4):
            b_ = bk[n]
            for c in range(16):
                P.I('pe', 'matmul', reads=[onT, wout], writes=[b_], out=b_[:], lhsT=onT[:, c, :], rhs=wout[:, c, n * 512:(n + 1) * 512],
                    start=(c == 0), stop=(c == 15))
            ysl = on[:, n * 512:(n + 1) * 512]
            P.I('dve', 'tensor_tensor', reads=[b_, gate], writes=[on], out=ysl, in0=b_[:], in1=gate[:, n * 512:(n + 1) * 512],
                op=ALU.mult)
        P.I('pool', 'tensor_tensor', reads=[on, xt], writes=[xt], out=xt[:], in0=on[:], in1=xt[:], op=ALU.add)
        P.dma('sp', xo_d[rows, :], xt[:], reads=[xt], writes=[xo_d])


def new_prog():
    nc = bass.Bass("TRN2", target_bir_lowering=False)
    return nc, Prog(nc)


def decl_common(P):
    d = {}
    d['cst'] = P.dram("cst", [128, NCST], F32, kind="ExternalInput")
    d['modv'] = P.dram("modv", [1, MODN], F32, kind="ExternalInput")
    return d


def build_L1():
    nc, P = new_prog()
    d = decl_common(P)
    x = P.dram("x", [TOK, D], F32, kind="ExternalInput")
    nmix = P.dram("norm_mix", [4, D], F32, kind="ExternalInput")
    win = P.dram("gla_w_in", [2, D, 6160], F32, kind="ExternalInput")
    wg2 = P.dram("gla_w_gate2", [2, 16, 1024], F32, kind="ExternalInput")
    bg2 = P.dram("gla_b_gate2", [2, 1024], F32, kind="ExternalInput")
    oloc = P.dram("oloc", [TOK, D], F32, kind="ExternalOutput")
    gs = P.dram("gs", [TOK, D], F32, kind="ExternalOutput")
    qc = P.dram("qc", [1024, TOK], F32, kind="ExternalOutput")
    send = P.dram("send", [1024, 512], F32, kind="ExternalOutput")
    cx = Ctx(P, d['cst'])
    gla_a(P, cx, 0, x, d['modv'], nmix, win, wg2, bg2, oloc, gs, qc, send)
    P.finish()
    return nc


def build_Lb_test():
    nc, P = new_prog()
    d = decl_common(P)
    x = P.dram("x", [TOK, D], F32, kind="ExternalInput")
    gnorm = P.dram("gla_norm", [2, 512], F32, kind="ExternalInput")
    wout = P.dram("gla_w_out", [2, D, D], F32, kind="ExternalInput")
    oloc = P.dram("oloc", [TOK, D], F32, kind="ExternalInput")
    gs = P.dram("gs", [TOK, D], F32, kind="ExternalInput")
    qc = P.dram("qc", [1024, TOK], F32, kind="ExternalInput")
    sprev = P.dram("sprev", [1024, 512], F32, kind="ExternalInput")
    xo = P.dram("xo", [TOK, D], F32, kind="ExternalOutput")
    cx = Ctx(P, d['cst'])
    gla_b(P, cx, 0, x, xo, d['modv'], gnorm, wout, oloc, gs, qc, sprev)
    P.finish()
    return nc


def peer(P, cx, l, x_d, xo_d, modv_d, nffn_d, wq_d, subk_d, uT_d, v_d):
    bk = cx.bank
    off = l * 12288 + 3 * D
    RG = P.sbuf([128, 25600], F32, name="RG")

    def carve(a, b, name, dt=F32):
        ap = RG[:, a:b]
        if dt == BF16:
            ap = ap.bitcast(BF16)
        return Buf(ap, name)
    hT = P.sbuf([128, 16, 512], BF16, name="hTp")
    acc = [P.sbuf([128, D], F32, name="acc%d" % i) for i in range(4)]
    ssb = [P.sbuf([128, 16, 128], F32, name="ssb%d" % i) for i in range(4)]
    tau = [P.sbuf([128, 8], F32, name="tau%d" % i) for i in range(4)]
    negC = [P.sbuf([128, 8], F32, name="negC%d" % i) for i in range(4)]
    subkT = P.sbuf([128, 16, 128], F32, name="subkT")
    idb = P.sbuf([128, 128], BF16, name="idb")
    P.I('dve', 'tensor_copy', reads=[cx.cst], writes=[idb], out=idb[:], in_=cx.c(C_ID))
    m16 = P.sbuf([128, 16, 16], F32, name="m16"); top = P.sbuf([128, 8, 16], F32, name="top")
    negm = P.sbuf([128, 8], F32, name="negm"); Z = P.sbuf([128, 8], F32, name="Z"); j16 = P.sbuf([128, 16], F32, name="j16")
    ss = P.sbuf([128, 4], F32, name="ssp"); rstd = P.sbuf([128, 4], F32, name="rstdp")
    stg = acc[0]
    P.dma('sp', stg[:].rearrange("p (a n) -> p a n", a=16), subk_d[l].rearrange("h q n d -> n (h q) d"), reads=[subk_d], writes=[stg])
    for g4 in range(4):
        b_ = bk[g4 % 2]
        for k in range(4):
            hp = g4 * 4 + k
            P.I('pe', 'transpose', reads=[stg, cx.cst], writes=[b_], out=b_[:, k * 128:(k + 1) * 128],
                in_=stg[:, hp * 128:(hp + 1) * 128], identity=cx.c(C_ID))
        P.I('act', 'activation', reads=[b_], writes=[subkT], out=subkT[:, g4 * 4:(g4 + 1) * 4, :],
            in_=b_[:].rearrange("p (k t) -> p k t", k=4), func=AF.Copy)
    P.barrier()
    for ps_ in range(4):
        xt = carve(0, 2048, "xt"); h = carve(2048, 4096, "h"); A = carve(4096, 6144, "A"); B = carve(6144, 8192, "B")
        qT = carve(8192, 16384, "qT"); cand = carve(16384, 18432, "cand"); candw = carve(18432, 20480, "candw")
        wqb = [carve(20480, 22528, "wq0", BF16), carve(22528, 24576, "wq1", BF16)]
        junk = carve(24576, 25600, "junk", BF16)
        make_AB(P, A, B, nffn_d, nffn_d[l:l + 1, :], modv_d, off, off + D, h)
        for t in range(4):
            g = ps_ * 4 + t
            P.dma('sp', xt[:], x_d[g * 128:(g + 1) * 128, :], reads=[x_d], writes=[xt])
            normmod_T(P, cx, xt, A, B, h, junk, ss, rstd, hT, t * 128, [bk[0], bk[1]])
        qTv = qT[:].rearrange("p (a n) -> p a n", a=16)
        for cb in range(8):
            w = wqb[cb % 2]
            wv = w[:].rearrange("p (c n) -> p c n", c=16)
            P.dma('pool', wv, wq_d[l, :, cb * 256:(cb + 1) * 256].rearrange("(c p) n -> p c n", p=128), reads=[wq_d], writes=[w])
            for m in range(2):
                b_ = bk[2 + (cb * 2 + m) % 2]
                for c in range(16):
                    P.I('pe', 'matmul', reads=[w, hT], writes=[b_], out=b_[:], lhsT=wv[:, c, m * 128:(m + 1) * 128], rhs=hT[:, c, :],
                        start=(c == 0), stop=(c == 15))
                P.I('act', 'activation', reads=[b_], writes=[qT], out=qTv[:, cb * 2 + m, :], in_=b_[:], func=AF.Copy)
        hv = h[:].rearrange("p (a n) -> p a n", a=16)
        cv4 = cand[:].rearrange("p (h a b) -> p h a b", h=8, a=16)
        cv = cand[:].rearrange("p (h n) -> p h n", h=8)
        cwv = candw[:].rearrange("p (h n) -> p h n", h=8)
        for t in range(4):
            s_ = ssb[t]
            for hp in range(16):
                b_ = bk[4 + hp // 4]
                P.I('pe', 'matmul', reads=[qT, subkT], writes=[b_], out=b_[:, (hp % 4) * 128:(hp % 4 + 1) * 128],
                    lhsT=qTv[:, hp, t * 128:(t + 1) * 128], rhs=subkT[:, hp, :], start=True, stop=True)
            for g4 in range(4):
                P.I('act', 'activation', reads=[bk[4 + g4]], writes=[s_], out=s_[:, g4 * 4:(g4 + 1) * 4, :],
                    in_=bk[4 + g4][:].rearrange("p (k t) -> p k t", k=4), func=AF.Copy)
            for hp in range(16):
                P.I('dve', 'max', reads=[s_], writes=[m16], out=m16[:, hp, 0:8], in_=s_[:, hp, :])
                P.I('dve', 'match_replace', reads=[s_, m16], writes=[h], out=hv[:, hp, :], in_to_replace=m16[:, hp, 0:8],
                    in_values=s_[:, hp, :], imm_value=-1e30)
                P.I('dve', 'max', reads=[h], writes=[m16], out=m16[:, hp, 8:16], in_=hv[:, hp, :])
            m16v = m16[:].rearrange("p (h q) k -> p h q k", q=2)
            P.I('dve', 'tensor_tensor', reads=[m16], writes=[cand], out=cv4,
                in0=m16v[:, :, 0, :].unsqueeze(3).to_broadcast([128, 8, 16, 16]),
                in1=m16v[:, :, 1, :].unsqueeze(2).to_broadcast([128, 8, 16, 16]), op=ALU.add)
            for hh in range(8):
                P.I('dve', 'max', reads=[cand], writes=[top], out=top[:, hh, 0:8], in_=cv[:, hh, :])
                P.I('dve', 'match_replace', reads=[cand, top], writes=[candw], out=cwv[:, hh, :], in_to_replace=top[:, hh, 0:8],
                    in_values=cv[:, hh, :], imm_value=-1e30)
                P.I('dve', 'max', reads=[candw], writes=[top], out=top[:, hh, 8:16], in_=cwv[:, hh, :])
            P.I('dve', 'tensor_scalar', reads=[top], writes=[negm], out=negm[:], in0=top[:, :, 0], scalar1=-1.0, scalar2=0.0,
                op0=ALU.mult, op1=ALU.add)
            for hh in range(8):
                P.I('act', 'activation', reads=[top, negm], writes=[j16, Z], out=j16[:], in_=top[:, hh, :], func=AF.Exp,
                    bias=negm[:, hh:hh + 1], scale=1.0, accum_out=Z[:, hh:hh + 1])
            P.I('act', 'activation', reads=[Z], writes=[Z], out=Z[:], in_=Z[:], func=AF.Ln)
            P.I('dve', 'tensor_tensor', reads=[negm, Z], writes=[negC[t]], out=negC[t][:], in0=negm[:], in1=Z[:], op=ALU.subtract)
            P.I('dve', 'tensor_scalar', reads=[top], writes=[tau[t]], out=tau[t][:], in0=top[:, :, 15], scalar1=1.0, scalar2=-1e-5,
                op0=ALU.mult, op1=ALU.add)
        P.barrier()
        uTb = [carve(0, 4096, "uT0", BF16), carve(4096, 8192, "uT1", BF16)]
        vb = [carve(8192, 12288, "v0", BF16), carve(12288, 16384, "v1", BF16)]
        gAb = [carve(16384, 17408, "gA0", BF16), carve(17408, 18432, "gA1", BF16)]
        Sb = [carve(18432, 18944, "S0"), carve(18944, 19456, "S1")]
        Xb = [carve(19456, 19968, "X0"), carve(19968, 20480, "X1")]
        Tb = [carve(20480, 22528, "T0", BF16), carve(22528, 24576, "T1", BF16)]
        GTb = [carve(24576, 24640, "GT0", BF16), carve(24640, 24704, "GT1", BF16)]
        nG = 0
        for g in range(32):
            uT = uTb[g % 2]; v = vb[g % 2]; gA = gAb[g % 2]
            uTv = uT[:].rearrange("p (c e) -> p c e", c=16)
            vv = v[:].rearrange("p (i n) -> p i n", i=4)
            gAv = gA[:].rearrange("p (i t) -> p i t", i=4)
            P.dma('pool', uTv, uT_d[:, g * 512:(g + 1) * 512].rearrange("(c p) e -> p c e", p=128), reads=[uT_d], writes=[uT])
            P.dma('pool', vv, v_d[g * 512:(g + 1) * 512, :].rearrange("(i j) n -> j i n", j=128), reads=[v_d], writes=[v])
            for i in range(4):
                b_ = bk[4 + i % 2]
                for c in range(16):
                    P.I('pe', 'matmul', reads=[uT, hT], writes=[b_], out=b_[:], lhsT=uTv[:, c, i * 128:(i + 1) * 128], rhs=hT[:, c, :],
                        start=(c == 0), stop=(c == 15))
                P.I('act', 'activation', reads=[b_], writes=[gA], out=gAv[:, i, :], in_=b_[:], func=AF.Gelu_apprx_tanh)
            for t in range(4):
                s_ = ssb[t]
                T = Tb[(g * 4 + t) % 2]
                Tv = T[:].rearrange("p (h i j) -> p h i j", h=8, i=4)
                wb = bk[6 + (g * 4 + t) % 2]
                for hh in range(8):
                    S = Sb[hh % 2]; X = Xb[hh % 2]
                    Sv = S[:].rearrange("p (i j) -> p i j", i=4)
                    Xv = X[:].rearrange("p (i j) -> p i j", i=4)
                    P.I('pool', 'tensor_tensor', reads=[s_], writes=[S], out=Sv,
                        in0=s_[:, 2 * hh, g * 4:(g + 1) * 4].unsqueeze(2).to_broadcast([128, 4, 128]),
                        in1=s_[:, 2 * hh + 1, :].unsqueeze(1).to_broadcast([128, 4, 128]), op=ALU.add)
                    P.I('act', 'activation', reads=[S, negC[t]], writes=[X], out=Xv, in_=Sv, func=AF.Exp,
                        bias=negC[t][:, hh:hh + 1], scale=1.0)
                    P.I('dve', 'scalar_tensor_tensor', reads=[S, tau[t], X], writes=[T], out=Tv[:, hh, :, :], in0=Sv,
                        scalar=tau[t][:, hh:hh + 1], in1=Xv, op0=ALU.is_ge, op1=ALU.mult)
                for i in range(4):
                    for hh in range(8):
                        P.I('pe', 'matmul', reads=[T, idb], writes=[wb], out=wb[:, i * 128:(i + 1) * 128], lhsT=Tv[:, hh, i, :],
                            rhs=idb[:], start=(hh == 0), stop=(hh == 7))
                    GT = GTb[nG % 2]; nG += 1
                    P.I('dve', 'tensor_tensor', reads=[wb, gA], writes=[GT], out=GT[:], in0=wb[:, i * 128:(i + 1) * 128],
                        in1=gAv[:, i, t * 128:(t + 1) * 128], op=ALU.mult)
                    for n in range(4):
                        P.I('pe', 'matmul', reads=[GT, v], writes=[bk[n]], out=bk[n][:], lhsT=GT[:], rhs=vv[:, i, n * 512:(n + 1) * 512],
                            start=(i == 0), stop=(i == 3))
                for n in range(4):
                    asl = acc[t][:, n * 512:(n + 1) * 512]
                    if g == 0:
                        P.I('dve', 'tensor_copy', reads=[bk[n]], writes=[acc[t]], out=asl, in_=bk[n][:])
                    else:
                        P.I('dve', 'tensor_tensor', reads=[bk[n], acc[t]], writes=[acc[t]], out=asl, in0=asl, in1=bk[n][:], op=ALU.add)
        P.barrier()
        gate = carve(0, 2048, "gate"); xt5 = carve(2048, 4096, "xt5")
        bcast_row(P, gate, gate[:], modv_d, modv_d[0:1, off + 2 * D:off + 3 * D])
        for t in range(4):
            g = ps_ * 4 + t
            P.dma('sp', xt5[:], x_d[g * 128:(g + 1) * 128, :], reads=[x_d], writes=[xt5])
            P.I('dve', 'tensor_tensor', reads=[acc[t], gate], writes=[acc[t]], out=acc[t][:], in0=acc[t][:], in1=gate[:], op=ALU.mult)
            P.I('pool', 'tensor_tensor', reads=[acc[t], xt5], writes=[xt5], out=xt5[:], in0=acc[t][:], in1=xt5[:], op=ALU.add)
            P.dma('sp', xo_d[g * 128:(g + 1) * 128, :], xt5[:], reads=[xt5], writes=[xo_d])
        P.barrier()


def build_peer(l):
    nc, P = new_prog()
    d = decl_common(P)
    x = P.dram("x", [TOK, D], F32, kind="ExternalInput")
    nffn = P.dram("norm_ffn", [4, D], F32, kind="ExternalInput")
    wq = P.dram("peer_w_q", [4, D, D], F32, kind="ExternalInput")
    subk = P.dram("peer_subkeys", [4, 8, 2, 128, 128], F32, kind="ExternalInput")
    uT = P.dram("uT", [D, 16384], F32, kind="ExternalInput")
    v = P.dram("v", [16384, D], F32, kind="ExternalInput")
    xo = P.dram("xo", [TOK, D], F32, kind="ExternalOutput")
    cx = Ctx(P, d['cst'])
    peer(P, cx, l, x, xo, d['modv'], nffn, wq, subk, uT, v)
    P.finish()
    return nc


def build_gla_a(l):
    nc, P = new_prog()
    d = decl_common(P)
    x = P.dram("x", [TOK, D], F32, kind="ExternalInput")
    nmix = P.dram("norm_mix", [4, D], F32, kind="ExternalInput")
    win = P.dram("gla_w_in", [2, D, 6160], F32, kind="ExternalInput")
    wg2 = P.dram("gla_w_gate2", [2, 16, 1024], F32, kind="ExternalInput")
    bg2 = P.dram("gla_b_gate2", [2, 1024], F32, kind="ExternalInput")
    oloc = P.dram("oloc", [TOK, D], F32, kind="ExternalOutput")
    gs = P.dram("gs", [TOK, D], F32, kind="ExternalOutput")
    qc = P.dram("qc", [1024, TOK], F32, kind="ExternalOutput")
    send = P.dram("send", [1024, 512], F32, kind="ExternalOutput")
    cx = Ctx(P, d['cst'])
    gla_a(P, cx, l, x, d['modv'], nmix, win, wg2, bg2, oloc, gs, qc, send)
    P.finish()
    return nc


def build_gla_b(l):
    nc, P = new_prog()
    d = decl_common(P)
    x = P.dram("x", [TOK, D], F32, kind="ExternalInput")
    gnorm = P.dram("gla_norm", [2, 512], F32, kind="ExternalInput")
    wout = P.dram("gla_w_out", [2, D, D], F32, kind="ExternalInput")
    oloc = P.dram("oloc", [TOK, D], F32, kind="ExternalInput")
    gs = P.dram("gs", [TOK, D], F32, kind="ExternalInput")
    qc = P.dram("qc", [1024, TOK], F32, kind="ExternalInput")
    sprev = P.dram("sprev", [1024, 512], F32, kind="ExternalInput")
    xo = P.dram("xo", [TOK, D], F32, kind="ExternalOutput")
    cx = Ctx(P, d['cst'])
    gla_b(P, cx, l, x, xo, d['modv'], gnorm, wout, oloc, gs, qc, sprev)
    P.finish()
    return nc


def build_kv():
    nc, P = new_prog()
    d = decl_common(P)
    x_d = P.dram("x", [TOK, D], F32, kind="ExternalInput")
    kvn = P.dram("kv_norm", [1, D], F32, kind="ExternalInput")
    wkv = P.dram("fox_w_kv", [D, 4112], F32, kind="ExternalInput")
    bf_d = P.dram("fox_b_f", [1, 16], F32, kind="ExternalInput")
    KT_d = P.dram("KT", [D, TOK], F32, kind="ExternalOutput")
    V_d = P.dram("V", [TOK, D], F32, kind="ExternalOutput")
    lf_d = P.dram("logf", [TOK, 16], F32, kind="ExternalOutput")
    cx = Ctx(P, d['cst']); bk = cx.bank
    A = P.sbuf([128, D], F32, name="A"); B = P.sbuf([128, D], F32, name="B")
    xt = P.sbuf([128, D], F32, name="xt"); h = P.sbuf([128, D], F32, name="h"); junk = P.sbuf([128, D], BF16, name="junk")
    make_AB(P, A, B, kvn, kvn[0:1, :], d['modv'], 49152, 51200, h)
    hT = P.sbuf([128, 16, 512], BF16, name="hT")
    ss = P.sbuf([128, 4], F32, name="ss"); rstd = P.sbuf([128, 4], F32, name="rstd")
    wblk = [P.sbuf([128, 16, 512], BF16, name="wb%d" % i) for i in range(2)]
    wfl = P.sbuf([128, 16, 16], BF16, name="wfl")
    P.dma('pool', wfl[:], wkv[:, 4096:4112].rearrange("(c p) n -> p c n", p=128), reads=[wkv], writes=[wfl])
    bfb = P.sbuf([128, 16], F32, name="bfb")
    bcast_row(P, bfb, bfb[:], bf_d, bf_d[0:1, :])
    ev = [P.sbuf([128, 512], F32, name="ev%d" % i) for i in range(2)]
    z16 = P.sbuf([128, 16], F32, name="z16")
    ne = 0
    for ps_ in range(4):
        cols = slice(ps_ * 512, (ps_ + 1) * 512)
        for t in range(4):
            g = ps_ * 4 + t
            P.dma('sp', xt[:], x_d[g * 128:(g + 1) * 128, :], reads=[x_d], writes=[xt])
            normmod_T(P, cx, xt, A, B, h, junk, ss, rstd, hT, t * 128, [bk[0], bk[1]])
        for cb in range(8):
            w = wblk[cb % 2]
            P.dma('pool', w[:], wkv[:, cb * 512:(cb + 1) * 512].rearrange("(c p) n -> p c n", p=128), reads=[wkv], writes=[w])
            if cb < 4:
                for m in range(4):
                    b_ = bk[2 + m % 2]
                    for c in range(16):
                        P.I('pe', 'matmul', reads=[w, hT], writes=[b_], out=b_[:], lhsT=w[:, c, m * 128:(m + 1) * 128], rhs=hT[:, c, :],
                            start=(c == 0), stop=(c == 15))
                    e_ = ev[ne % 2]; ne += 1
                    P.I('act', 'activation', reads=[b_], writes=[e_], out=e_[:], in_=b_[:], func=AF.Copy)
                    P.dma('sp', KT_d[(cb * 4 + m) * 128:(cb * 4 + m + 1) * 128, cols], e_[:], reads=[e_], writes=[KT_d])
            else:
                for t in range(4):
                    g = ps_ * 4 + t
                    b_ = bk[4 + t % 2]
                    for c in range(16):
                        P.I('pe', 'matmul', reads=[w, hT], writes=[b_], out=b_[:], lhsT=hT[:, c, t * 128:(t + 1) * 128], rhs=w[:, c, :],
                            start=(c == 0), stop=(c == 15))
                    e_ = ev[ne % 2]; ne += 1
                    P.I('dve', 'tensor_copy', reads=[b_], writes=[e_], out=e_[:], in_=b_[:])
                    P.dma('sp', V_d[g * 128:(g + 1) * 128, (cb - 4) * 512:(cb - 3) * 512], e_[:], reads=[e_], writes=[V_d])
        for t in range(4):
            g = ps_ * 4 + t
            for c in range(16):
                P.I('pe', 'matmul', reads=[wfl, hT], writes=[bk[6]], out=bk[6][:, 0:16], lhsT=hT[:, c, t * 128:(t + 1) * 128], rhs=wfl[:, c, :],
                    start=(c == 0), stop=(c == 15))
            P.I('dve', 'tensor_tensor', reads=[bk[6], bfb], writes=[z16], out=z16[:], in0=bk[6][:, 0:16], in1=bfb[:], op=ALU.add)
            P.I('act', 'activation', reads=[z16], writes=[z16], out=z16[:], in_=z16[:], func=AF.Exp, scale=-1.0)
            P.I('act', 'activation', reads=[z16], writes=[z16], out=z16[:], in_=z16[:], func=AF.Ln, bias=1.0)
            P.I('dve', 'tensor_scalar', reads=[z16], writes=[z16], out=z16[:], in0=z16[:], scalar1=-1.0, scalar2=0.0, op0=ALU.mult, op1=ALU.add)
            P.dma('sp', lf_d[g * 128:(g + 1) * 128, :], z16[:], reads=[z16], writes=[lf_d])
    P.finish()
    return nc


def build_fox_a(l):
    nc, P = new_prog()
    d = decl_common(P)
    bq = l - 2
    x_d = P.dram("x", [TOK, D], F32, kind="ExternalInput")
    nmix = P.dram("norm_mix", [4, D], F32, kind="ExternalInput")
    wq_d = P.dram("fox_w_q", [2, D, 2 * D], F32, kind="ExternalInput")
    KT_d = P.dram("KT", [D, 4096], F32, kind="ExternalInput")
    V_d = P.dram("V", [4096, D], F32, kind="ExternalInput")
    lf_d = P.dram("logf", [4096, 16], F32, kind="ExternalInput")
    o_d = P.dram("o", [TOK, D], F32, kind="ExternalOutput")
    sg_d = P.dram("sg", [TOK, D], F32, kind="ExternalOutput")
    cx = Ctx(P, d['cst']); bk = cx.bank
    off = l * 12288
    A = P.sbuf([128, D], F32, name="A"); B = P.sbuf([128, D], F32, name="B")
    xt = P.sbuf([128, D], F32, name="xt"); h = P.sbuf([128, D], F32, name="h"); junk = P.sbuf([128, D], BF16, name="junk")
    make_AB(P, A, B, nmix, nmix[l:l + 1, :], d['modv'], off, off + D, h)
    hT = P.sbuf([128, 16, 512], BF16, name="hT")
    ss = P.sbuf([128, 4], F32, name="ss"); rstd = P.sbuf([128, 4], F32, name="rstd")
    wblk = [P.sbuf([128, 16, 512], BF16, name="wb%d" % i) for i in range(2)]
    qTall = P.sbuf([128, 16, TOK], BF16, name="qTall")
    ev = [P.sbuf([128, 512], F32, name="ev0")] * 2
    lf = P.sbuf([128, 512], F32, name="lf"); Fw = P.sbuf([128, 512], F32, name="Fw"); Tot = P.sbuf([128, 512], F32, name="Tot")
    Pre = P.sbuf([128, 512], F32, name="Pre")
    P.dma('sp', lf[:].rearrange("s (b h) -> s b h", h=16), lf_d[:, :].rearrange("(b s) h -> s b h", s=128), reads=[lf_d], writes=[lf])
    P.I('pe', 'matmul', reads=[lf, cx.cst], writes=[bk[6]], out=bk[6][:], lhsT=cx.c(C_M1), rhs=lf[:], start=True, stop=True)
    P.I('pe', 'matmul', reads=[lf, cx.cst], writes=[bk[7]], out=bk[7][:], lhsT=cx.c(C_ONES), rhs=lf[:], start=True, stop=True)
    P.I('dve', 'tensor_copy', reads=[bk[7]], writes=[Tot], out=Tot[:], in_=bk[7][:])
    P.I('pool', 'memset', writes=[Pre], ap=Pre[:], constant=0.0)
    for b_i in range(1, 32):
        P.I('dve', 'tensor_tensor', reads=[Pre, Tot], writes=[Pre], out=Pre[:, b_i * 16:(b_i + 1) * 16], in0=Pre[:, (b_i - 1) * 16:b_i * 16],
            in1=Tot[:, (b_i - 1) * 16:b_i * 16], op=ALU.add)
    P.I('dve', 'tensor_tensor', reads=[bk[6], Pre], writes=[Fw], out=Fw[:], in0=bk[6][:], in1=Pre[:], op=ALU.add)
    Fv = Fw[:].rearrange("s (b h) -> s b h", h=16)
    Pv = Pre[:].rearrange("s (b h) -> s b h", h=16)
    ne = 0
    for ps_ in range(4):
        cols = slice(ps_ * 512, (ps_ + 1) * 512)
        for t in range(4):
            g = ps_ * 4 + t
            P.dma('sp', xt[:], x_d[g * 128:(g + 1) * 128, :], reads=[x_d], writes=[xt])
            normmod_T(P, cx, xt, A, B, h, junk, ss, rstd, hT, t * 128, [bk[0], bk[1]])
        for cb in range(8):
            w = wblk[cb % 2]
            P.dma('pool', w[:], wq_d[bq, :, cb * 512:(cb + 1) * 512].rearrange("(c p) n -> p c n", p=128), reads=[wq_d], writes=[w])
            if cb < 4:
                for m in range(4):
                    b_ = bk[2 + m % 2]
                    for c in range(16):
                        P.I('pe', 'matmul', reads=[w, hT], writes=[b_], out=b_[:], lhsT=w[:, c, m * 128:(m + 1) * 128], rhs=hT[:, c, :],
                            start=(c == 0), stop=(c == 15))
                    P.I('act', 'activation', reads=[b_], writes=[qTall], out=qTall[:, cb * 4 + m, cols], in_=b_[:], func=AF.Copy,
                        scale=float(128 ** -0.5))
            else:
                for t in range(4):
                    g = ps_ * 4 + t
                    b_ = bk[4 + t % 2]
                    for c in range(16):
                        P.I('pe', 'matmul', reads=[w, hT], writes=[b_], out=b_[:], lhsT=hT[:, c, t * 128:(t + 1) * 128], rhs=w[:, c, :],
                            start=(c == 0), stop=(c == 15))
                    e_ = ev[ne % 2]; ne += 1
                    P.I('act', 'activation', reads=[b_], writes=[e_], out=e_[:], in_=b_[:], func=AF.Sigmoid)
                    P.dma('sp', sg_d[g * 128:(g + 1) * 128, (cb - 4) * 512:(cb - 3) * 512], e_[:], reads=[e_], writes=[sg_d])
    KTh = [P.sbuf([128, 4096], BF16, name="KTh%d" % i) for i in range(2)]
    Vh = [P.sbuf([128, 32, 129], BF16, name="Vh%d" % i) for i in range(2)]
    for i in range(2):
        P.I('pool', 'memset', writes=[Vh[i]], ap=Vh[i][:], constant=1.0)
    mA = P.sbuf([128, 128], BF16, name="mA"); mB = P.sbuf([128, 128], BF16, name="mB")
    P.I('dve', 'tensor_copy', reads=[cx.cst], writes=[mA], out=mA[:], in_=cx.c(C_MA))
    P.I('dve', 'tensor_copy', reads=[cx.cst], writes=[mB], out=mB[:], in_=cx.c(C_MB))
    bias = [P.sbuf([128, 32], F32, name="bias%d" % i) for i in range(2)]
    PTb = [P.sbuf([128, 128], BF16, name="PT%d" % i) for i in range(4)]
    oh = [P.sbuf([128, 16, 128], F32, name="oh0")] * 2
    rZ = P.sbuf([128, 2], F32, name="rZ")
    npt = 0; nbi = 0; nst = 0
    for hh in range(16):
        Kt = KTh[hh % 2]; Vt = Vh[hh % 2]; o_h = oh[hh % 2]
        P.dma('pool', Kt[:], KT_d[hh * 128:(hh + 1) * 128, :], reads=[KT_d], writes=[Kt])
        P.dma('pool', Vt[:, :, 0:128], V_d[:, hh * 128:(hh + 1) * 128].rearrange("(b s) d -> s b d", s=128), reads=[V_d], writes=[Vt])
        for m in range(16):
            nblk = 2 * m + 2
            bi = bias[nbi % 2]; nbi += 1
            P.I('dve', 'scalar_tensor_tensor', reads=[Fw, Pre], writes=[bi], out=bi[:, 0:nblk], in0=Fv[:, 0:nblk, hh], scalar=-1.0,
                in1=Pv[:, 2 * m + 1, hh:hh + 1].to_broadcast([128, nblk]), op0=ALU.mult, op1=ALU.add)
            ob = bk[4 + (hh * 16 + m) % 2]
            for j in range(nblk):
                sb_ = bk[nst % 4]; nst += 1
                P.I('pe', 'matmul', reads=[Kt, qTall], writes=[sb_], out=sb_[:, 0:128], lhsT=Kt[:, j * 128:(j + 1) * 128],
                    rhs=qTall[:, hh, m * 128:(m + 1) * 128], start=True, stop=True)
                pt = PTb[npt % 4]; npt += 1
                P.I('act', 'activation', reads=[sb_, bi], writes=[pt], out=pt[:], in_=sb_[:, 0:128], func=AF.Exp, bias=bi[:, j:j + 1], scale=1.0)
                if j == nblk - 2:
                    P.I('dve', 'tensor_tensor', reads=[pt, mA], writes=[pt], out=pt[:], in0=pt[:], in1=mA[:], op=ALU.mult)
                if j == nblk - 1:
                    P.I('dve', 'tensor_tensor', reads=[pt, mB], writes=[pt], out=pt[:], in0=pt[:], in1=mB[:], op=ALU.mult)
                P.I('pe', 'matmul', reads=[pt, Vt], writes=[ob], out=ob[:, 0:129], lhsT=pt[:], rhs=Vt[:, j, :], start=(j == 0), stop=(j == nblk - 1))
            P.I('dve', 'reciprocal', reads=[ob], writes=[rZ], out=rZ[:, 0:1], in_=ob[:, 128:129])
            P.I('dve', 'tensor_scalar', reads=[ob, rZ], writes=[o_h], out=o_h[:, m, :], in0=ob[:, 0:128], scalar1=rZ[:, 0:1], scalar2=0.0,
                op0=ALU.mult, op1=ALU.add)
        P.dma('sp', o_d[:, hh * 128:(hh + 1) * 128].rearrange("(m t) d -> t m d", t=128), o_h[:], reads=[o_h], writes=[o_d])
    P.finish()
    return nc


def build_fox_b(l):
    nc, P = new_prog()
    d = decl_common(P)
    x_d = P.dram("x", [TOK, D], F32, kind="ExternalInput")
    o_d = P.dram("o", [TOK, D], F32, kind="ExternalInput")
    sg_d = P.dram("sg", [TOK, D], F32, kind="ExternalInput")
    wout_d = P.dram("fox_w_out", [2, D, D], F32, kind="ExternalInput")
    xo_d = P.dram("xo", [TOK, D], F32, kind="ExternalOutput")
    cx = Ctx(P, d['cst']); bk = cx.bank
    off = l * 12288
    gate = P.sbuf([128, D], F32, name="gate")
    bcast_row(P, gate, gate[:], d['modv'], d['modv'][0:1, off + 2 * D:off + 3 * D])
    wout = P.sbuf([128, 16, D], BF16, name="wout")
    for n in range(4):
        P.dma('pool', wout[:, :, n * 512:(n + 1) * 512], wout_d[l - 2, :, n * 512:(n + 1) * 512].rearrange("(c p) n -> p c n", p=128),
              reads=[wout_d], writes=[wout])
    o = P.sbuf([128, D], F32, name="o"); sg = P.sbuf([128, D], F32, name="sg"); xt = P.sbuf([128, D], F32, name="xt")
    onT = P.sbuf([128, 16, 128], BF16, name="onT")
    for g in range(NT):
        rows = slice(g * 128, (g + 1) * 128)
        P.dma('sp', o[:], o_d[rows, :], reads=[o_d], writes=[o])
        P.dma('act', sg[:], sg_d[rows, :], reads=[sg_d], writes=[sg])
        P.dma('sp', xt[:], x_d[rows, :], reads=[x_d], writes=[xt])
        P.I('pool', 'tensor_tensor', reads=[o, sg], writes=[o], out=o[:], in0=o[:], in1=sg[:], op=ALU.mult)
        transpose_T(P, cx, o, onT, 0, [bk[4], bk[5]])
        for n in range(4):
            b_ = bk[n]
            for c in range(16):
                P.I('pe', 'matmul', reads=[onT, wout], writes=[b_], out=b_[:], lhsT=onT[:, c, :], rhs=wout[:, c, n * 512:(n + 1) * 512],
                    start=(c == 0), stop=(c == 15))
            P.I('dve', 'tensor_tensor', reads=[b_, gate], writes=[sg], out=sg[:, n * 512:(n + 1) * 512], in0=b_[:],
                in1=gate[:, n * 512:(n + 1) * 512], op=ALU.mult)
        P.I('pool', 'tensor_tensor', reads=[sg, xt], writes=[xt], out=xt[:], in0=sg[:], in1=xt[:], op=ALU.add)
        P.dma('sp', xo_d[rows, :], xt[:], reads=[xt], writes=[xo_d])
    P.finish()
    return nc


def build_final():
    nc, P = new_prog()
    d = decl_common(P)
    x_d = P.dram("x", [TOK, D], F32, kind="ExternalInput")
    fn_d = P.dram("final_norm", [1, D], F32, kind="ExternalInput")
    o_d = P.dram("out", [TOK, D], F32, kind="ExternalOutput")
    A = P.sbuf([128, D], F32, name="A")
    bcast_row(P, A, A[:], fn_d, fn_d[0:1, :])
    xt = [P.sbuf([128, D], F32, name="xt%d" % i) for i in range(2)]
    h = [P.sbuf([128, D], F32, name="h%d" % i) for i in range(2)]
    junk = P.sbuf([128, D], BF16, name="junk")
    ss = P.sbuf([128, 4], F32, name="ss"); rstd = P.sbuf([128, 4], F32, name="rstd")
    for g in range(NT):
        x_, h_ = xt[g % 2], h[g % 2]
        P.dma('sp', x_[:], x_d[g * 128:(g + 1) * 128, :], reads=[x_d], writes=[x_])
        P.I('act', 'activation', reads=[x_], writes=[junk, ss], out=junk[:], in_=x_[:], func=AF.Square, accum_out=ss[:, 0:1])
        rstd_from_ss(P, ss, rstd, 1, 1.0 / D)
        P.I('dve', 'scalar_tensor_tensor', reads=[x_, rstd, A], writes=[h_], out=h_[:], in0=x_[:], scalar=rstd[:, 0:1], in1=A[:],
            op0=ALU.mult, op1=ALU.mult)
        P.dma('sp', o_d[g * 128:(g + 1) * 128, :], h_[:], reads=[h_], writes=[o_d])
    P.finish()
    return nc


def _run(nc, ins):
    return run_bass_kernel_spmd(nc, ins, core_ids=list(range(8))).results


def kernel(x, c, ada_w, ada_b, norm_mix, norm_ffn, gla_w_in, gla_w_gate2, gla_b_gate2, gla_norm, gla_w_out, kv_norm,
           kv_ada_w, kv_ada_b, fox_w_kv, fox_b_f, fox_w_q, fox_w_out, peer_w_q, peer_subkeys, peer_u, peer_v, final_norm):
    f = lambda a: np.ascontiguousarray(np.asarray(a, dtype=np.float32))
    x = f(x); c = f(c)
    R8 = range(8)
    cst = [make_consts(r % 2) for r in R8]
    Wm = np.ascontiguousarray(np.concatenate([f(ada_w[l]) for l in range(4)] + [f(kv_ada_w)], axis=1))
    Bm = np.ascontiguousarray(np.concatenate([f(ada_b[l]) for l in range(4)] + [f(kv_ada_b)])[None, :])
    res = _run(build_mod(), [{"c": c[r // 2:r // 2 + 1], "w": Wm, "b": Bm} for r in R8])
    modv = [res[r]["modv"] for r in R8]
    del Wm
    common = lambda r: {"cst": cst[r], "modv": modv[r]}
    xs = [np.ascontiguousarray(x[r // 2, (r % 2) * TOK:(r % 2 + 1) * TOK]) for r in R8]
    nmix = f(norm_mix); nffn = f(norm_ffn); wq_p = f(peer_w_q); subk = f(peer_subkeys)

    def run_peer(l, xs):
        uT = np.ascontiguousarray(f(peer_u[l]).T); v = f(peer_v[l])
        r_ = _run(build_peer(l), [dict(common(r), x=xs[r], norm_ffn=nffn, peer_w_q=wq_p, peer_subkeys=subk, uT=uT, v=v) for r in R8])
        return [r_[r]["xo"] for r in R8]

    win = f(gla_w_in); wg2 = f(gla_w_gate2); bg2 = f(gla_b_gate2); gnorm = f(gla_norm); gwout = f(gla_w_out)
    for l in range(2):
        ra = _run(build_gla_a(l), [dict(common(r), x=xs[r], norm_mix=nmix, gla_w_in=win, gla_w_gate2=wg2, gla_b_gate2=bg2) for r in R8])
        zero = np.zeros((1024, 512), np.float32)
        rb = _run(build_gla_b(l), [dict(common(r), x=xs[r], gla_norm=gnorm, gla_w_out=gwout, oloc=ra[r]["oloc"], gs=ra[r]["gs"],
                                        qc=ra[r]["qc"], sprev=(ra[r - 1]["send"] if r % 2 == 1 else zero)) for r in R8])
        xs = [rb[r]["xo"] for r in R8]
        del ra, rb
        xs = run_peer(l, xs)
    rk = _run(build_kv(), [dict(common(r), x=xs[r], kv_norm=f(kv_norm)[None, :], fox_w_kv=f(fox_w_kv), fox_b_f=f(fox_b_f)[None, :]) for r in R8])
    KT = [np.ascontiguousarray(np.concatenate([rk[2 * b]["KT"], rk[2 * b + 1]["KT"]], axis=1)) for b in range(4)]
    V = [np.ascontiguousarray(np.concatenate([rk[2 * b]["V"], rk[2 * b + 1]["V"]], axis=0)) for b in range(4)]
    LF = [np.ascontiguousarray(np.concatenate([rk[2 * b]["logf"], rk[2 * b + 1]["logf"]], axis=0)) for b in range(4)]
    xb = [np.concatenate([xs[2 * b], xs[2 * b + 1]], axis=0).reshape(32, 128, D) for b in range(4)]
    xs = [np.ascontiguousarray(xb[r // 2][(r % 2)::2].reshape(TOK, D)) for r in R8]
    del rk, xb
    fwq = f(fox_w_q); fwo = f(fox_w_out)
    for l in (2, 3):
        ra = _run(build_fox_a(l), [dict(common(r), x=xs[r], norm_mix=nmix, fox_w_q=fwq, KT=KT[r // 2], V=V[r // 2], logf=LF[r // 2]) for r in R8])
        rb = _run(build_fox_b(l), [dict(common(r), x=xs[r], o=ra[r]["o"], sg=ra[r]["sg"], fox_w_out=fwo) for r in R8])
        xs = [rb[r]["xo"] for r in R8]
        del ra, rb
        xs = run_peer(l, xs)
    rf = _run(build_final(), [dict(common(r), x=xs[r], final_norm=f(final_norm)[None, :]) for r in R8])
    out = np.empty((4, 32, 128, D), np.float32)
    for r in R8:
        out[r // 2, (r % 2)::2] = rf[r]["out"].reshape(16, 128, D)
    return out.reshape(4, 4096, D)
```

```python
import contextlib
import numpy as np
import concourse.bass as bass
import concourse.mybir as mybir

F32 = mybir.dt.float32
BF16 = mybir.dt.bfloat16
AF = mybir.ActivationFunctionType
ALU = mybir.AluOpType
AX = mybir.AxisListType

ENGS = ['pe', 'act', 'dve', 'pool', 'sp']
DMA_SLOTS = {'sp': 12, 'act': 4, 'pool': 6}


class Buf:
    def __init__(self, ap_full, name=""):
        self.t = ap_full
        self.name = name
        self.last_w = None
        self.readers = []

    def __getitem__(self, k):
        return self.t[k]


class Prog:
    def __init__(self, nc):
        self.nc = nc
        self.es = contextlib.ExitStack()
        self.ops = {e: [] for e in ENGS}
        self.cnt = {e: 0 for e in ENGS}
        self.seen = {e: {f: 0 for f in ENGS} for e in ENGS}
        self.sem = {}
        for e in ENGS:
            self.sem[e] = self.es.enter_context(nc.semaphore("s_" + e))
        self.dsem = {}
        self.dval = {}
        self.dseen = {e: {} for e in ENGS}
        self.drr = {q: 0 for q in DMA_SLOTS}
        for q, n in DMA_SLOTS.items():
            for i in range(n):
                key = (q, i)
                self.dsem[key] = self.es.enter_context(nc.semaphore("d_%s%d" % (q, i)))
                self.dval[key] = 0
        self.n_alloc = 0

    def sbuf(self, shape, dtype=F32, name=None, stack=None):
        self.n_alloc += 1
        name = name or ("sb%d" % self.n_alloc)
        t = (stack or getattr(self, 'cur', None) or self.es).enter_context(self.nc.sbuf_tensor(name + "_%d" % self.n_alloc, list(shape), dtype))
        return Buf(t, name)

    def psum(self, shape, dtype=F32, name=None, stack=None):
        self.n_alloc += 1
        name = name or ("ps%d" % self.n_alloc)
        t = (stack or self.es).enter_context(self.nc.psum_tensor(name + "_%d" % self.n_alloc, list(shape), dtype))
        return Buf(t, name)

    def dram(self, name, shape, dtype=F32, kind="Internal"):
        t = self.nc.dram_tensor(name, list(shape), dtype, kind=kind)
        return Buf(t.ap(), name)

    def _collect(self, eng, reads, writes):
        deps = []
        for b in reads:
            if b.last_w is not None:
                deps.append(b.last_w)
        for b in writes:
            if b.last_w is not None:
                deps.append(b.last_w)
            deps.extend(b.readers)
        waits = []
        for d in deps:
            if d[0] == 'eng':
                _, f, k = d
                if f == eng and eng == 'pe':
                    continue
                if self.seen[eng][f] < k:
                    self.seen[eng][f] = k
                    waits.append((self.sem[f], k))
            else:
                _, key, val = d
                if self.dseen[eng].get(key, 0) < val:
                    self.dseen[eng][key] = val
                    waits.append((self.dsem[key], val))
        best = {}
        for s, v in waits:
            if id(s) not in best or best[id(s)][1] < v:
                best[id(s)] = (s, v)
        return list(best.values())

    def _mark(self, tok, reads, writes):
        for b in writes:
            b.last_w = tok
            b.readers = []
        for b in reads:
            if b not in writes:
                b.readers.append(tok)
                if len(b.readers) > 64:
                    b.readers = b.readers[-64:]

    def op(self, eng, fn, reads=(), writes=()):
        waits = self._collect(eng, reads, writes)
        self.cnt[eng] += 1
        k = self.cnt[eng]
        sem = self.sem[eng]

        def run(e, waits=waits, fn=fn, sem=sem):
            for s, v in waits:
                e.wait_ge(s, v)
            fn(e).then_inc(sem, 1)
        self.ops[eng].append(run)
        self._mark(('eng', eng, k), reads, writes)

    def I(self, eng, meth, reads=(), writes=(), **kw):
        self.op(eng, lambda e, meth=meth, kw=kw: getattr(e, meth)(**kw), reads, writes)

    def dma(self, q, out, in_, reads=(), writes=(), **kw):
        waits = self._collect(q, reads, writes)
        n = DMA_SLOTS[q]
        i = self.drr[q]
        self.drr[q] = (i + 1) % n
        key = (q, i)
        prev = self.dval[key]
        if prev > 0 and self.dseen[q].get(key, 0) < prev:
            self.dseen[q][key] = prev
            waits.append((self.dsem[key], prev))
        self.dval[key] = prev + 16
        val = prev + 16
        sem = self.dsem[key]

        def run(e, waits=waits, out=out, in_=in_, sem=sem, kw=kw):
            for s, v in waits:
                e.wait_ge(s, v)
            e.dma_start(out=out, in_=in_, **kw).then_inc(sem, 16)
        self.ops[q].append(run)
        self._mark(('dma', key, val), reads, writes)

    def barrier(self):
        for e in ENGS:
            waits = []
            for f in ENGS:
                if f != e and self.seen[e][f] < self.cnt[f]:
                    self.seen[e][f] = self.cnt[f]
                    waits.append((self.sem[f], self.cnt[f]))
            for key, val in self.dval.items():
                if val > 0 and self.dseen[e].get(key, 0) < val:
                    self.dseen[e][key] = val
                    waits.append((self.dsem[key], val))

            def run(en, waits=waits):
                for s, v in waits:
                    en.wait_ge(s, v)
            self.ops[e].append(run)

    def collective(self, kind, groups, in_buf, in_ap, out_buf, out_ap):
        q = 'pool'
        waits = self._collect(q, [in_buf], [out_buf])
        n = DMA_SLOTS[q]
        i = self.drr[q]
        self.drr[q] = (i + 1) % n
        key = (q, i)
        prev = self.dval[key]
        if prev > 0 and self.dseen[q].get(key, 0) < prev:
            self.dseen[q][key] = prev
            waits.append((self.dsem[key], prev))
        self.dval[key] = prev + 16
        val = prev + 16
        sem = self.dsem[key]

        def run(e, waits=waits, sem=sem):
            for s_, v in waits:
                e.wait_ge(s_, v)
            e.collective_compute(kind, ALU.bypass, replica_groups=groups, ins=[in_ap], outs=[out_ap]).then_inc(sem, 16)
        self.ops[q].append(run)
        self._mark(('dma', key, val), [in_buf], [out_buf])

    @contextlib.contextmanager
    def phase(self):
        st = contextlib.ExitStack()
        self.cur = st
        try:
            yield
            self.flush()
        finally:
            self.cur = None
            st.close()

    def flush(self):
        self.barrier()
        self._emit()
        self.ops = {e: [] for e in ENGS}
        for b in ():
            pass

    def finish(self):
        self.barrier()
        self._emit()
        self.es.close()

    def _emit(self):
        nc = self.nc
        with nc.Block() as block:
            @block.tensor
            def _(e):
                for f in self.ops['pe']:
                    f(e)

            @block.scalar
            def _(e):
                for f in self.ops['act']:
                    f(e)

            @block.vector
            def _(e):
                for f in self.ops['dve']:
                    f(e)

            @block.gpsimd
            def _(e):
                for f in self.ops['pool']:
                    f(e)

            @block.sync
            def _(e):
                for f in self.ops['sp']:
                    f(e)

from concourse.bass_utils import run_bass_kernel_spmd

D = 2048
NT = 16
TOK = 2048
EPS = 1e-6
MODN = 4 * 12288 + 4096

C_ID, C_M1, C_M2, C_LINC, C_USUF, C_ONES, C_MA, C_MB, C_P = 0, 128, 256, 384, 512, 640, 768, 896, 1024
NCST = 1025


def make_consts(p):
    c = np.zeros((128, NCST), np.float32)
    s = np.arange(128)[:, None]
    t = np.arange(128)[None, :]
    c[:, C_ID:C_ID + 128] = (s == t)
    m1 = (s <= t).astype(np.float32)
    c[:, C_M1:C_M1 + 128] = m1
    c[:, C_M2:C_M2 + 128] = ((s > t) & ((s // 64) == (t // 64)))
    c[:, C_LINC:C_LINC + 128] = m1 * (-1.0 / 16.0)
    c[:, C_USUF:C_USUF + 128] = (s > t) * (-1.0 / 16.0)
    c[:, C_ONES:C_ONES + 128] = 1.0
    if p == 0:
        c[:, C_MA:C_MA + 128] = m1
        c[:, C_MB:C_MB + 128] = 0.0
    else:
        c[:, C_MA:C_MA + 128] = 1.0
        c[:, C_MB:C_MB + 128] = m1
    c[:, C_P] = float(p)
    return c


class Ctx:
    def __init__(self, P, cst_d):
        self.P = P
        self.bank = [P.psum([128, 512], F32, name="bank%d" % i) for i in range(8)]
        self.cst = P.sbuf([128, NCST], F32, name="cst")
        P.dma('sp', self.cst[:], cst_d[:, :], reads=[cst_d], writes=[self.cst])
        self.small = {}

    def c(self, off, n=128):
        return self.cst[:, off:off + n]


def bcast_row(P, dst, dst_ap, src_buf, src_ap_row, q='sp'):
    P.dma(q, dst_ap, src_ap_row.partition_broadcast(128), reads=[src_buf], writes=[dst])


def rstd_from_ss(P, ss, rstd, n, inv_n):
    P.I('dve', 'tensor_scalar', reads=[ss], writes=[rstd], out=rstd[:, 0:n], in0=ss[:, 0:n],
        scalar1=inv_n, scalar2=EPS, op0=ALU.mult, op1=ALU.add)
    P.I('act', 'activation', reads=[rstd], writes=[rstd], out=rstd[:, 0:n], in_=rstd[:, 0:n], func=AF.Sqrt)
    P.I('dve', 'reciprocal', reads=[rstd], writes=[rstd], out=rstd[:, 0:n], in_=rstd[:, 0:n])


def normmod_T(P, cx, xt, A, B, h, junk, ss, rstd, hT, tcol, banks):
    P.I('act', 'activation', reads=[xt], writes=[junk, ss], out=junk[:], in_=xt[:], func=AF.Square,
        accum_out=ss[:, 0:1])
    rstd_from_ss(P, ss, rstd, 1, 1.0 / D)
    P.I('dve', 'scalar_tensor_tensor', reads=[xt, rstd, A], writes=[h], out=h[:], in0=xt[:],
        scalar=rstd[:, 0:1], in1=A[:], op0=ALU.mult, op1=ALU.mult)
    if B is not None:
        P.I('pool', 'tensor_tensor', reads=[h, B], writes=[h], out=h[:], in0=h[:], in1=B[:], op=ALU.add)
    transpose_T(P, cx, h, hT, tcol, banks)


def transpose_T(P, cx, h, hT, tcol, banks, nch=16):
    for g in range(nch // 4):
        bk = banks[g % len(banks)]
        for k in range(4):
            c = g * 4 + k
            P.I('pe', 'transpose', reads=[h, cx.cst], writes=[bk], out=bk[:, k * 128:(k + 1) * 128],
                in_=h[:, c * 128:(c + 1) * 128], identity=cx.c(C_ID))
        eng = 'act' if g % 2 == 0 else 'dve'
        src = bk[:].rearrange("p (k t) -> p k t", k=4)
        dst = hT[:, g * 4:(g + 1) * 4, tcol:tcol + 128]
        if eng == 'act':
            P.I('act', 'activation', reads=[bk], writes=[hT], out=dst, in_=src, func=AF.Copy)
        else:
            P.I('dve', 'tensor_copy', reads=[bk], writes=[hT], out=dst, in_=src)


def make_AB(P, A, B, gain_d, gain_row, modv_d, off_shift, off_scale, tmp):
    bcast_row(P, A, A[:], gain_d, gain_row)
    if modv_d is not None:
        bcast_row(P, tmp, tmp[:], modv_d, modv_d[0:1, off_scale:off_scale + D])
        bcast_row(P, B, B[:], modv_d, modv_d[0:1, off_shift:off_shift + D])
        P.I('dve', 'scalar_tensor_tensor', reads=[tmp, A], writes=[A], out=A[:], in0=tmp[:], scalar=1.0,
            in1=A[:], op0=ALU.add, op1=ALU.mult)


def phase_mod(P, c_d, w_d, b_d, o_d):
    ca = P.sbuf([128, 16], F32, name="ca")
    P.dma('sp', ca[:], c_d[0:1, :].rearrange("o (c p) -> p (o c)", p=128), reads=[c_d], writes=[ca],
          allow_slow_non_contiguous=True)
    P.I('act', 'activation', reads=[ca], writes=[ca], out=ca[:], in_=ca[:], func=AF.Silu)
    wt = [P.sbuf([128, 16, 512], F32, name="wt%d" % i) for i in range(2)]
    bt = [P.sbuf([1, 512], F32, name="bt%d" % i) for i in range(2)]
    ot = [P.sbuf([1, 512], F32, name="ot%d" % i) for i in range(2)]
    ps = P.cx.bank
    for n in range(MODN // 512):
        w, b_, o, p_ = wt[n % 2], bt[n % 2], ot[n % 2], ps[n % 2]
        P.dma('sp' if n % 2 == 0 else 'act', w[:], w_d[:, n * 512:(n + 1) * 512].rearrange("(c p) n -> p c n", p=128),
              reads=[w_d], writes=[w])
        P.dma('pool', b_[:], b_d[0:1, n * 512:(n + 1) * 512], reads=[b_d], writes=[b_])
        for c in range(16):
            P.I('pe', 'matmul', reads=[ca, w], writes=[p_], out=p_[0:1, :], lhsT=ca[:, c:c + 1], rhs=w[:, c, :],
                start=(c == 0), stop=(c == 15))
        P.I('dve', 'tensor_tensor', reads=[p_, b_], writes=[o], out=o[:], in0=p_[0:1, :], in1=b_[:], op=ALU.add)
        P.dma('sp', o_d[0:1, n * 512:(n + 1) * 512], o[:], reads=[o], writes=[o_d])


def gla_a(P, cx, l, x_d, modv_d, nmix_d, win_d, wg2_d, bg2_d, oloc_d, gs_d, qc_d=None, send_d=None, nt=NT):
    bk = cx.bank
    A = P.sbuf([128, D], F32, name="A"); B = P.sbuf([128, D], F32, name="B");
    off = l * 12288
    PW = 256
    TP = 2
    hT = P.sbuf([128, 16, PW], BF16, name="hT")
    xt = [P.sbuf([128, D], F32, name="xt0")] * 2
    h = [P.sbuf([128, D], F32, name="h0")] * 2
    make_AB(P, A, B, nmix_d, nmix_d[l:l + 1, :], modv_d, off, off + D, h[0])
    junk = P.sbuf([128, D], BF16, name="junk")
    ss = P.sbuf([128, 4], F32, name="ss"); rstd = P.sbuf([128, 4], F32, name="rstd")
    wblk = [P.sbuf([128, 16, 512], BF16, name="wblk%d" % i) for i in range(2)]
    wgl = P.sbuf([128, 16, 16], BF16, name="wgl")
    P.dma('pool', wgl[:], win_d[l, :, 6144:6160].rearrange("(c p) n -> p c n", p=128), reads=[win_d], writes=[wgl])
    wg2 = P.sbuf([17, 1024], F32, name="wg2")
    P.dma('sp', wg2[0:16, :], wg2_d[l, :, :], reads=[wg2_d], writes=[wg2])
    P.dma('sp', wg2[16:17, :], bg2_d[l:l + 1, :], reads=[bg2_d], writes=[wg2])
    glT = P.sbuf([17, PW], F32, name="glT")
    P.I('pool', 'memset', writes=[glT], ap=glT[:], constant=1.0)
    qT = P.sbuf([128, 8, PW], BF16, name="qT"); kT = P.sbuf([128, 8, PW], BF16, name="kT")
    ktm = [P.sbuf([128, 1024], F32, name="ktm%d" % i) for i in range(TP)]
    vbf = [P.sbuf([128, 2048], BF16, name="vbf%d" % i) for i in range(TP)]
    gst = [P.sbuf([128, 512], F32, name="gst%d" % i) for i in range(2)]
    ez = P.sbuf([128, 1024], F32, name="ez"); la = P.sbuf([128, 1024], F32, name="la")
    EA = P.sbuf([128, 8, 128], F32, name="EA"); EB = P.sbuf([128, 8, 128], F32, name="EB")
    ER = P.sbuf([128, 1024], F32, name="ER")
    QA = P.sbuf([128, 8, 128], BF16, name="QA"); QB = P.sbuf([128, 8, 128], BF16, name="QB")
    KA = P.sbuf([128, 8, 128], BF16, name="KA"); KB = P.sbuf([128, 8, 128], BF16, name="KB")
    KE = P.sbuf([128, 1024], BF16, name="KE")
    QC = P.sbuf([128, 8, 128], F32, name="QC")
    cumP = P.sbuf([128, 8], F32, name="cumP"); expP = P.sbuf([128, 8], F32, name="expP")
    P.I('pool', 'memset', writes=[cumP], ap=cumP[:], constant=0.0)
    t1 = P.sbuf([128, 4, 128], F32, name="t1"); t2 = P.sbuf([128, 4, 128], F32, name="t2")
    PT = P.sbuf([128, 4, 128], BF16, name="PT")
    S32 = [P.sbuf([128, 512], F32, name="S32_%d" % i) for i in range(8)]
    Sbf = [P.sbuf([128, 512], BF16, name="Sbf_%d" % i) for i in range(8)]
    for i in range(8):
        P.I('pool', 'memset', writes=[S32[i]], ap=S32[i][:], constant=0.0)
        P.I('pool', 'memset', writes=[Sbf[i]], ap=Sbf[i][:], constant=0.0)
    ot = [P.sbuf([128, 2048], F32, name="ot0")] * 2
    m1b = cx.c(C_M1).unsqueeze(1).to_broadcast([128, 4, 128])
    m2b = cx.c(C_M2).unsqueeze(1).to_broadcast([128, 4, 128])
    nb = 0
    for ps_ in range(nt // TP):
        for t in range(TP):
            g = ps_ * TP + t
            P.dma('sp', xt[t % 2][:], x_d[g * 128:(g + 1) * 128, :], reads=[x_d], writes=[xt[t % 2]])
            normmod_T(P, cx, xt[t % 2], A, B, h[t % 2], junk, ss, rstd, hT, t * 128, [bk[0], bk[1]])
        for cb in range(12):
            w = wblk[nb % 2]; nb += 1
            P.dma('pool', w[:], win_d[l, :, cb * 512:(cb + 1) * 512].rearrange("(c p) n -> p c n", p=128),
                  reads=[win_d], writes=[w])
            if cb < 4:
                dstT = qT if cb < 2 else kT
                for m in range(4):
                    b_ = bk[(cb * 4 + m) % 2]
                    for c in range(16):
                        P.I('pe', 'matmul', reads=[w, hT], writes=[b_], out=b_[:, 0:PW], lhsT=w[:, c, m * 128:(m + 1) * 128],
                            rhs=hT[:, c, :], start=(c == 0), stop=(c == 15))
                    P.I('act', 'activation', reads=[b_], writes=[dstT], out=dstT[:, (cb % 2) * 4 + m, :], in_=b_[:, 0:PW],
                        func=AF.Copy)
            if cb >= 2:
                for t in range(TP):
                    g = ps_ * TP + t
                    b_ = bk[2 + (t % 2)]
                    for c in range(16):
                        P.I('pe', 'matmul', reads=[w, hT], writes=[b_], out=b_[:], lhsT=hT[:, c, t * 128:(t + 1) * 128],
                            rhs=w[:, c, :], start=(c == 0), stop=(c == 15))
                    if cb < 4:
                        P.I('dve', 'tensor_copy', reads=[b_], writes=[ktm[t]], out=ktm[t][:, (cb - 2) * 512:(cb - 1) * 512],
                            in_=b_[:])
                    elif cb < 8:
                        P.I('dve', 'tensor_copy', reads=[b_], writes=[vbf[t]], out=vbf[t][:, (cb - 4) * 512:(cb - 3) * 512],
                            in_=b_[:])
                    else:
                        go = gst[(cb * 4 + t) % 2]
                        P.I('act', 'activation', reads=[b_], writes=[go], out=go[:], in_=b_[:], func=AF.Silu)
                        P.dma('sp', gs_d[g * 128:(g + 1) * 128, (cb - 8) * 512:(cb - 7) * 512], go[:], reads=[go],
                              writes=[gs_d])
        for c in range(16):
            P.I('pe', 'matmul', reads=[wgl, hT], writes=[bk[0]], out=bk[0][0:16, 0:PW], lhsT=wgl[:, c, :], rhs=hT[:, c, :],
                start=(c == 0), stop=(c == 15))
        P.I('act', 'activation', reads=[bk[0]], writes=[glT], out=glT[0:16, :], in_=bk[0][0:16, 0:PW], func=AF.Copy)
        for t in range(TP):
            g = ps_ * TP + t
            for n in range(2):
                P.I('pe', 'matmul', reads=[glT, wg2], writes=[bk[4 + n]], out=bk[4 + n][:], lhsT=glT[:, t * 128:(t + 1) * 128],
                    rhs=wg2[:, n * 512:(n + 1) * 512], start=True, stop=True)
                P.I('act', 'activation', reads=[bk[4 + n]], writes=[ez], out=ez[:, n * 512:(n + 1) * 512], in_=bk[4 + n][:],
                    func=AF.Exp, scale=-1.0)
            P.I('act', 'activation', reads=[ez], writes=[la], out=la[:], in_=ez[:], func=AF.Ln, bias=1.0)
            for m in range(8):
                b_ = bk[4 + m // 4]
                P.I('pe', 'matmul', reads=[la, cx.cst], writes=[b_], out=b_[:, (m % 4) * 128:(m % 4 + 1) * 128],
                    lhsT=la[:, m * 128:(m + 1) * 128], rhs=cx.c(C_LINC), start=True, stop=True)
            for n in range(2):
                P.I('pe', 'matmul', reads=[la, cx.cst], writes=[bk[6 + n]], out=bk[6 + n][:], lhsT=cx.c(C_USUF),
                    rhs=la[:, n * 512:(n + 1) * 512], start=True, stop=True)
            for n in range(2):
                src = bk[4 + n][:].rearrange("p (k t) -> p k t", k=4)
                P.I('act', 'activation', reads=[bk[4 + n]], writes=[EA], out=EA[:, n * 4:(n + 1) * 4, :], in_=src, func=AF.Exp)
                P.I('act', 'activation', reads=[bk[4 + n]], writes=[EB], out=EB[:, n * 4:(n + 1) * 4, :], in_=src, func=AF.Exp,
                    scale=-1.0)
                P.I('act', 'activation', reads=[bk[6 + n]], writes=[ER], out=ER[:, n * 512:(n + 1) * 512], in_=bk[6 + n][:],
                    func=AF.Exp)
            if qc_d is not None:
                P.I('act', 'activation', reads=[cumP], writes=[expP], out=expP[:], in_=cumP[:], func=AF.Exp)
            for n in range(2 if qc_d is not None else 0):
                P.I('dve', 'tensor_tensor', reads=[cumP, bk[4 + n]], writes=[cumP], out=cumP[:, n * 4:(n + 1) * 4],
                    in0=cumP[:, n * 4:(n + 1) * 4],
                    in1=bk[4 + n][:].rearrange("p (k t) -> p k t", k=4)[:, :, 127], op=ALU.add)
            qs = qT[:, :, t * 128:(t + 1) * 128]
            ks = kT[:, :, t * 128:(t + 1) * 128]
            P.I('dve', 'scalar_tensor_tensor', reads=[qT, EA], writes=[QA], out=QA[:], in0=qs, scalar=0.0625, in1=EA[:],
                op0=ALU.mult, op1=ALU.mult)
            P.I('dve', 'scalar_tensor_tensor', reads=[qT, EB], writes=[QB], out=QB[:], in0=qs, scalar=0.0625, in1=EB[:],
                op0=ALU.mult, op1=ALU.mult)
            P.I('pool', 'tensor_tensor', reads=[kT, EA], writes=[KA], out=KA[:], in0=ks, in1=EA[:], op=ALU.mult)
            P.I('pool', 'tensor_tensor', reads=[kT, EB], writes=[KB], out=KB[:], in0=ks, in1=EB[:], op=ALU.mult)
            P.I('pool', 'tensor_tensor', reads=[ktm[t], ER], writes=[KE], out=KE[:], in0=ktm[t][:], in1=ER[:], op=ALU.mult)
            if qc_d is not None:
                P.I('dve', 'scalar_tensor_tensor', reads=[qT, EA, expP], writes=[QC], out=QC[:], in0=qs, scalar=0.0625, in1=EA[:],
                    op0=ALU.mult, op1=ALU.mult)
                P.I('dve', 'tensor_tensor', reads=[QC, expP], writes=[QC], out=QC[:], in0=QC[:],
                    in1=expP[:].unsqueeze(2).to_broadcast([128, 8, 128]), op=ALU.mult)
                P.dma('sp', qc_d[:, g * 128:(g + 1) * 128].rearrange("(m p) t -> p m t", p=128), QC[:], reads=[QC], writes=[qc_d])
            for hd in range(4):
                for j in range(2):
                    m = 2 * hd + j
                    P.I('pe', 'matmul', reads=[KB, QA], writes=[bk[0]], out=bk[0][:, hd * 128:(hd + 1) * 128], lhsT=KB[:, m, :],
                        rhs=QA[:, m, :], start=(j == 0), stop=(j == 1))
                for j in range(2):
                    m = 2 * hd + j
                    P.I('pe', 'matmul', reads=[KA, QB], writes=[bk[1]], out=bk[1][:, hd * 128:(hd + 1) * 128], lhsT=KA[:, m, :],
                        rhs=QB[:, m, :], start=(j == 0), stop=(j == 1))
            P.I('dve', 'tensor_tensor', reads=[bk[0], cx.cst], writes=[t1], out=t1[:],
                in0=bk[0][:].rearrange("p (k t) -> p k t", k=4), in1=m1b, op=ALU.mult)
            P.I('dve', 'tensor_tensor', reads=[bk[1], cx.cst], writes=[t2], out=t2[:],
                in0=bk[1][:].rearrange("p (k t) -> p k t", k=4), in1=m2b, op=ALU.mult)
            P.I('pool', 'tensor_tensor', reads=[t1, t2], writes=[PT], out=PT[:], in0=t1[:], in1=t2[:], op=ALU.add)
            o_ = ot[t % 2]
            for hd in range(4):
                b_ = bk[4 + hd]
                for j in range(2):
                    m = 2 * hd + j
                    P.I('pe', 'matmul', reads=[QA, Sbf[m]], writes=[b_], out=b_[:], lhsT=QA[:, m, :], rhs=Sbf[m][:],
                        start=(j == 0), stop=False)
                P.I('pe', 'matmul', reads=[PT, vbf[t]], writes=[b_], out=b_[:], lhsT=PT[:, hd, :],
                    rhs=vbf[t][:, hd * 512:(hd + 1) * 512], start=False, stop=True)
                P.I('act', 'activation', reads=[b_], writes=[o_], out=o_[:, hd * 512:(hd + 1) * 512], in_=b_[:], func=AF.Copy)
            P.dma('sp', oloc_d[g * 128:(g + 1) * 128, :], o_[:], reads=[o_], writes=[oloc_d])
            for hd in range(4):
                for j in range(2):
                    m = 2 * hd + j
                    b_ = bk[2 + (m % 2)]
                    P.I('pe', 'matmul', reads=[KE, vbf[t]], writes=[b_], out=b_[:], lhsT=KE[:, m * 128:(m + 1) * 128],
                        rhs=vbf[t][:, hd * 512:(hd + 1) * 512], start=True, stop=True)
                    P.I('dve', 'scalar_tensor_tensor', reads=[S32[m], EA, b_], writes=[S32[m]], out=S32[m][:], in0=S32[m][:],
                        scalar=EA[:, m, 127:128], in1=b_[:], op0=ALU.mult, op1=ALU.add)
                    P.I('act', 'activation', reads=[S32[m]], writes=[Sbf[m]], out=Sbf[m][:], in_=S32[m][:], func=AF.Copy)
    if send_d is not None:
        for m in range(8):
            P.dma('sp', send_d[m * 128:(m + 1) * 128, :], S32[m][:], reads=[S32[m]], writes=[send_d])


def gla_b(P, cx, l, x_d, xo_d, modv_d, gnorm_d, wout_d, oloc_d, gs_d, qc_d=None, sprev_d=None, nt=NT):
    bk = cx.bank
    off = l * 12288
    gate = P.sbuf([128, D], F32, name="gate")
    bcast_row(P, gate, gate[:], modv_d, modv_d[0:1, off + 2 * D:off + 3 * D])
    gn = P.sbuf([128, 512], F32, name="gn")
    bcast_row(P, gn, gn[:], gnorm_d, gnorm_d[l:l + 1, :])
    wout = P.sbuf([128, 16, D], BF16, name="wout")
    for n in range(4):
        P.dma('pool', wout[:, :, n * 512:(n + 1) * 512], wout_d[l, :, n * 512:(n + 1) * 512].rearrange("(c p) n -> p c n", p=128),
              reads=[wout_d], writes=[wout])
    corr = qc_d is not None
    if corr:
        sp = P.sbuf([128, 8, 512], BF16, name="sprev")
        P.dma('pool', sp[:], sprev_d[:, :].rearrange("(m p) n -> p m n", p=128), reads=[sprev_d], writes=[sp])
        qc = P.sbuf([128, 8, 128], BF16, name="qcb")
    o = P.sbuf([128, D], F32, name="o"); gs = P.sbuf([128, D], F32, name="gs"); xt = P.sbuf([128, D], F32, name="xtb")
    on = P.sbuf([128, D], F32, name="on"); junk = P.sbuf([128, 512], BF16, name="junkb")
    onT = P.sbuf([128, 16, 128], BF16, name="onT")
    ss = P.sbuf([128, 4], F32, name="ssb"); rstd = P.sbuf([128, 4], F32, name="rstdb")
    for g in range(nt):
        rows = slice(g * 128, (g + 1) * 128)
        P.dma('sp', o[:], oloc_d[rows, :], reads=[oloc_d], writes=[o])
        P.dma('act', gs[:], gs_d[rows, :], reads=[gs_d], writes=[gs])
        P.dma('sp', xt[:], x_d[rows, :], reads=[x_d], writes=[xt])
        if corr:
            P.dma('pool', qc[:], qc_d[:, rows].rearrange("(m p) t -> p m t", p=128), reads=[qc_d], writes=[qc])
        for hd in range(4):
            b_ = bk[hd]
            osl = o[:, hd * 512:(hd + 1) * 512]
            if corr:
                for j in range(2):
                    m = 2 * hd + j
                    P.I('pe', 'matmul', reads=[qc, sp], writes=[b_], out=b_[:], lhsT=qc[:, m, :], rhs=sp[:, m, :],
                        start=(j == 0), stop=(j == 1))
                P.I('dve', 'tensor_tensor', reads=[o, b_], writes=[o], out=osl, in0=osl, in1=b_[:], op=ALU.add)
            P.I('act', 'activation', reads=[o], writes=[junk, ss], out=junk[:], in_=osl, func=AF.Square,
                accum_out=ss[:, hd:hd + 1])
        rstd_from_ss(P, ss, rstd, 4, 1.0 / 512)
        for hd in range(4):
            P.I('dve', 'scalar_tensor_tensor', reads=[o, rstd, gn], writes=[on], out=on[:, hd * 512:(hd + 1) * 512],
                in0=o[:, hd * 512:(hd + 1) * 512], scalar=rstd[:, hd:hd + 1], in1=gn[:], op0=ALU.mult, op1=ALU.mult)
        P.I('pool', 'tensor_tensor', reads=[on, gs], writes=[on], out=on[:], in0=on[:], in1=gs[:], op=ALU.mult)
        transpose_T(P, cx, on, onT, 0, [bk[4], bk[5]])
        for n in range(4):
            b_ = bk[n]
            for c in range(16):
                P.I('pe', 'matmul', reads=[onT, wout], writes=[b_], out=b_[:], lhsT=onT[:, c, :], rhs=wout[:, c, n * 512:(n + 1) * 512],
                    start=(c == 0), stop=(c == 15))
            ysl = on[:, n * 512:(n + 1) * 512]
            P.I('dve', 'tensor_tensor', reads=[b_, gate], writes=[on], out=ysl, in0=b_[:], in1=gate[:, n * 512:(n + 1) * 512],
                op=ALU.mult)
        P.I('pool', 'tensor_tensor', reads=[on, xt], writes=[xt], out=xt[:], in0=on[:], in1=xt[:], op=ALU.add)
        P.dma('sp', xo_d[rows, :], xt[:], reads=[xt], writes=[xo_d])


def peer(P, cx, l, x_d, xo_d, modv_d, nffn_d, wq_d, subk_d, uT_d, v_d, nt=NT):
    bk = cx.bank
    off = l * 12288 + 3 * D
    RG = P.sbuf([128, 25600], F32, name="RG")

    def carve(a, b, name, dt=F32):
        ap = RG[:, a:b]
        if dt == BF16:
            ap = ap.bitcast(BF16)
        return Buf(ap, name)
    hT = P.sbuf([128, 16, 512], BF16, name="hTp")
    acc = [P.sbuf([128, D], F32, name="acc%d" % i) for i in range(4)]
    ssb = [P.sbuf([128, 16, 128], F32, name="ssb%d" % i) for i in range(4)]
    tau = [P.sbuf([128, 8], F32, name="tau%d" % i) for i in range(4)]
    negC = [P.sbuf([128, 8], F32, name="negC%d" % i) for i in range(4)]
    xi = [P.sbuf([128, 8], F32, name="xi%d" % i) for i in range(4)]
    halfC = P.sbuf([128, 8], F32, name="halfC")
    XD = P.sbuf([128, 1536], F32, name="XD")
    subkT = P.sbuf([128, 16, 128], F32, name="subkT")
    idb = P.sbuf([128, 128], BF16, name="idb")
    P.I('dve', 'tensor_copy', reads=[cx.cst], writes=[idb], out=idb[:], in_=cx.c(C_ID))
    m16 = P.sbuf([128, 16, 16], F32, name="m16"); top = P.sbuf([128, 8, 16], F32, name="top")
    negm = P.sbuf([128, 8], F32, name="negm"); Z = P.sbuf([128, 8], F32, name="Z"); j16 = P.sbuf([128, 16], F32, name="j16")
    ss = P.sbuf([128, 4], F32, name="ssp"); rstd = P.sbuf([128, 4], F32, name="rstdp")
    stg = acc[0]
    P.dma('sp', stg[:].rearrange("p (a n) -> p a n", a=16), subk_d[l].rearrange("h q n d -> n (h q) d"), reads=[subk_d], writes=[stg])
    for g4 in range(4):
        b_ = bk[g4 % 2]
        for k in range(4):
            hp = g4 * 4 + k
            P.I('pe', 'transpose', reads=[stg, cx.cst], writes=[b_], out=b_[:, k * 128:(k + 1) * 128],
                in_=stg[:, hp * 128:(hp + 1) * 128], identity=cx.c(C_ID))
        P.I('act', 'activation', reads=[b_], writes=[subkT], out=subkT[:, g4 * 4:(g4 + 1) * 4, :],
            in_=b_[:].rearrange("p (k t) -> p k t", k=4), func=AF.Copy)
    P.barrier()
    for ps_ in range(nt // 4):
        xt = carve(0, 2048, "xt"); h = carve(2048, 4096, "h"); A = carve(4096, 6144, "A"); B = carve(6144, 8192, "B")
        qT = carve(8192, 16384, "qT"); cand = carve(16384, 18432, "cand"); candw = carve(18432, 20480, "candw")
        wqb = [carve(20480, 22528, "wq0", BF16), carve(22528, 24576, "wq1", BF16)]
        junk = carve(24576, 25600, "junk", BF16)
        make_AB(P, A, B, nffn_d, nffn_d[l:l + 1, :], modv_d, off, off + D, h)
        for t in range(4):
            g = ps_ * 4 + t
            P.dma('sp', xt[:], x_d[g * 128:(g + 1) * 128, :], reads=[x_d], writes=[xt])
            normmod_T(P, cx, xt, A, B, h, junk, ss, rstd, hT, t * 128, [bk[0], bk[1]])
        qTv = qT[:].rearrange("p (a n) -> p a n", a=16)
        for cb in range(8):
            w = wqb[cb % 2]
            wv = w[:].rearrange("p (c n) -> p c n", c=16)
            P.dma('pool', wv, wq_d[l, :, cb * 256:(cb + 1) * 256].rearrange("(c p) n -> p c n", p=128), reads=[wq_d], writes=[w])
            for m in range(2):
                b_ = bk[2 + (cb * 2 + m) % 2]
                for c in range(16):
                    P.I('pe', 'matmul', reads=[w, hT], writes=[b_], out=b_[:], lhsT=wv[:, c, m * 128:(m + 1) * 128], rhs=hT[:, c, :],
                        start=(c == 0), stop=(c == 15))
                P.I('act', 'activation', reads=[b_], writes=[qT], out=qTv[:, cb * 2 + m, :], in_=b_[:], func=AF.Copy)
        hv = h[:].rearrange("p (a n) -> p a n", a=16)
        cv4 = cand[:].rearrange("p (h a b) -> p h a b", h=8, a=16)
        cv = cand[:].rearrange("p (h n) -> p h n", h=8)
        cwv = candw[:].rearrange("p (h n) -> p h n", h=8)
        for t in range(4):
            s_ = ssb[t]
            for hp in range(16):
                b_ = bk[4 + hp // 4]
                P.I('pe', 'matmul', reads=[qT, subkT], writes=[b_], out=b_[:, (hp % 4) * 128:(hp % 4 + 1) * 128],
                    lhsT=qTv[:, hp, t * 128:(t + 1) * 128], rhs=subkT[:, hp, :], start=True, stop=True)
            for g4 in range(4):
                P.I('act', 'activation', reads=[bk[4 + g4]], writes=[s_], out=s_[:, g4 * 4:(g4 + 1) * 4, :],
                    in_=bk[4 + g4][:].rearrange("p (k t) -> p k t", k=4), func=AF.Copy)
            for hp in range(16):
                P.I('dve', 'max', reads=[s_], writes=[m16], out=m16[:, hp, 0:8], in_=s_[:, hp, :])
                P.I('dve', 'match_replace', reads=[s_, m16], writes=[h], out=hv[:, hp, :], in_to_replace=m16[:, hp, 0:8],
                    in_values=s_[:, hp, :], imm_value=-1e30)
                P.I('dve', 'max', reads=[h], writes=[m16], out=m16[:, hp, 8:16], in_=hv[:, hp, :])
            m16v = m16[:].rearrange("p (h q) k -> p h q k", q=2)
            P.I('dve', 'tensor_tensor', reads=[m16], writes=[cand], out=cv4,
                in0=m16v[:, :, 0, :].unsqueeze(3).to_broadcast([128, 8, 16, 16]),
                in1=m16v[:, :, 1, :].unsqueeze(2).to_broadcast([128, 8, 16, 16]), op=ALU.add)
            for hh in range(8):
                P.I('dve', 'max', reads=[cand], writes=[top], out=top[:, hh, 0:8], in_=cv[:, hh, :])
                P.I('dve', 'match_replace', reads=[cand, top], writes=[candw], out=cwv[:, hh, :], in_to_replace=top[:, hh, 0:8],
                    in_values=cv[:, hh, :], imm_value=-1e30)
                P.I('dve', 'max', reads=[candw], writes=[top], out=top[:, hh, 8:16], in_=cwv[:, hh, :])
            P.I('dve', 'tensor_scalar', reads=[top], writes=[negm], out=negm[:], in0=top[:, :, 0], scalar1=-1.0, scalar2=0.0,
                op0=ALU.mult, op1=ALU.add)
            for hh in range(8):
                P.I('act', 'activation', reads=[top, negm], writes=[j16, Z], out=j16[:], in_=top[:, hh, :], func=AF.Exp,
                    bias=negm[:, hh:hh + 1], scale=1.0, accum_out=Z[:, hh:hh + 1])
            P.I('act', 'activation', reads=[Z], writes=[Z], out=Z[:], in_=Z[:], func=AF.Ln)
            P.I('dve', 'tensor_tensor', reads=[negm, Z], writes=[negC[t]], out=negC[t][:], in0=negm[:], in1=Z[:], op=ALU.subtract)
            P.I('dve', 'tensor_scalar', reads=[top], writes=[tau[t]], out=tau[t][:], in0=top[:, :, 15], scalar1=1.0, scalar2=-1e-5,
                op0=ALU.mult, op1=ALU.add)
            P.I('dve', 'tensor_tensor', reads=[tau[t], negC[t]], writes=[xi[t]], out=xi[t][:], in0=tau[t][:], in1=negC[t][:], op=ALU.add)
            P.I('act', 'activation', reads=[xi[t]], writes=[xi[t]], out=xi[t][:], in_=xi[t][:], func=AF.Exp)
            P.I('dve', 'tensor_scalar', reads=[negC[t]], writes=[halfC], out=halfC[:], in0=negC[t][:], scalar1=0.5, scalar2=0.0,
                op0=ALU.mult, op1=ALU.add)
            for hh in range(8):
                P.I('act', 'activation', reads=[s_, halfC], writes=[s_], out=s_[:, 2 * hh:2 * hh + 2, :], in_=s_[:, 2 * hh:2 * hh + 2, :],
                    func=AF.Exp, bias=halfC[:, hh:hh + 1], scale=1.0)
        P.barrier()
        uTb = [carve(0, 4096, "uT0", BF16), carve(4096, 8192, "uT1", BF16)]
        vb = [carve(8192, 12288, "v0", BF16), carve(12288, 16384, "v1", BF16)]
        gAb = [[carve(16384 + k * 1024 + i * 256, 16384 + k * 1024 + (i + 1) * 256, "gA%d_%d" % (k, i), BF16) for i in range(4)]
               for k in range(2)]
        Xb = [carve(18432 + k * 512, 18432 + (k + 1) * 512, "X%d" % k) for k in range(4)]
        Tb = [[carve(20992 + k * 2048 + hh * 256, 20992 + k * 2048 + (hh + 1) * 256, "T%d_%d" % (k, hh), BF16) for hh in range(8)]
              for k in range(2)]
        GTb = [carve(20480 + k * 64, 20480 + (k + 1) * 64, "GT%d" % k, BF16) for k in range(8)]
        wbb = [[Buf(bk[6 + k][:, i * 128:(i + 1) * 128], "wb%d_%d" % (k, i)) for i in range(4)] for k in range(2)]
        accb = [[Buf(acc[t][:, n * 512:(n + 1) * 512], "acc%d_%d" % (t, n)) for n in range(4)] for t in range(4)]
        cnt = {'x': 0, 'gt': 0}
        units = [(g, t) for g in range(32) for t in range(4)]
        NU = len(units)
        HA = 5

        def load_group(g):
            uT = uTb[g % 2]; v = vb[g % 2]
            uTv = uT[:].rearrange("p (c e) -> p c e", c=16)
            vv = v[:].rearrange("p (i n) -> p i n", i=4)
            P.dma('pool', uTv, uT_d[l, :, g * 512:(g + 1) * 512].rearrange("(c p) e -> p c e", p=128), reads=[uT_d], writes=[uT])
            P.dma('pool', vv, v_d[l, g * 512:(g + 1) * 512, :].rearrange("(i j) n -> j i n", j=128), reads=[v_d], writes=[v])

        def group_A(g):
            uT = uTb[g % 2]
            uTv = uT[:].rearrange("p (c e) -> p c e", c=16)
            for i in range(4):
                b_ = bk[4 + i % 2]
                gA = gAb[g % 2][i]
                for c in range(16):
                    P.I('pe', 'matmul', reads=[uT, hT], writes=[b_], out=b_[:], lhsT=uTv[:, c, i * 128:(i + 1) * 128], rhs=hT[:, c, :],
                        start=(c == 0), stop=(c == 15))
                P.I('act', 'activation', reads=[b_], writes=[gA], out=gA[:], in_=b_[:], func=AF.Gelu_apprx_tanh)

        def stage_A(u):
            g, t = units[u]
            s_ = ssb[t]
            sv = s_[:].rearrange("p (h q) n -> p h q n", q=2)
            for hh in range(HA):
                X = Xb[cnt['x'] % 4]; cnt['x'] += 1
                T = Tb[u % 2][hh]
                Xv = X[:].rearrange("p (i j) -> p i j", i=4)
                for i in range(4):
                    P.I('act', 'activation', reads=[s_], writes=[X], out=Xv[:, i, :], in_=s_[:, 2 * hh + 1, :], func=AF.Copy,
                        scale=s_[:, 2 * hh, g * 4 + i:g * 4 + i + 1])
                P.I('dve', 'scalar_tensor_tensor', reads=[X, xi[t]], writes=[T], out=T[:], in0=X[:],
                    scalar=xi[t][:, hh:hh + 1], in1=X[:], op0=ALU.is_ge, op1=ALU.mult)
            nd = 8 - HA
            P.I('dve', 'tensor_tensor', reads=[s_], writes=[XD], out=XD[:].rearrange("p (h i j) -> p h i j", h=nd, i=4),
                in0=sv[:, HA:8, 0, g * 4:(g + 1) * 4].unsqueeze(3).to_broadcast([128, nd, 4, 128]),
                in1=sv[:, HA:8, 1, :].unsqueeze(2).to_broadcast([128, nd, 4, 128]), op=ALU.mult)
            for k in range(nd):
                hh = HA + k
                T = Tb[u % 2][hh]
                P.I('dve', 'scalar_tensor_tensor', reads=[XD, xi[t]], writes=[T], out=T[:], in0=XD[:, k * 512:(k + 1) * 512],
                    scalar=xi[t][:, hh:hh + 1], in1=XD[:, k * 512:(k + 1) * 512], op0=ALU.is_ge, op1=ALU.mult)

        def stage_B1(u):
            for i in range(4):
                wb = wbb[u % 2][i]
                for hh in range(8):
                    T = Tb[u % 2][hh]
                    P.I('pe', 'matmul', reads=[T, idb], writes=[wb], out=wb[:], lhsT=T[:, i * 128:(i + 1) * 128],
                        rhs=idb[:], start=(hh == 0), stop=(hh == 7))

        def stage_B2(u):
            g, t = units[u]
            v = vb[g % 2]
            vv = v[:].rearrange("p (i n) -> p i n", i=4)
            gts = []
            for i in range(4):
                GT = GTb[cnt['gt'] % 8]; cnt['gt'] += 1
                gts.append(GT)
                P.I('dve', 'tensor_tensor', reads=[wbb[u % 2][i], gAb[g % 2][i]], writes=[GT], out=GT[:], in0=wbb[u % 2][i][:],
                    in1=gAb[g % 2][i][:, t * 128:(t + 1) * 128], op=ALU.mult)
            for i in range(4):
                for n in range(4):
                    P.I('pe', 'matmul', reads=[gts[i], v], writes=[bk[n]], out=bk[n][:], lhsT=gts[i][:], rhs=vv[:, i, n * 512:(n + 1) * 512],
                        start=(i == 0), stop=(i == 3))

        def stage_B3(u):
            g, t = units[u]
            for n in range(4):
                a_ = accb[t][n]
                if g == 0:
                    P.I('dve', 'tensor_copy', reads=[bk[n]], writes=[a_], out=a_[:], in_=bk[n][:])
                else:
                    P.I('dve', 'tensor_tensor', reads=[bk[n], a_], writes=[a_], out=a_[:], in0=a_[:], in1=bk[n][:], op=ALU.add)

        load_group(0)
        group_A(0)
        stage_A(0)
        stage_A(1)
        stage_B1(0)
        for k in range(NU):
            g, t = units[k]
            if t == 0 and g + 1 < 32:
                load_group(g + 1)
            if k + 2 < NU:
                stage_A(k + 2)
            if t == 1 and g + 1 < 32:
                group_A(g + 1)
            if k + 1 < NU:
                stage_B1(k + 1)
            if k >= 1:
                stage_B3(k - 1)
            stage_B2(k)
        stage_B3(NU - 1)
        P.barrier()
        gate = carve(0, 2048, "gate"); xt5 = carve(2048, 4096, "xt5")
        bcast_row(P, gate, gate[:], modv_d, modv_d[0:1, off + 2 * D:off + 3 * D])
        for t in range(4):
            g = ps_ * 4 + t
            P.dma('sp', xt5[:], x_d[g * 128:(g + 1) * 128, :], reads=[x_d], writes=[xt5])
            P.I('dve', 'tensor_tensor', reads=[acc[t], gate], writes=[acc[t]], out=acc[t][:], in0=acc[t][:], in1=gate[:], op=ALU.mult)
            P.I('pool', 'tensor_tensor', reads=[acc[t], xt5], writes=[xt5], out=xt5[:], in0=acc[t][:], in1=xt5[:], op=ALU.add)
            P.dma('sp', xo_d[g * 128:(g + 1) * 128, :], xt5[:], reads=[xt5], writes=[xo_d])
        P.barrier()


def phase_kv(P, cx, x_d, modv_d, kvn, wkv, bf_d, KT_d, V_d, lf_d, nt):
    bk = cx.bank
    A = P.sbuf([128, D], F32, name="A"); B = P.sbuf([128, D], F32, name="B")
    xt = P.sbuf([128, D], F32, name="xt"); h = P.sbuf([128, D], F32, name="h"); junk = P.sbuf([128, D], BF16, name="junk")
    make_AB(P, A, B, kvn, kvn[0:1, :], modv_d, 49152, 51200, h)
    hT = P.sbuf([128, 16, 512], BF16, name="hT")
    ss = P.sbuf([128, 4], F32, name="ss"); rstd = P.sbuf([128, 4], F32, name="rstd")
    wblk = [P.sbuf([128, 16, 512], BF16, name="wb%d" % i) for i in range(2)]
    wfl = P.sbuf([128, 16, 16], BF16, name="wfl")
    P.dma('pool', wfl[:], wkv[:, 4096:4112].rearrange("(c p) n -> p c n", p=128), reads=[wkv], writes=[wfl])
    bfb = P.sbuf([128, 16], F32, name="bfb")
    bcast_row(P, bfb, bfb[:], bf_d, bf_d[0:1, :])
    ev = [P.sbuf([128, 512], F32, name="ev%d" % i) for i in range(2)]
    z16 = P.sbuf([128, 16], F32, name="z16")
    ne = 0
    for ps_ in range(nt // 4):
        cols = slice(ps_ * 512, (ps_ + 1) * 512)
        for t in range(4):
            g = ps_ * 4 + t
            P.dma('sp', xt[:], x_d[g * 128:(g + 1) * 128, :], reads=[x_d], writes=[xt])
            normmod_T(P, cx, xt, A, B, h, junk, ss, rstd, hT, t * 128, [bk[0], bk[1]])
        for cb in range(8):
            w = wblk[cb % 2]
            P.dma('pool', w[:], wkv[:, cb * 512:(cb + 1) * 512].rearrange("(c p) n -> p c n", p=128), reads=[wkv], writes=[w])
            if cb < 4:
                for m in range(4):
                    b_ = bk[2 + m % 2]
                    for c in range(16):
                        P.I('pe', 'matmul', reads=[w, hT], writes=[b_], out=b_[:], lhsT=w[:, c, m * 128:(m + 1) * 128], rhs=hT[:, c, :],
                            start=(c == 0), stop=(c == 15))
                    e_ = ev[ne % 2]; ne += 1
                    P.I('act', 'activation', reads=[b_], writes=[e_], out=e_[:], in_=b_[:], func=AF.Copy)
                    P.dma('sp', KT_d[(cb * 4 + m) * 128:(cb * 4 + m + 1) * 128, cols], e_[:], reads=[e_], writes=[KT_d])
            else:
                for t in range(4):
                    g = ps_ * 4 + t
                    b_ = bk[4 + t % 2]
                    for c in range(16):
                        P.I('pe', 'matmul', reads=[w, hT], writes=[b_], out=b_[:], lhsT=hT[:, c, t * 128:(t + 1) * 128], rhs=w[:, c, :],
                            start=(c == 0), stop=(c == 15))
                    e_ = ev[ne % 2]; ne += 1
                    P.I('dve', 'tensor_copy', reads=[b_], writes=[e_], out=e_[:], in_=b_[:])
                    P.dma('sp', V_d[g * 128:(g + 1) * 128, (cb - 4) * 512:(cb - 3) * 512], e_[:], reads=[e_], writes=[V_d])
        for t in range(4):
            g = ps_ * 4 + t
            for c in range(16):
                P.I('pe', 'matmul', reads=[wfl, hT], writes=[bk[6]], out=bk[6][:, 0:16], lhsT=hT[:, c, t * 128:(t + 1) * 128], rhs=wfl[:, c, :],
                    start=(c == 0), stop=(c == 15))
            P.I('dve', 'tensor_tensor', reads=[bk[6], bfb], writes=[z16], out=z16[:], in0=bk[6][:, 0:16], in1=bfb[:], op=ALU.add)
            P.I('act', 'activation', reads=[z16], writes=[z16], out=z16[:], in_=z16[:], func=AF.Exp, scale=-1.0)
            P.I('act', 'activation', reads=[z16], writes=[z16], out=z16[:], in_=z16[:], func=AF.Ln, bias=1.0)
            P.I('dve', 'tensor_scalar', reads=[z16], writes=[z16], out=z16[:], in0=z16[:], scalar1=-1.0, scalar2=0.0, op0=ALU.mult, op1=ALU.add)
            P.dma('sp', lf_d[g * 128:(g + 1) * 128, :], z16[:], reads=[z16], writes=[lf_d])


def phase_fox_a(P, cx, l, x_d, modv_d, nmix, wq_d, KT_d, V_d, lf_d, o_d, sg_d):
    bk = cx.bank
    bq = l - 2
    off = l * 12288
    A = P.sbuf([128, D], F32, name="A"); B = P.sbuf([128, D], F32, name="B")
    xt = P.sbuf([128, D], F32, name="xt"); h = P.sbuf([128, D], F32, name="h"); junk = P.sbuf([128, D], BF16, name="junk")
    make_AB(P, A, B, nmix, nmix[l:l + 1, :], modv_d, off, off + D, h)
    hT = P.sbuf([128, 16, 512], BF16, name="hT")
    ss = P.sbuf([128, 4], F32, name="ss"); rstd = P.sbuf([128, 4], F32, name="rstd")
    wblk = [P.sbuf([128, 16, 512], BF16, name="wb%d" % i) for i in range(2)]
    qTall = P.sbuf([128, 16, TOK], BF16, name="qTall")
    ev = [P.sbuf([128, 512], F32, name="ev0")] * 2
    lf = P.sbuf([128, 512], F32, name="lf"); Fw = P.sbuf([128, 512], F32, name="Fw"); Tot = P.sbuf([128, 512], F32, name="Tot")
    Pre = P.sbuf([128, 512], F32, name="Pre")
    P.dma('sp', lf[:].rearrange("s (b h) -> s b h", h=16), lf_d[:, :].rearrange("(b s) h -> s b h", s=128), reads=[lf_d], writes=[lf])
    P.I('pe', 'matmul', reads=[lf, cx.cst], writes=[bk[6]], out=bk[6][:], lhsT=cx.c(C_M1), rhs=lf[:], start=True, stop=True)
    P.I('pe', 'matmul', reads=[lf, cx.cst], writes=[bk[7]], out=bk[7][:], lhsT=cx.c(C_ONES), rhs=lf[:], start=True, stop=True)
    P.I('dve', 'tensor_copy', reads=[bk[7]], writes=[Tot], out=Tot[:], in_=bk[7][:])
    P.I('pool', 'memset', writes=[Pre], ap=Pre[:], constant=0.0)
    for b_i in range(1, 32):
        P.I('dve', 'tensor_tensor', reads=[Pre, Tot], writes=[Pre], out=Pre[:, b_i * 16:(b_i + 1) * 16], in0=Pre[:, (b_i - 1) * 16:b_i * 16],
            in1=Tot[:, (b_i - 1) * 16:b_i * 16], op=ALU.add)
    P.I('dve', 'tensor_tensor', reads=[bk[6], Pre], writes=[Fw], out=Fw[:], in0=bk[6][:], in1=Pre[:], op=ALU.add)
    Fv = Fw[:].rearrange("s (b h) -> s b h", h=16)
    Pv = Pre[:].rearrange("s (b h) -> s b h", h=16)
    ne = 0
    for ps_ in range(4):
        cols = slice(ps_ * 512, (ps_ + 1) * 512)
        for t in range(4):
            g = ps_ * 4 + t
            P.dma('sp', xt[:], x_d[g * 128:(g + 1) * 128, :], reads=[x_d], writes=[xt])
            normmod_T(P, cx, xt, A, B, h, junk, ss, rstd, hT, t * 128, [bk[0], bk[1]])
        for cb in range(8):
            w = wblk[cb % 2]
            P.dma('pool', w[:], wq_d[bq, :, cb * 512:(cb + 1) * 512].rearrange("(c p) n -> p c n", p=128), reads=[wq_d], writes=[w])
            if cb < 4:
                for m in range(4):
                    b_ = bk[2 + m % 2]
                    for c in range(16):
                        P.I('pe', 'matmul', reads=[w, hT], writes=[b_], out=b_[:], lhsT=w[:, c, m * 128:(m + 1) * 128], rhs=hT[:, c, :],
                            start=(c == 0), stop=(c == 15))
                    P.I('act', 'activation', reads=[b_], writes=[qTall], out=qTall[:, cb * 4 + m, cols], in_=b_[:], func=AF.Copy,
                        scale=float(128 ** -0.5))
            else:
                for t in range(4):
                    g = ps_ * 4 + t
                    b_ = bk[4 + t % 2]
                    for c in range(16):
                        P.I('pe', 'matmul', reads=[w, hT], writes=[b_], out=b_[:], lhsT=hT[:, c, t * 128:(t + 1) * 128], rhs=w[:, c, :],
                            start=(c == 0), stop=(c == 15))
                    e_ = ev[ne % 2]; ne += 1
                    P.I('act', 'activation', reads=[b_], writes=[e_], out=e_[:], in_=b_[:], func=AF.Sigmoid)
                    P.dma('sp', sg_d[g * 128:(g + 1) * 128, (cb - 4) * 512:(cb - 3) * 512], e_[:], reads=[e_], writes=[sg_d])
    KTh = [P.sbuf([128, 4096], BF16, name="KTh%d" % i) for i in range(2)]
    Vh = [P.sbuf([128, 32, 129], BF16, name="Vh%d" % i) for i in range(2)]
    for i in range(2):
        P.I('pool', 'memset', writes=[Vh[i]], ap=Vh[i][:], constant=1.0)
    mA = P.sbuf([128, 128], BF16, name="mA"); mB = P.sbuf([128, 128], BF16, name="mB")
    P.I('dve', 'tensor_copy', reads=[cx.cst], writes=[mA], out=mA[:], in_=cx.c(C_MA))
    P.I('dve', 'tensor_copy', reads=[cx.cst], writes=[mB], out=mB[:], in_=cx.c(C_MB))
    bias = [P.sbuf([128, 32], F32, name="bias%d" % i) for i in range(2)]
    PTb = [P.sbuf([128, 128], BF16, name="PT%d" % i) for i in range(4)]
    oh = [P.sbuf([128, 16, 128], F32, name="oh0")] * 2
    rZ = P.sbuf([128, 2], F32, name="rZ")
    npt = 0; nbi = 0; nst = 0
    for hh in range(16):
        Kt = KTh[hh % 2]; Vt = Vh[hh % 2]; o_h = oh[hh % 2]
        P.dma('pool', Kt[:], KT_d[hh * 128:(hh + 1) * 128, :], reads=[KT_d], writes=[Kt])
        P.dma('pool', Vt[:, :, 0:128], V_d[:, hh * 128:(hh + 1) * 128].rearrange("(b s) d -> s b d", s=128), reads=[V_d], writes=[Vt])
        for m in range(16):
            nblk = 2 * m + 2
            bi = bias[nbi % 2]; nbi += 1
            P.I('dve', 'scalar_tensor_tensor', reads=[Fw, Pre], writes=[bi], out=bi[:, 0:nblk], in0=Fv[:, 0:nblk, hh], scalar=-1.0,
                in1=Pv[:, 2 * m + 1, hh:hh + 1].to_broadcast([128, nblk]), op0=ALU.mult, op1=ALU.add)
            ob = bk[4 + (hh * 16 + m) % 2]
            for j in range(nblk):
                sb_ = bk[nst % 4]; nst += 1
                P.I('pe', 'matmul', reads=[Kt, qTall], writes=[sb_], out=sb_[:, 0:128], lhsT=Kt[:, j * 128:(j + 1) * 128],
                    rhs=qTall[:, hh, m * 128:(m + 1) * 128], start=True, stop=True)
                pt = PTb[npt % 4]; npt += 1
                P.I('act', 'activation', reads=[sb_, bi], writes=[pt], out=pt[:], in_=sb_[:, 0:128], func=AF.Exp, bias=bi[:, j:j + 1], scale=1.0)
                if j == nblk - 2:
                    P.I('dve', 'tensor_tensor', reads=[pt, mA], writes=[pt], out=pt[:], in0=pt[:], in1=mA[:], op=ALU.mult)
                if j == nblk - 1:
                    P.I('dve', 'tensor_tensor', reads=[pt, mB], writes=[pt], out=pt[:], in0=pt[:], in1=mB[:], op=ALU.mult)
                P.I('pe', 'matmul', reads=[pt, Vt], writes=[ob], out=ob[:, 0:129], lhsT=pt[:], rhs=Vt[:, j, :], start=(j == 0), stop=(j == nblk - 1))
            P.I('dve', 'reciprocal', reads=[ob], writes=[rZ], out=rZ[:, 0:1], in_=ob[:, 128:129])
            P.I('dve', 'tensor_scalar', reads=[ob, rZ], writes=[o_h], out=o_h[:, m, :], in0=ob[:, 0:128], scalar1=rZ[:, 0:1], scalar2=0.0,
                op0=ALU.mult, op1=ALU.add)
        P.dma('sp', o_d[:, hh * 128:(hh + 1) * 128].rearrange("(m t) d -> t m d", t=128), o_h[:], reads=[o_h], writes=[o_d])


def phase_fox_b(P, cx, l, x_d, o_d, sg_d, wout_d, xo_d, modv_d):
    bk = cx.bank
    off = l * 12288
    gate = P.sbuf([128, D], F32, name="gate")
    bcast_row(P, gate, gate[:], modv_d, modv_d[0:1, off + 2 * D:off + 3 * D])
    wout = P.sbuf([128, 16, D], BF16, name="wout")
    for n in range(4):
        P.dma('pool', wout[:, :, n * 512:(n + 1) * 512], wout_d[l - 2, :, n * 512:(n + 1) * 512].rearrange("(c p) n -> p c n", p=128),
              reads=[wout_d], writes=[wout])
    o = P.sbuf([128, D], F32, name="o"); sg = P.sbuf([128, D], F32, name="sg"); xt = P.sbuf([128, D], F32, name="xt")
    onT = P.sbuf([128, 16, 128], BF16, name="onT")
    for g in range(NT):
        rows = slice(g * 128, (g + 1) * 128)
        P.dma('sp', o[:], o_d[rows, :], reads=[o_d], writes=[o])
        P.dma('act', sg[:], sg_d[rows, :], reads=[sg_d], writes=[sg])
        P.dma('sp', xt[:], x_d[rows, :], reads=[x_d], writes=[xt])
        P.I('pool', 'tensor_tensor', reads=[o, sg], writes=[o], out=o[:], in0=o[:], in1=sg[:], op=ALU.mult)
        transpose_T(P, cx, o, onT, 0, [bk[4], bk[5]])
        for n in range(4):
            b_ = bk[n]
            for c in range(16):
                P.I('pe', 'matmul', reads=[onT, wout], writes=[b_], out=b_[:], lhsT=onT[:, c, :], rhs=wout[:, c, n * 512:(n + 1) * 512],
                    start=(c == 0), stop=(c == 15))
            P.I('dve', 'tensor_tensor', reads=[b_, gate], writes=[sg], out=sg[:, n * 512:(n + 1) * 512], in0=b_[:],
                in1=gate[:, n * 512:(n + 1) * 512], op=ALU.mult)
        P.I('pool', 'tensor_tensor', reads=[sg, xt], writes=[xt], out=xt[:], in0=sg[:], in1=xt[:], op=ALU.add)
        P.dma('sp', xo_d[rows, :], xt[:], reads=[xt], writes=[xo_d])


def phase_final(P, x_d, fn_d, o_d):
    A = P.sbuf([128, D], F32, name="A")
    bcast_row(P, A, A[:], fn_d, fn_d[0:1, :])
    xt = [P.sbuf([128, D], F32, name="xt%d" % i) for i in range(2)]
    h = [P.sbuf([128, D], F32, name="h%d" % i) for i in range(2)]
    junk = P.sbuf([128, D], BF16, name="junk")
    ss = P.sbuf([128, 4], F32, name="ss"); rstd = P.sbuf([128, 4], F32, name="rstd")
    for g in range(NT):
        x_, h_ = xt[g % 2], h[g % 2]
        P.dma('sp', x_[:], x_d[g * 128:(g + 1) * 128, :], reads=[x_d], writes=[x_])
        P.I('act', 'activation', reads=[x_], writes=[junk, ss], out=junk[:], in_=x_[:], func=AF.Square, accum_out=ss[:, 0:1])
        rstd_from_ss(P, ss, rstd, 1, 1.0 / D)
        P.I('dve', 'scalar_tensor_tensor', reads=[x_, rstd, A], writes=[h_], out=h_[:], in0=x_[:], scalar=rstd[:, 0:1], in1=A[:],
            op0=ALU.mult, op1=ALU.mult)
        P.dma('sp', o_d[g * 128:(g + 1) * 128, :], h_[:], reads=[h_], writes=[o_d])


def phase_xsel(P, cx, xfull_d, xB_d):
    omp = P.sbuf([128, 1], F32, name="omp")
    P.I('dve', 'tensor_scalar', reads=[cx.cst], writes=[omp], out=omp[:], in0=cx.cst[:, C_P:C_P + 1], scalar1=-1.0, scalar2=1.0,
        op0=ALU.mult, op1=ALU.add)
    xe = [P.sbuf([128, D], F32, name="xe%d" % i) for i in range(2)]
    xo = [P.sbuf([128, D], F32, name="xo%d" % i) for i in range(2)]
    for m in range(16):
        a, b_ = xe[m % 2], xo[m % 2]
        P.dma('sp', a[:], xfull_d[(2 * m) * 128:(2 * m + 1) * 128, :], reads=[xfull_d], writes=[a])
        P.dma('act', b_[:], xfull_d[(2 * m + 1) * 128:(2 * m + 2) * 128, :], reads=[xfull_d], writes=[b_])
        P.I('dve', 'tensor_scalar', reads=[a, omp], writes=[a], out=a[:], in0=a[:], scalar1=omp[:, 0:1], scalar2=0.0,
            op0=ALU.mult, op1=ALU.add)
        P.I('dve', 'scalar_tensor_tensor', reads=[b_, a, cx.cst], writes=[a], out=a[:], in0=b_[:], scalar=cx.cst[:, C_P:C_P + 1],
            in1=a[:], op0=ALU.mult, op1=ALU.add)
        P.dma('sp', xB_d[m * 128:(m + 1) * 128, :], a[:], reads=[a], writes=[xB_d])


def build_fused():
    nc = bass.Bass("TRN2", target_bir_lowering=False)
    P = Prog(nc)
    EI = "ExternalInput"
    S = 4096
    cst = P.dram("cst", [128, NCST], F32, kind=EI)
    x_in = P.dram("x", [S, D], F32, kind=EI)
    c_d = P.dram("c", [1, D], F32, kind=EI)
    Wm = P.dram("Wm", [D, MODN], F32, kind=EI)
    Bm = P.dram("Bm", [1, MODN], F32, kind=EI)
    nmix = P.dram("norm_mix", [4, D], F32, kind=EI)
    nffn = P.dram("norm_ffn", [4, D], F32, kind=EI)
    win = P.dram("gla_w_in", [2, D, 6160], F32, kind=EI)
    wg2 = P.dram("gla_w_gate2", [2, 16, 1024], F32, kind=EI)
    bg2 = P.dram("gla_b_gate2", [2, 1024], F32, kind=EI)
    gnorm = P.dram("gla_norm", [2, 512], F32, kind=EI)
    gwout = P.dram("gla_w_out", [2, D, D], F32, kind=EI)
    kvn = P.dram("kv_norm", [1, D], F32, kind=EI)
    wkv = P.dram("fox_w_kv", [D, 4112], F32, kind=EI)
    bf_d = P.dram("fox_b_f", [1, 16], F32, kind=EI)
    fwq = P.dram("fox_w_q", [2, D, 2 * D], F32, kind=EI)
    fwo = P.dram("fox_w_out", [2, D, D], F32, kind=EI)
    pwq = P.dram("peer_w_q", [4, D, D], F32, kind=EI)
    subk = P.dram("peer_subkeys", [4, 8, 2, 128, 128], F32, kind=EI)
    uT = P.dram("uT", [4, D, 16384], F32, kind=EI)
    pv = P.dram("pv", [4, 16384, D], F32, kind=EI)
    fn_d = P.dram("final_norm", [1, D], F32, kind=EI)
    out_d = P.dram("out", [TOK, D], F32, kind="ExternalOutput")
    modv = P.dram("modv", [1, MODN], F32)
    XA = P.dram("XA", [S, D], F32); XB = P.dram("XB", [S, D], F32)
    oloc = P.dram("oloc", [S, D], F32); gs = P.dram("gs", [S, D], F32)
    KT = P.dram("KT", [D, S], F32); V = P.dram("V", [S, D], F32); lf = P.dram("lf", [S, 16], F32)
    xs0 = P.dram("xs0", [TOK, D], F32); xs1 = P.dram("xs1", [TOK, D], F32)
    o_s = P.dram("o_s", [TOK, D], F32); sg_s = P.dram("sg_s", [TOK, D], F32)
    cx = Ctx(P, cst)
    P.cx = cx
    with P.phase():
        phase_mod(P, c_d, Wm, Bm, modv)
    xin = x_in
    for l in range(2):
        with P.phase():
            gla_a(P, cx, l, xin, modv, nmix, win, wg2, bg2, oloc, gs, nt=32)
        with P.phase():
            gla_b(P, cx, l, xin, XA, modv, gnorm, gwout, oloc, gs, nt=32)
        with P.phase():
            peer(P, cx, l, XA, XB, modv, nffn, pwq, subk, uT, pv, nt=32)
        xin = XB
    with P.phase():
        phase_kv(P, cx, XB, modv, kvn, wkv, bf_d, KT, V, lf, 32)
    with P.phase():
        phase_xsel(P, cx, XB, xs0)
    for l in (2, 3):
        with P.phase():
            phase_fox_a(P, cx, l, xs0, modv, nmix, fwq, KT, V, lf, o_s, sg_s)
        with P.phase():
            phase_fox_b(P, cx, l, xs0, o_s, sg_s, fwo, xs1, modv)
        with P.phase():
            peer(P, cx, l, xs1, xs0, modv, nffn, pwq, subk, uT, pv, nt=16)
    with P.phase():
        phase_final(P, xs0, fn_d, out_d)
    P.finish()
    return nc


def kernel(x, c, ada_w, ada_b, norm_mix, norm_ffn, gla_w_in, gla_w_gate2, gla_b_gate2, gla_norm, gla_w_out, kv_norm,
           kv_ada_w, kv_ada_b, fox_w_kv, fox_b_f, fox_w_q, fox_w_out, peer_w_q, peer_subkeys, peer_u, peer_v, final_norm):
    f = lambda a: np.ascontiguousarray(np.asarray(a, dtype=np.float32))
    x = f(x); c = f(c)
    Wm = np.ascontiguousarray(np.concatenate([f(ada_w[l]) for l in range(4)] + [f(kv_ada_w)], axis=1))
    Bm = np.ascontiguousarray(np.concatenate([f(ada_b[l]) for l in range(4)] + [f(kv_ada_b)])[None, :])
    uT = np.ascontiguousarray(np.transpose(f(peer_u), (0, 2, 1)))
    shared = dict(Wm=Wm, Bm=Bm, norm_mix=f(norm_mix), norm_ffn=f(norm_ffn), gla_w_in=f(gla_w_in), gla_w_gate2=f(gla_w_gate2),
                  gla_b_gate2=f(gla_b_gate2), gla_norm=f(gla_norm), gla_w_out=f(gla_w_out), kv_norm=f(kv_norm)[None, :],
                  fox_w_kv=f(fox_w_kv), fox_b_f=f(fox_b_f)[None, :], fox_w_q=f(fox_w_q), fox_w_out=f(fox_w_out),
                  peer_w_q=f(peer_w_q), peer_subkeys=f(peer_subkeys), uT=uT, pv=f(peer_v), final_norm=f(final_norm)[None, :])
    ins = [dict(shared, cst=make_consts(r % 2), x=x[r // 2], c=c[r // 2:r // 2 + 1]) for r in range(8)]
    res = run_bass_kernel_spmd(build_fused(), ins, core_ids=list(range(8))).results
    out = np.empty((4, 32, 128, D), np.float32)
    for r in range(8):
        out[r // 2, (r % 2)::2] = res[r]["out"].reshape(16, 128, D)
    return out.reshape(4, 4096, D)
```

```python
import contextlib
import numpy as np
import concourse.bass as bass
import concourse.mybir as mybir

F32 = mybir.dt.float32
BF16 = mybir.dt.bfloat16
AF = mybir.ActivationFunctionType
ALU = mybir.AluOpType
AX = mybir.AxisListType

ENGS = ['pe', 'act', 'dve', 'pool', 'sp']
DMA_SLOTS = {'sp': 12, 'act': 4, 'pool': 6}


class Buf:
    def __init__(self, ap_full, name=""):
        self.t = ap_full
        self.name = name
        self.last_w = None
        self.readers = []

    def __getitem__(self, k):
        return self.t[k]


class Prog:
    def __init__(self, nc):
        self.nc = nc
        self.es = contextlib.ExitStack()
        self.ops = {e: [] for e in ENGS}
        self.cnt = {e: 0 for e in ENGS}
        self.seen = {e: {f: 0 for f in ENGS} for e in ENGS}
        self.sem = {}
        for e in ENGS:
            self.sem[e] = self.es.enter_context(nc.semaphore("s_" + e))
        self.dsem = {}
        self.dval = {}
        self.dseen = {e: {} for e in ENGS}
        self.drr = {q: 0 for q in DMA_SLOTS}
        for q, n in DMA_SLOTS.items():
            for i in range(n):
                key = (q, i)
                self.dsem[key] = self.es.enter_context(nc.semaphore("d_%s%d" % (q, i)))
                self.dval[key] = 0
        self.n_alloc = 0

    def sbuf(self, shape, dtype=F32, name=None, stack=None):
        self.n_alloc += 1
        name = name or ("sb%d" % self.n_alloc)
        t = (stack or getattr(self, 'cur', None) or self.es).enter_context(self.nc.sbuf_tensor(name + "_%d" % self.n_alloc, list(shape), dtype))
        return Buf(t, name)

    def psum(self, shape, dtype=F32, name=None, stack=None):
        self.n_alloc += 1
        name = name or ("ps%d" % self.n_alloc)
        t = (stack or self.es).enter_context(self.nc.psum_tensor(name + "_%d" % self.n_alloc, list(shape), dtype))
        return Buf(t, name)

    def dram(self, name, shape, dtype=F32, kind="Internal"):
        t = self.nc.dram_tensor(name, list(shape), dtype, kind=kind)
        return Buf(t.ap(), name)

    def _collect(self, eng, reads, writes):
        deps = []
        for b in reads:
            if b.last_w is not None:
                deps.append(b.last_w)
        for b in writes:
            if b.last_w is not None:
                deps.append(b.last_w)
            deps.extend(b.readers)
        waits = []
        for d in deps:
            if d[0] == 'eng':
                _, f, k = d
                if f == eng and eng == 'pe':
                    continue
                if self.seen[eng][f] < k:
                    self.seen[eng][f] = k
                    waits.append((self.sem[f], k))
            else:
                _, key, val = d
                if self.dseen[eng].get(key, 0) < val:
                    self.dseen[eng][key] = val
                    waits.append((self.dsem[key], val))
        best = {}
        for s, v in waits:
            if id(s) not in best or best[id(s)][1] < v:
                best[id(s)] = (s, v)
        return list(best.values())

    def _mark(self, tok, reads, writes):
        for b in writes:
            b.last_w = tok
            b.readers = []
        for b in reads:
            if b not in writes:
                b.readers.append(tok)
                if len(b.readers) > 64:
                    b.readers = b.readers[-64:]

    def op(self, eng, fn, reads=(), writes=()):
        waits = self._collect(eng, reads, writes)
        self.cnt[eng] += 1
        k = self.cnt[eng]
        sem = self.sem[eng]

        def run(e, waits=waits, fn=fn, sem=sem):
            for s, v in waits:
                e.wait_ge(s, v)
            fn(e).then_inc(sem, 1)
        self.ops[eng].append(run)
        self._mark(('eng', eng, k), reads, writes)

    def I(self, eng, meth, reads=(), writes=(), **kw):
        self.op(eng, lambda e, meth=meth, kw=kw: getattr(e, meth)(**kw), reads, writes)

    def dma(self, q, out, in_, reads=(), writes=(), **kw):
        waits = self._collect(q, reads, writes)
        n = DMA_SLOTS[q]
        i = self.drr[q]
        self.drr[q] = (i + 1) % n
        key = (q, i)
        prev = self.dval[key]
        if prev > 0 and self.dseen[q].get(key, 0) < prev:
            self.dseen[q][key] = prev
            waits.append((self.dsem[key], prev))
        self.dval[key] = prev + 16
        val = prev + 16
        sem = self.dsem[key]

        def run(e, waits=waits, out=out, in_=in_, sem=sem, kw=kw):
            for s, v in waits:
                e.wait_ge(s, v)
            e.dma_start(out=out, in_=in_, **kw).then_inc(sem, 16)
        self.ops[q].append(run)
        self._mark(('dma', key, val), reads, writes)

    def barrier(self):
        for e in ENGS:
            waits = []
            for f in ENGS:
                if f != e and self.seen[e][f] < self.cnt[f]:
                    self.seen[e][f] = self.cnt[f]
                    waits.append((self.sem[f], self.cnt[f]))
            for key, val in self.dval.items():
                if val > 0 and self.dseen[e].get(key, 0) < val:
                    self.dseen[e][key] = val
                    waits.append((self.dsem[key], val))

            def run(en, waits=waits):
                for s, v in waits:
                    en.wait_ge(s, v)
            self.ops[e].append(run)

    def collective(self, kind, groups, in_buf, in_ap, out_buf, out_ap):
        q = 'pool'
        waits = self._collect(q, [in_buf], [out_buf])
        n = DMA_SLOTS[q]
        i = self.drr[q]
        self.drr[q] = (i + 1) % n
        key = (q, i)
        prev = self.dval[key]
        if prev > 0 and self.dseen[q].get(key, 0) < prev:
            self.dseen[q][key] = prev
            waits.append((self.dsem[key], prev))
        self.dval[key] = prev + 16
        val = prev + 16
        sem = self.dsem[key]

        def run(e, waits=waits, sem=sem):
            for s_, v in waits:
                e.wait_ge(s_, v)
            e.collective_compute(kind, ALU.bypass, replica_groups=groups, ins=[in_ap], outs=[out_ap]).then_inc(sem, 16)
        self.ops[q].append(run)
        self._mark(('dma', key, val), [in_buf], [out_buf])

    @contextlib.contextmanager
    def phase(self):
        st = contextlib.ExitStack()
        self.cur = st
        try:
            yield
            self.flush()
        finally:
            self.cur = None
            st.close()

    def flush(self):
        self.barrier()
        self._emit()
        self.ops = {e: [] for e in ENGS}
        for b in ():
            pass

    def finish(self):
        self.barrier()
        self._emit()
        self.es.close()

    def _emit(self):
        nc = self.nc
        with nc.Block() as block:
            @block.tensor
            def _(e):
                for f in self.ops['pe']:
                    f(e)

            @block.scalar
            def _(e):
                for f in self.ops['act']:
                    f(e)

            @block.vector
            def _(e):
                for f in self.ops['dve']:
                    f(e)

            @block.gpsimd
            def _(e):
                for f in self.ops['pool']:
                    f(e)

            @block.sync
            def _(e):
                for f in self.ops['sp']:
                    f(e)

from concourse.bass_utils import run_bass_kernel_spmd

D = 2048
NT = 16
TOK = 2048
EPS = 1e-6
MODN = 4 * 12288 + 4096

C_ID, C_M1, C_M2, C_LINC, C_USUF, C_ONES, C_MA, C_MB, C_P = 0, 128, 256, 384, 512, 640, 768, 896, 1024
NCST = 1025


def make_consts(p):
    c = np.zeros((128, NCST), np.float32)
    s = np.arange(128)[:, None]
    t = np.arange(128)[None, :]
    c[:, C_ID:C_ID + 128] = (s == t)
    m1 = (s <= t).astype(np.float32)
    c[:, C_M1:C_M1 + 128] = m1
    c[:, C_M2:C_M2 + 128] = ((s > t) & ((s // 64) == (t // 64)))
    c[:, C_LINC:C_LINC + 128] = m1 * (-1.0 / 16.0)
    c[:, C_USUF:C_USUF + 128] = (s > t) * (-1.0 / 16.0)
    c[:, C_ONES:C_ONES + 128] = 1.0
    if p == 0:
        c[:, C_MA:C_MA + 128] = m1
        c[:, C_MB:C_MB + 128] = 0.0
    else:
        c[:, C_MA:C_MA + 128] = 1.0
        c[:, C_MB:C_MB + 128] = m1
    c[:, C_P] = float(p)
    return c


class Ctx:
    def __init__(self, P, cst_d):
        self.P = P
        self.bank = [P.psum([128, 512], F32, name="bank%d" % i) for i in range(8)]
        self.cst = P.sbuf([128, NCST], F32, name="cst")
        P.dma('sp', self.cst[:], cst_d[:, :], reads=[cst_d], writes=[self.cst])
        self.small = {}

    def c(self, off, n=128):
        return self.cst[:, off:off + n]


def bcast_row(P, dst, dst_ap, src_buf, src_ap_row, q='sp'):
    P.dma(q, dst_ap, src_ap_row.partition_broadcast(128), reads=[src_buf], writes=[dst])


def rstd_from_ss(P, ss, rstd, n, inv_n):
    P.I('dve', 'tensor_scalar', reads=[ss], writes=[rstd], out=rstd[:, 0:n], in0=ss[:, 0:n],
        scalar1=inv_n, scalar2=EPS, op0=ALU.mult, op1=ALU.add)
    P.I('act', 'activation', reads=[rstd], writes=[rstd], out=rstd[:, 0:n], in_=rstd[:, 0:n], func=AF.Sqrt)
    P.I('dve', 'reciprocal', reads=[rstd], writes=[rstd], out=rstd[:, 0:n], in_=rstd[:, 0:n])


def normmod_T(P, cx, xt, A, B, h, junk, ss, rstd, hT, tcol, banks):
    P.I('act', 'activation', reads=[xt], writes=[junk, ss], out=junk[:], in_=xt[:], func=AF.Square,
        accum_out=ss[:, 0:1])
    rstd_from_ss(P, ss, rstd, 1, 1.0 / D)
    P.I('dve', 'scalar_tensor_tensor', reads=[xt, rstd, A], writes=[h], out=h[:], in0=xt[:],
        scalar=rstd[:, 0:1], in1=A[:], op0=ALU.mult, op1=ALU.mult)
    if B is not None:
        P.I('pool', 'tensor_tensor', reads=[h, B], writes=[h], out=h[:], in0=h[:], in1=B[:], op=ALU.add)
    transpose_T(P, cx, h, hT, tcol, banks)


def transpose_T(P, cx, h, hT, tcol, banks, nch=16):
    for g in range(nch // 4):
        bk = banks[g % len(banks)]
        for k in range(4):
            c = g * 4 + k
            P.I('pe', 'transpose', reads=[h, cx.cst], writes=[bk], out=bk[:, k * 128:(k + 1) * 128],
                in_=h[:, c * 128:(c + 1) * 128], identity=cx.c(C_ID))
        eng = 'act' if g % 2 == 0 else 'dve'
        src = bk[:].rearrange("p (k t) -> p k t", k=4)
        dst = hT[:, g * 4:(g + 1) * 4, tcol:tcol + 128]
        if eng == 'act':
            P.I('act', 'activation', reads=[bk], writes=[hT], out=dst, in_=src, func=AF.Copy)
        else:
            P.I('dve', 'tensor_copy', reads=[bk], writes=[hT], out=dst, in_=src)


def make_AB(P, A, B, gain_d, gain_row, modv_d, off_shift, off_scale, tmp):
    bcast_row(P, A, A[:], gain_d, gain_row)
    if modv_d is not None:
        bcast_row(P, tmp, tmp[:], modv_d, modv_d[0:1, off_scale:off_scale + D])
        bcast_row(P, B, B[:], modv_d, modv_d[0:1, off_shift:off_shift + D])
        P.I('dve', 'scalar_tensor_tensor', reads=[tmp, A], writes=[A], out=A[:], in0=tmp[:], scalar=1.0,
            in1=A[:], op0=ALU.add, op1=ALU.mult)


def phase_mod(P, c_d, w_d, b_d, o_d):
    ca = P.sbuf([128, 16], F32, name="ca")
    P.dma('sp', ca[:], c_d[0:1, :].rearrange("o (c p) -> p (o c)", p=128), reads=[c_d], writes=[ca],
          allow_slow_non_contiguous=True)
    P.I('act', 'activation', reads=[ca], writes=[ca], out=ca[:], in_=ca[:], func=AF.Silu)
    wt = [P.sbuf([128, 16, 512], F32, name="wt%d" % i) for i in range(2)]
    bt = [P.sbuf([1, 512], F32, name="bt%d" % i) for i in range(2)]
    ot = [P.sbuf([1, 512], F32, name="ot%d" % i) for i in range(2)]
    ps = P.cx.bank
    for n in range(MODN // 512):
        w, b_, o, p_ = wt[n % 2], bt[n % 2], ot[n % 2], ps[n % 2]
        P.dma('sp' if n % 2 == 0 else 'act', w[:], w_d[:, n * 512:(n + 1) * 512].rearrange("(c p) n -> p c n", p=128),
              reads=[w_d], writes=[w])
        P.dma('pool', b_[:], b_d[0:1, n * 512:(n + 1) * 512], reads=[b_d], writes=[b_])
        for c in range(16):
            P.I('pe', 'matmul', reads=[ca, w], writes=[p_], out=p_[0:1, :], lhsT=ca[:, c:c + 1], rhs=w[:, c, :],
                start=(c == 0), stop=(c == 15))
        P.I('dve', 'tensor_tensor', reads=[p_, b_], writes=[o], out=o[:], in0=p_[0:1, :], in1=b_[:], op=ALU.add)
        P.dma('sp', o_d[0:1, n * 512:(n + 1) * 512], o[:], reads=[o], writes=[o_d])


def gla_a(P, cx, l, x_d, modv_d, nmix_d, win_d, wg2_d, bg2_d, oloc_d, gs_d, qc_d=None, send_d=None, nt=NT):
    bk = cx.bank
    A = P.sbuf([128, D], F32, name="A"); B = P.sbuf([128, D], F32, name="B");
    off = l * 12288
    PW = 512
    TP = 4
    hT = P.sbuf([128, 16, PW], BF16, name="hT")
    xt = [P.sbuf([128, D], F32, name="xt0")] * 2
    h = [P.sbuf([128, D], F32, name="h0")] * 2
    make_AB(P, A, B, nmix_d, nmix_d[l:l + 1, :], modv_d, off, off + D, h[0])
    junk = P.sbuf([128, D], BF16, name="junk")
    ss = P.sbuf([128, 4], F32, name="ss"); rstd = P.sbuf([128, 4], F32, name="rstd")
    wblk = [P.sbuf([128, 16, 256], BF16, name="wblk%d" % i) for i in range(2)]
    wgl = P.sbuf([128, 16, 16], BF16, name="wgl")
    P.dma('pool', wgl[:], win_d[l, :, 6144:6160].rearrange("(c p) n -> p c n", p=128), reads=[win_d], writes=[wgl])
    wg2 = P.sbuf([17, 1024], F32, name="wg2")
    P.dma('sp', wg2[0:16, :], wg2_d[l, :, :], reads=[wg2_d], writes=[wg2])
    P.dma('sp', wg2[16:17, :], bg2_d[l:l + 1, :], reads=[bg2_d], writes=[wg2])
    glT = P.sbuf([17, PW], F32, name="glT")
    P.I('pool', 'memset', writes=[glT], ap=glT[:], constant=1.0)
    qT = P.sbuf([128, 8, PW], BF16, name="qT"); kT = P.sbuf([128, 8, PW], BF16, name="kT")
    ktm = [P.sbuf([128, 1024], F32, name="ktm%d" % i) for i in range(TP)]
    vbf = [P.sbuf([128, 2048], BF16, name="vbf%d" % i) for i in range(TP)]
    gst = [P.sbuf([128, 512], F32, name="gst%d" % i) for i in range(2)]
    ez = P.sbuf([128, 1024], F32, name="ez"); la = P.sbuf([128, 1024], F32, name="la")
    EA = P.sbuf([128, 8, 128], F32, name="EA"); EB = P.sbuf([128, 8, 128], F32, name="EB")
    ER = P.sbuf([128, 1024], F32, name="ER")
    QA = P.sbuf([128, 8, 128], BF16, name="QA"); QB = P.sbuf([128, 8, 128], BF16, name="QB")
    KA = P.sbuf([128, 8, 128], BF16, name="KA"); KB = P.sbuf([128, 8, 128], BF16, name="KB")
    KE = P.sbuf([128, 1024], BF16, name="KE")
    QC = P.sbuf([128, 8, 128], F32, name="QC")
    cumP = P.sbuf([128, 8], F32, name="cumP"); expP = P.sbuf([128, 8], F32, name="expP")
    P.I('pool', 'memset', writes=[cumP], ap=cumP[:], constant=0.0)
    t1 = P.sbuf([128, 4, 128], F32, name="t1"); t2 = P.sbuf([128, 4, 128], F32, name="t2")
    PT = P.sbuf([128, 4, 128], BF16, name="PT")
    S32 = [P.sbuf([128, 512], F32, name="S32_%d" % i) for i in range(8)]
    Sbf = [P.sbuf([128, 512], BF16, name="Sbf_%d" % i) for i in range(8)]
    for i in range(8):
        P.I('pool', 'memset', writes=[S32[i]], ap=S32[i][:], constant=0.0)
        P.I('pool', 'memset', writes=[Sbf[i]], ap=Sbf[i][:], constant=0.0)
    ot = [P.sbuf([128, 2048], F32, name="ot0")] * 2
    m1b = cx.c(C_M1).unsqueeze(1).to_broadcast([128, 4, 128])
    m2b = cx.c(C_M2).unsqueeze(1).to_broadcast([128, 4, 128])
    nb = 0
    for ps_ in range(nt // TP):
        for t in range(TP):
            g = ps_ * TP + t
            P.dma('sp', xt[t % 2][:], x_d[g * 128:(g + 1) * 128, :], reads=[x_d], writes=[xt[t % 2]])
            normmod_T(P, cx, xt[t % 2], A, B, h[t % 2], junk, ss, rstd, hT, t * 128, [bk[0], bk[1]])
        for cb in range(24):
            w = wblk[nb % 2]; nb += 1
            P.dma('pool', w[:], win_d[l, :, cb * 256:(cb + 1) * 256].rearrange("(c p) n -> p c n", p=128),
                  reads=[win_d], writes=[w])
            if cb < 8:
                dstT = qT if cb < 4 else kT
                for m in range(2):
                    b_ = bk[(cb * 2 + m) % 2]
                    for c in range(16):
                        P.I('pe', 'matmul', reads=[w, hT], writes=[b_], out=b_[:, 0:PW], lhsT=w[:, c, m * 128:(m + 1) * 128],
                            rhs=hT[:, c, :], start=(c == 0), stop=(c == 15))
                    P.I('act', 'activation', reads=[b_], writes=[dstT], out=dstT[:, (cb % 4) * 2 + m, :], in_=b_[:, 0:PW],
                        func=AF.Copy)
            if cb >= 4:
                for t in range(TP):
                    g = ps_ * TP + t
                    b_ = bk[2 + (t % 2)]
                    for c in range(16):
                        P.I('pe', 'matmul', reads=[w, hT], writes=[b_], out=b_[:, 0:256], lhsT=hT[:, c, t * 128:(t + 1) * 128],
                            rhs=w[:, c, :], start=(c == 0), stop=(c == 15))
                    if cb < 8:
                        P.I('dve', 'tensor_copy', reads=[b_], writes=[ktm[t]], out=ktm[t][:, (cb - 4) * 256:(cb - 3) * 256],
                            in_=b_[:, 0:256])
                    elif cb < 16:
                        P.I('dve', 'tensor_copy', reads=[b_], writes=[vbf[t]], out=vbf[t][:, (cb - 8) * 256:(cb - 7) * 256],
                            in_=b_[:, 0:256])
                    else:
                        go = gst[(cb * TP + t) % 2]
                        P.I('act', 'activation', reads=[b_], writes=[go], out=go[:, 0:256], in_=b_[:, 0:256], func=AF.Silu)
                        P.dma('sp', gs_d[g * 128:(g + 1) * 128, (cb - 16) * 256:(cb - 15) * 256], go[:, 0:256], reads=[go],
                              writes=[gs_d])
        for c in range(16):
            P.I('pe', 'matmul', reads=[wgl, hT], writes=[bk[0]], out=bk[0][0:16, 0:PW], lhsT=wgl[:, c, :], rhs=hT[:, c, :],
                start=(c == 0), stop=(c == 15))
        P.I('act', 'activation', reads=[bk[0]], writes=[glT], out=glT[0:16, :], in_=bk[0][0:16, 0:PW], func=AF.Copy)
        for t in range(TP):
            g = ps_ * TP + t
            for n in range(2):
                P.I('pe', 'matmul', reads=[glT, wg2], writes=[bk[4 + n]], out=bk[4 + n][:], lhsT=glT[:, t * 128:(t + 1) * 128],
                    rhs=wg2[:, n * 512:(n + 1) * 512], start=True, stop=True)
                P.I('act', 'activation', reads=[bk[4 + n]], writes=[ez], out=ez[:, n * 512:(n + 1) * 512], in_=bk[4 + n][:],
                    func=AF.Exp, scale=-1.0)
            P.I('act', 'activation', reads=[ez], writes=[la], out=la[:], in_=ez[:], func=AF.Ln, bias=1.0)
            for m in range(8):
                b_ = bk[4 + m // 4]
                P.I('pe', 'matmul', reads=[la, cx.cst], writes=[b_], out=b_[:, (m % 4) * 128:(m % 4 + 1) * 128],
                    lhsT=la[:, m * 128:(m + 1) * 128], rhs=cx.c(C_LINC), start=True, stop=True)
            for n in range(2):
                P.I('pe', 'matmul', reads=[la, cx.cst], writes=[bk[6 + n]], out=bk[6 + n][:], lhsT=cx.c(C_USUF),
                    rhs=la[:, n * 512:(n + 1) * 512], start=True, stop=True)
            for n in range(2):
                src = bk[4 + n][:].rearrange("p (k t) -> p k t", k=4)
                P.I('act', 'activation', reads=[bk[4 + n]], writes=[EA], out=EA[:, n * 4:(n + 1) * 4, :], in_=src, func=AF.Exp)
                P.I('act', 'activation', reads=[bk[4 + n]], writes=[EB], out=EB[:, n * 4:(n + 1) * 4, :], in_=src, func=AF.Exp,
                    scale=-1.0)
                P.I('act', 'activation', reads=[bk[6 + n]], writes=[ER], out=ER[:, n * 512:(n + 1) * 512], in_=bk[6 + n][:],
                    func=AF.Exp)
            if qc_d is not None:
                P.I('act', 'activation', reads=[cumP], writes=[expP], out=expP[:], in_=cumP[:], func=AF.Exp)
            for n in range(2 if qc_d is not None else 0):
                P.I('dve', 'tensor_tensor', reads=[cumP, bk[4 + n]], writes=[cumP], out=cumP[:, n * 4:(n + 1) * 4],
                    in0=cumP[:, n * 4:(n + 1) * 4],
                    in1=bk[4 + n][:].rearrange("p (k t) -> p k t", k=4)[:, :, 127], op=ALU.add)
            qs = qT[:, :, t * 128:(t + 1) * 128]
            ks = kT[:, :, t * 128:(t + 1) * 128]
            P.I('dve', 'scalar_tensor_tensor', reads=[qT, EA], writes=[QA], out=QA[:], in0=qs, scalar=0.0625, in1=EA[:],
                op0=ALU.mult, op1=ALU.mult)
            P.I('dve', 'scalar_tensor_tensor', reads=[qT, EB], writes=[QB], out=QB[:], in0=qs, scalar=0.0625, in1=EB[:],
                op0=ALU.mult, op1=ALU.mult)
            P.I('pool', 'tensor_tensor', reads=[kT, EA], writes=[KA], out=KA[:], in0=ks, in1=EA[:], op=ALU.mult)
            P.I('pool', 'tensor_tensor', reads=[kT, EB], writes=[KB], out=KB[:], in0=ks, in1=EB[:], op=ALU.mult)
            P.I('pool', 'tensor_tensor', reads=[ktm[t], ER], writes=[KE], out=KE[:], in0=ktm[t][:], in1=ER[:], op=ALU.mult)
            if qc_d is not None:
                P.I('dve', 'scalar_tensor_tensor', reads=[qT, EA, expP], writes=[QC], out=QC[:], in0=qs, scalar=0.0625, in1=EA[:],
                    op0=ALU.mult, op1=ALU.mult)
                P.I('dve', 'tensor_tensor', reads=[QC, expP], writes=[QC], out=QC[:], in0=QC[:],
                    in1=expP[:].unsqueeze(2).to_broadcast([128, 8, 128]), op=ALU.mult)
                P.dma('sp', qc_d[:, g * 128:(g + 1) * 128].rearrange("(m p) t -> p m t", p=128), QC[:], reads=[QC], writes=[qc_d])
            for hd in range(4):
                for j in range(2):
                    m = 2 * hd + j
                    P.I('pe', 'matmul', reads=[KB, QA], writes=[bk[0]], out=bk[0][:, hd * 128:(hd + 1) * 128], lhsT=KB[:, m, :],
                        rhs=QA[:, m, :], start=(j == 0), stop=(j == 1))
                for j in range(2):
                    m = 2 * hd + j
                    P.I('pe', 'matmul', reads=[KA, QB], writes=[bk[1]], out=bk[1][:, hd * 128:(hd + 1) * 128], lhsT=KA[:, m, :],
                        rhs=QB[:, m, :], start=(j == 0), stop=(j == 1))
            P.I('dve', 'tensor_tensor', reads=[bk[0], cx.cst], writes=[t1], out=t1[:],
                in0=bk[0][:].rearrange("p (k t) -> p k t", k=4), in1=m1b, op=ALU.mult)
            P.I('dve', 'tensor_tensor', reads=[bk[1], cx.cst], writes=[t2], out=t2[:],
                in0=bk[1][:].rearrange("p (k t) -> p k t", k=4), in1=m2b, op=ALU.mult)
            P.I('pool', 'tensor_tensor', reads=[t1, t2], writes=[PT], out=PT[:], in0=t1[:], in1=t2[:], op=ALU.add)
            o_ = ot[t % 2]
            for hd in range(4):
                b_ = bk[4 + hd]
                for j in range(2):
                    m = 2 * hd + j
                    P.I('pe', 'matmul', reads=[QA, Sbf[m]], writes=[b_], out=b_[:], lhsT=QA[:, m, :], rhs=Sbf[m][:],
                        start=(j == 0), stop=False)
                P.I('pe', 'matmul', reads=[PT, vbf[t]], writes=[b_], out=b_[:], lhsT=PT[:, hd, :],
                    rhs=vbf[t][:, hd * 512:(hd + 1) * 512], start=False, stop=True)
                P.I('act', 'activation', reads=[b_], writes=[o_], out=o_[:, hd * 512:(hd + 1) * 512], in_=b_[:], func=AF.Copy)
            P.dma('sp', oloc_d[g * 128:(g + 1) * 128, :], o_[:], reads=[o_], writes=[oloc_d])
            for hd in range(4):
                for j in range(2):
                    m = 2 * hd + j
                    b_ = bk[2 + (m % 2)]
                    P.I('pe', 'matmul', reads=[KE, vbf[t]], writes=[b_], out=b_[:], lhsT=KE[:, m * 128:(m + 1) * 128],
                        rhs=vbf[t][:, hd * 512:(hd + 1) * 512], start=True, stop=True)
                    P.I('dve', 'scalar_tensor_tensor', reads=[S32[m], EA, b_], writes=[S32[m]], out=S32[m][:], in0=S32[m][:],
                        scalar=EA[:, m, 127:128], in1=b_[:], op0=ALU.mult, op1=ALU.add)
                    P.I('act', 'activation', reads=[S32[m]], writes=[Sbf[m]], out=Sbf[m][:], in_=S32[m][:], func=AF.Copy)
    if send_d is not None:
        for m in range(8):
            P.dma('sp', send_d[m * 128:(m + 1) * 128, :], S32[m][:], reads=[S32[m]], writes=[send_d])


def gla_b(P, cx, l, x_d, xo_d, modv_d, gnorm_d, wout_d, oloc_d, gs_d, qc_d=None, sprev_d=None, nt=NT):
    bk = cx.bank
    off = l * 12288
    gate = P.sbuf([128, D], F32, name="gate")
    bcast_row(P, gate, gate[:], modv_d, modv_d[0:1, off + 2 * D:off + 3 * D])
    gn = P.sbuf([128, 512], F32, name="gn")
    bcast_row(P, gn, gn[:], gnorm_d, gnorm_d[l:l + 1, :])
    wout = P.sbuf([128, 16, D], BF16, name="wout")
    for n in range(4):
        P.dma('pool', wout[:, :, n * 512:(n + 1) * 512], wout_d[l, :, n * 512:(n + 1) * 512].rearrange("(c p) n -> p c n", p=128),
              reads=[wout_d], writes=[wout])
    corr = qc_d is not None
    if corr:
        sp = P.sbuf([128, 8, 512], BF16, name="sprev")
        P.dma('pool', sp[:], sprev_d[:, :].rearrange("(m p) n -> p m n", p=128), reads=[sprev_d], writes=[sp])
        qc = P.sbuf([128, 8, 128], BF16, name="qcb")
    o = P.sbuf([128, D], F32, name="o"); gs = P.sbuf([128, D], F32, name="gs"); xt = P.sbuf([128, D], F32, name="xtb")
    on = P.sbuf([128, D], F32, name="on"); junk = P.sbuf([128, 512], BF16, name="junkb")
    onT = P.sbuf([128, 16, 128], BF16, name="onT")
    ss = P.sbuf([128, 4], F32, name="ssb"); rstd = P.sbuf([128, 4], F32, name="rstdb")
    for g in range(nt):
        rows = slice(g * 128, (g + 1) * 128)
        P.dma('sp', o[:], oloc_d[rows, :], reads=[oloc_d], writes=[o])
        P.dma('act', gs[:], gs_d[rows, :], reads=[gs_d], writes=[gs])
        P.dma('sp', xt[:], x_d[rows, :], reads=[x_d], writes=[xt])
        if corr:
            P.dma('pool', qc[:], qc_d[:, rows].rearrange("(m p) t -> p m t", p=128), reads=[qc_d], writes=[qc])
        for hd in range(4):
            b_ = bk[hd]
            osl = o[:, hd * 512:(hd + 1) * 512]
            if corr:
                for j in range(2):
                    m = 2 * hd + j
                    P.I('pe', 'matmul', reads=[qc, sp], writes=[b_], out=b_[:], lhsT=qc[:, m, :], rhs=sp[:, m, :],
                        start=(j == 0), stop=(j == 1))
                P.I('dve', 'tensor_tensor', reads=[o, b_], writes=[o], out=osl, in0=osl, in1=b_[:], op=ALU.add)
            P.I('act', 'activation', reads=[o], writes=[junk, ss], out=junk[:], in_=osl, func=AF.Square,
                accum_out=ss[:, hd:hd + 1])
        rstd_from_ss(P, ss, rstd, 4, 1.0 / 512)
        for hd in range(4):
            P.I('dve', 'scalar_tensor_tensor', reads=[o, rstd, gn], writes=[on], out=on[:, hd * 512:(hd + 1) * 512],
                in0=o[:, hd * 512:(hd + 1) * 512], scalar=rstd[:, hd:hd + 1], in1=gn[:], op0=ALU.mult, op1=ALU.mult)
        P.I('pool', 'tensor_tensor', reads=[on, gs], writes=[on], out=on[:], in0=on[:], in1=gs[:], op=ALU.mult)
        transpose_T(P, cx, on, onT, 0, [bk[4], bk[5]])
        for n in range(4):
            b_ = bk[n]
            for c in range(16):
                P.I('pe', 'matmul', reads=[onT, wout], writes=[b_], out=b_[:], lhsT=onT[:, c, :], rhs=wout[:, c, n * 512:(n + 1) * 512],
                    start=(c == 0), stop=(c == 15))
            ysl = on[:, n * 512:(n + 1) * 512]
            P.I('dve', 'tensor_tensor', reads=[b_, gate], writes=[on], out=ysl, in0=b_[:], in1=gate[:, n * 512:(n + 1) * 512],
                op=ALU.mult)
        P.I('pool', 'tensor_tensor', reads=[on, xt], writes=[xt], out=xt[:], in0=on[:], in1=xt[:], op=ALU.add)
        P.dma('sp', xo_d[rows, :], xt[:], reads=[xt], writes=[xo_d])


def peer(P, cx, l, x_d, xo_d, modv_d, nffn_d, wq_d, subk_d, uT_d, v_d, nt=NT):
    bk = cx.bank
    off = l * 12288 + 3 * D
    RG = P.sbuf([128, 25600], F32, name="RG")

    def carve(a, b, name, dt=F32):
        ap = RG[:, a:b]
        if dt == BF16:
            ap = ap.bitcast(BF16)
        return Buf(ap, name)
    hT = P.sbuf([128, 16, 512], BF16, name="hTp")
    acc = [P.sbuf([128, D], F32, name="acc%d" % i) for i in range(4)]
    ssb = [P.sbuf([128, 16, 128], F32, name="ssb%d" % i) for i in range(4)]
    tau = [P.sbuf([128, 8], F32, name="tau%d" % i) for i in range(4)]
    negC = [P.sbuf([128, 8], F32, name="negC%d" % i) for i in range(4)]
    xi = [P.sbuf([128, 8], F32, name="xi%d" % i) for i in range(4)]
    halfC = P.sbuf([128, 8], F32, name="halfC")
    XD = P.sbuf([128, 2048], F32, name="XD")
    subkT = P.sbuf([128, 16, 128], F32, name="subkT")
    idb = P.sbuf([128, 128], BF16, name="idb")
    P.I('dve', 'tensor_copy', reads=[cx.cst], writes=[idb], out=idb[:], in_=cx.c(C_ID))
    m16 = P.sbuf([128, 16, 16], F32, name="m16"); top = P.sbuf([128, 8, 16], F32, name="top")
    negm = P.sbuf([128, 8], F32, name="negm"); Z = P.sbuf([128, 8], F32, name="Z"); j16 = P.sbuf([128, 16], F32, name="j16")
    ss = P.sbuf([128, 4], F32, name="ssp"); rstd = P.sbuf([128, 4], F32, name="rstdp")
    stg = acc[0]
    P.dma('sp', stg[:].rearrange("p (a n) -> p a n", a=16), subk_d[l].rearrange("h q n d -> n (h q) d"), reads=[subk_d], writes=[stg])
    for g4 in range(4):
        b_ = bk[g4 % 2]
        for k in range(4):
            hp = g4 * 4 + k
            P.I('pe', 'transpose', reads=[stg, cx.cst], writes=[b_], out=b_[:, k * 128:(k + 1) * 128],
                in_=stg[:, hp * 128:(hp + 1) * 128], identity=cx.c(C_ID))
        P.I('act', 'activation', reads=[b_], writes=[subkT], out=subkT[:, g4 * 4:(g4 + 1) * 4, :],
            in_=b_[:].rearrange("p (k t) -> p k t", k=4), func=AF.Copy)
    P.barrier()
    for ps_ in range(nt // 4):
        xt = carve(0, 2048, "xt"); h = carve(2048, 4096, "h"); A = carve(4096, 6144, "A"); B = carve(6144, 8192, "B")
        qT = carve(8192, 16384, "qT"); cand = carve(16384, 18432, "cand"); candw = carve(18432, 20480, "candw")
        wqb = [carve(20480, 22528, "wq0", BF16), carve(22528, 24576, "wq1", BF16)]
        junk = carve(24576, 25600, "junk", BF16)
        make_AB(P, A, B, nffn_d, nffn_d[l:l + 1, :], modv_d, off, off + D, h)
        for t in range(4):
            g = ps_ * 4 + t
            P.dma('sp', xt[:], x_d[g * 128:(g + 1) * 128, :], reads=[x_d], writes=[xt])
            normmod_T(P, cx, xt, A, B, h, junk, ss, rstd, hT, t * 128, [bk[0], bk[1]])
        qTv = qT[:].rearrange("p (a n) -> p a n", a=16)
        for cb in range(8):
            w = wqb[cb % 2]
            wv = w[:].rearrange("p (c n) -> p c n", c=16)
            P.dma('pool', wv, wq_d[l, :, cb * 256:(cb + 1) * 256].rearrange("(c p) n -> p c n", p=128), reads=[wq_d], writes=[w])
            for m in range(2):
                b_ = bk[2 + (cb * 2 + m) % 2]
                for c in range(16):
                    P.I('pe', 'matmul', reads=[w, hT], writes=[b_], out=b_[:], lhsT=wv[:, c, m * 128:(m + 1) * 128], rhs=hT[:, c, :],
                        start=(c == 0), stop=(c == 15))
                P.I('act', 'activation', reads=[b_], writes=[qT], out=qTv[:, cb * 2 + m, :], in_=b_[:], func=AF.Copy)
        hv = h[:].rearrange("p (a n) -> p a n", a=16)
        cv4 = cand[:].rearrange("p (h a b) -> p h a b", h=8, a=16)
        cv = cand[:].rearrange("p (h n) -> p h n", h=8)
        cwv = candw[:].rearrange("p (h n) -> p h n", h=8)
        for t in range(4):
            s_ = ssb[t]
            for hp in range(16):
                b_ = bk[4 + hp // 4]
                P.I('pe', 'matmul', reads=[qT, subkT], writes=[b_], out=b_[:, (hp % 4) * 128:(hp % 4 + 1) * 128],
                    lhsT=qTv[:, hp, t * 128:(t + 1) * 128], rhs=subkT[:, hp, :], start=True, stop=True)
            for g4 in range(4):
                P.I('act', 'activation', reads=[bk[4 + g4]], writes=[s_], out=s_[:, g4 * 4:(g4 + 1) * 4, :],
                    in_=bk[4 + g4][:].rearrange("p (k t) -> p k t", k=4), func=AF.Copy)
            for hp in range(16):
                P.I('dve', 'max', reads=[s_], writes=[m16], out=m16[:, hp, 0:8], in_=s_[:, hp, :])
                P.I('dve', 'match_replace', reads=[s_, m16], writes=[h], out=hv[:, hp, :], in_to_replace=m16[:, hp, 0:8],
                    in_values=s_[:, hp, :], imm_value=-1e30)
                P.I('dve', 'max', reads=[h], writes=[m16], out=m16[:, hp, 8:16], in_=hv[:, hp, :])
            m16v = m16[:].rearrange("p (h q) k -> p h q k", q=2)
            P.I('dve', 'tensor_tensor', reads=[m16], writes=[cand], out=cv4,
                in0=m16v[:, :, 0, :].unsqueeze(3).to_broadcast([128, 8, 16, 16]),
                in1=m16v[:, :, 1, :].unsqueeze(2).to_broadcast([128, 8, 16, 16]), op=ALU.add)
            for hh in range(8):
                P.I('dve', 'max', reads=[cand], writes=[top], out=top[:, hh, 0:8], in_=cv[:, hh, :])
                P.I('dve', 'match_replace', reads=[cand, top], writes=[candw], out=cwv[:, hh, :], in_to_replace=top[:, hh, 0:8],
                    in_values=cv[:, hh, :], imm_value=-1e30)
                P.I('dve', 'max', reads=[candw], writes=[top], out=top[:, hh, 8:16], in_=cwv[:, hh, :])
            P.I('dve', 'tensor_scalar', reads=[top], writes=[negm], out=negm[:], in0=top[:, :, 0], scalar1=-1.0, scalar2=0.0,
                op0=ALU.mult, op1=ALU.add)
            for hh in range(8):
                P.I('act', 'activation', reads=[top, negm], writes=[j16, Z], out=j16[:], in_=top[:, hh, :], func=AF.Exp,
                    bias=negm[:, hh:hh + 1], scale=1.0, accum_out=Z[:, hh:hh + 1])
            P.I('act', 'activation', reads=[Z], writes=[Z], out=Z[:], in_=Z[:], func=AF.Ln)
            P.I('dve', 'tensor_tensor', reads=[negm, Z], writes=[negC[t]], out=negC[t][:], in0=negm[:], in1=Z[:], op=ALU.subtract)
            P.I('dve', 'tensor_scalar', reads=[top], writes=[tau[t]], out=tau[t][:], in0=top[:, :, 15], scalar1=1.0, scalar2=-1e-5,
                op0=ALU.mult, op1=ALU.add)
            P.I('dve', 'tensor_tensor', reads=[tau[t], negC[t]], writes=[xi[t]], out=xi[t][:], in0=tau[t][:], in1=negC[t][:], op=ALU.add)
            P.I('act', 'activation', reads=[xi[t]], writes=[xi[t]], out=xi[t][:], in_=xi[t][:], func=AF.Exp)
            P.I('dve', 'tensor_scalar', reads=[negC[t]], writes=[halfC], out=halfC[:], in0=negC[t][:], scalar1=0.5, scalar2=0.0,
                op0=ALU.mult, op1=ALU.add)
            for hh in range(8):
                P.I('act', 'activation', reads=[s_, halfC], writes=[s_], out=s_[:, 2 * hh:2 * hh + 2, :], in_=s_[:, 2 * hh:2 * hh + 2, :],
                    func=AF.Exp, bias=halfC[:, hh:hh + 1], scale=1.0)
        P.barrier()
        uTb = [carve(0, 4096, "uT0", BF16), carve(4096, 8192, "uT1", BF16)]
        vb = [carve(8192, 12288, "v0", BF16), carve(12288, 16384, "v1", BF16)]
        gAb = [[carve(16384 + k * 1024 + i * 256, 16384 + k * 1024 + (i + 1) * 256, "gA%d_%d" % (k, i), BF16) for i in range(4)]
               for k in range(2)]
        Xb = [carve(18432 + k * 512, 18432 + (k + 1) * 512, "X%d" % k) for k in range(4)]
        Tb = [[carve(20992 + k * 2048 + hh * 256, 20992 + k * 2048 + (hh + 1) * 256, "T%d_%d" % (k, hh), BF16) for hh in range(8)]
              for k in range(2)]
        GTb = [carve(20480 + k * 64, 20480 + (k + 1) * 64, "GT%d" % k, BF16) for k in range(8)]
        wbb = [[Buf(bk[6 + k][:, i * 128:(i + 1) * 128], "wb%d_%d" % (k, i)) for i in range(4)] for k in range(2)]
        accb = [[Buf(acc[t][:, n * 512:(n + 1) * 512], "acc%d_%d" % (t, n)) for n in range(4)] for t in range(4)]
        cnt = {'x': 0, 'gt': 0}
        units = [(g, t) for g in range(32) for t in range(4)]
        NU = len(units)
        HA = 4

        def load_group(g):
            uT = uTb[g % 2]; v = vb[g % 2]
            uTv = uT[:].rearrange("p (c e) -> p c e", c=16)
            vv = v[:].rearrange("p (i n) -> p i n", i=4)
            P.dma('pool', uTv, uT_d[l, :, g * 512:(g + 1) * 512].rearrange("(c p) e -> p c e", p=128), reads=[uT_d], writes=[uT])
            P.dma('pool', vv, v_d[l, g * 512:(g + 1) * 512, :].rearrange("(i j) n -> j i n", j=128), reads=[v_d], writes=[v])

        def group_A(g):
            uT = uTb[g % 2]
            uTv = uT[:].rearrange("p (c e) -> p c e", c=16)
            for i in range(4):
                b_ = bk[4 + i % 2]
                gA = gAb[g % 2][i]
                for c in range(16):
                    P.I('pe', 'matmul', reads=[uT, hT], writes=[b_], out=b_[:], lhsT=uTv[:, c, i * 128:(i + 1) * 128], rhs=hT[:, c, :],
                        start=(c == 0), stop=(c == 15))
                P.I('act', 'activation', reads=[b_], writes=[gA], out=gA[:], in_=b_[:], func=AF.Gelu_apprx_tanh)

        def stage_A_act(u):
            g, t = units[u]
            s_ = ssb[t]
            for hh in range(HA):
                X = Xb[hh]
                Xv = X[:].rearrange("p (i j) -> p i j", i=4)
                for i in range(4):
                    P.I('act', 'activation', reads=[s_], writes=[X], out=Xv[:, i, :], in_=s_[:, 2 * hh + 1, :], func=AF.Copy,
                        scale=s_[:, 2 * hh, g * 4 + i:g * 4 + i + 1])

        def stage_A_dve_indep(u):
            g, t = units[u]
            s_ = ssb[t]
            sv = s_[:].rearrange("p (h q) n -> p h q n", q=2)
            nd = 8 - HA
            P.I('dve', 'tensor_tensor', reads=[s_], writes=[XD], out=XD[:].rearrange("p (h i j) -> p h i j", h=nd, i=4),
                in0=sv[:, HA:8, 0, g * 4:(g + 1) * 4].unsqueeze(3).to_broadcast([128, nd, 4, 128]),
                in1=sv[:, HA:8, 1, :].unsqueeze(2).to_broadcast([128, nd, 4, 128]), op=ALU.mult)
            for k in range(nd):
                hh = HA + k
                T = Tb[u % 2][hh]
                P.I('dve', 'scalar_tensor_tensor', reads=[XD, xi[t]], writes=[T], out=T[:], in0=XD[:, k * 512:(k + 1) * 512],
                    scalar=xi[t][:, hh:hh + 1], in1=XD[:, k * 512:(k + 1) * 512], op0=ALU.is_ge, op1=ALU.mult)

        def stage_A_dve_dep(u):
            g, t = units[u]
            for hh in range(HA):
                X = Xb[hh]
                T = Tb[u % 2][hh]
                P.I('dve', 'scalar_tensor_tensor', reads=[X, xi[t]], writes=[T], out=T[:], in0=X[:],
                    scalar=xi[t][:, hh:hh + 1], in1=X[:], op0=ALU.is_ge, op1=ALU.mult)

        def stage_B1(u):
            for i in range(4):
                wb = wbb[u % 2][i]
                for hh in range(8):
                    T = Tb[u % 2][hh]
                    P.I('pe', 'matmul', reads=[T, idb], writes=[wb], out=wb[:], lhsT=T[:, i * 128:(i + 1) * 128],
                        rhs=idb[:], start=(hh == 0), stop=(hh == 7))

        def stage_B2(u):
            g, t = units[u]
            v = vb[g % 2]
            vv = v[:].rearrange("p (i n) -> p i n", i=4)
            gts = []
            for i in range(4):
                GT = GTb[cnt['gt'] % 8]; cnt['gt'] += 1
                gts.append(GT)
                P.I('dve', 'tensor_tensor', reads=[wbb[u % 2][i], gAb[g % 2][i]], writes=[GT], out=GT[:], in0=wbb[u % 2][i][:],
                    in1=gAb[g % 2][i][:, t * 128:(t + 1) * 128], op=ALU.mult)
            for i in range(4):
                for n in range(4):
                    P.I('pe', 'matmul', reads=[gts[i], v], writes=[bk[n]], out=bk[n][:], lhsT=gts[i][:], rhs=vv[:, i, n * 512:(n + 1) * 512],
                        start=(i == 0), stop=(i == 3))

        def stage_B3(u):
            g, t = units[u]
            for n in range(4):
                a_ = accb[t][n]
                if g == 0:
                    P.I('dve', 'tensor_copy', reads=[bk[n]], writes=[a_], out=a_[:], in_=bk[n][:])
                else:
                    P.I('dve', 'tensor_tensor', reads=[bk[n], a_], writes=[a_], out=a_[:], in0=a_[:], in1=bk[n][:], op=ALU.add)

        load_group(0)
        group_A(0)
        for u0 in (0, 1):
            stage_A_act(u0)
            stage_A_dve_indep(u0)
            stage_A_dve_dep(u0)
        stage_B1(0)
        for k in range(NU):
            g, t = units[k]
            if t == 0 and g + 1 < 32:
                load_group(g + 1)
            if k + 2 < NU:
                stage_A_act(k + 2)
            if t == 1 and g + 1 < 32:
                group_A(g + 1)
            if k + 1 < NU:
                stage_B1(k + 1)
            if k + 2 < NU:
                stage_A_dve_indep(k + 2)
            if k >= 1:
                stage_B3(k - 1)
            stage_B2(k)
            if k + 2 < NU:
                stage_A_dve_dep(k + 2)
        stage_B3(NU - 1)
        P.barrier()
        gate = carve(0, 2048, "gate"); xt5 = carve(2048, 4096, "xt5")
        bcast_row(P, gate, gate[:], modv_d, modv_d[0:1, off + 2 * D:off + 3 * D])
        for t in range(4):
            g = ps_ * 4 + t
            P.dma('sp', xt5[:], x_d[g * 128:(g + 1) * 128, :], reads=[x_d], writes=[xt5])
            P.I('dve', 'tensor_tensor', reads=[acc[t], gate], writes=[acc[t]], out=acc[t][:], in0=acc[t][:], in1=gate[:], op=ALU.mult)
            P.I('pool', 'tensor_tensor', reads=[acc[t], xt5], writes=[xt5], out=xt5[:], in0=acc[t][:], in1=xt5[:], op=ALU.add)
            P.dma('sp', xo_d[g * 128:(g + 1) * 128, :], xt5[:], reads=[xt5], writes=[xo_d])
        P.barrier()


def phase_kv(P, cx, x_d, modv_d, kvn, wkv, bf_d, KT_d, V_d, lf_d, nt):
    bk = cx.bank
    A = P.sbuf([128, D], F32, name="A"); B = P.sbuf([128, D], F32, name="B")
    xt = P.sbuf([128, D], F32, name="xt"); h = P.sbuf([128, D], F32, name="h"); junk = P.sbuf([128, D], BF16, name="junk")
    make_AB(P, A, B, kvn, kvn[0:1, :], modv_d, 49152, 51200, h)
    hT = P.sbuf([128, 16, 512], BF16, name="hT")
    ss = P.sbuf([128, 4], F32, name="ss"); rstd = P.sbuf([128, 4], F32, name="rstd")
    wblk = [P.sbuf([128, 16, 512], BF16, name="wb%d" % i) for i in range(2)]
    wfl = P.sbuf([128, 16, 16], BF16, name="wfl")
    P.dma('pool', wfl[:], wkv[:, 4096:4112].rearrange("(c p) n -> p c n", p=128), reads=[wkv], writes=[wfl])
    bfb = P.sbuf([128, 16], F32, name="bfb")
    bcast_row(P, bfb, bfb[:], bf_d, bf_d[0:1, :])
    ev = [P.sbuf([128, 512], F32, name="ev%d" % i) for i in range(2)]
    z16 = P.sbuf([128, 16], F32, name="z16")
    ne = 0
    for ps_ in range(nt // 4):
        cols = slice(ps_ * 512, (ps_ + 1) * 512)
        for t in range(4):
            g = ps_ * 4 + t
            P.dma('sp', xt[:], x_d[g * 128:(g + 1) * 128, :], reads=[x_d], writes=[xt])
            normmod_T(P, cx, xt, A, B, h, junk, ss, rstd, hT, t * 128, [bk[0], bk[1]])
        for cb in range(8):
            w = wblk[cb % 2]
            P.dma('pool', w[:], wkv[:, cb * 512:(cb + 1) * 512].rearrange("(c p) n -> p c n", p=128), reads=[wkv], writes=[w])
            if cb < 4:
                for m in range(4):
                    b_ = bk[2 + m % 2]
                    for c in range(16):
                        P.I('pe', 'matmul', reads=[w, hT], writes=[b_], out=b_[:], lhsT=w[:, c, m * 128:(m + 1) * 128], rhs=hT[:, c, :],
                            start=(c == 0), stop=(c == 15))
                    e_ = ev[ne % 2]; ne += 1
                    P.I('act', 'activation', reads=[b_], writes=[e_], out=e_[:], in_=b_[:], func=AF.Copy)
                    P.dma('sp', KT_d[(cb * 4 + m) * 128:(cb * 4 + m + 1) * 128, cols], e_[:], reads=[e_], writes=[KT_d])
            else:
                for t in range(4):
                    g = ps_ * 4 + t
                    b_ = bk[4 + t % 2]
                    for c in range(16):
                        P.I('pe', 'matmul', reads=[w, hT], writes=[b_], out=b_[:], lhsT=hT[:, c, t * 128:(t + 1) * 128], rhs=w[:, c, :],
                            start=(c == 0), stop=(c == 15))
                    e_ = ev[ne % 2]; ne += 1
                    P.I('dve', 'tensor_copy', reads=[b_], writes=[e_], out=e_[:], in_=b_[:])
                    P.dma('sp', V_d[g * 128:(g + 1) * 128, (cb - 4) * 512:(cb - 3) * 512], e_[:], reads=[e_], writes=[V_d])
        for t in range(4):
            g = ps_ * 4 + t
            for c in range(16):
                P.I('pe', 'matmul', reads=[wfl, hT], writes=[bk[6]], out=bk[6][:, 0:16], lhsT=hT[:, c, t * 128:(t + 1) * 128], rhs=wfl[:, c, :],
                    start=(c == 0), stop=(c == 15))
            P.I('dve', 'tensor_tensor', reads=[bk[6], bfb], writes=[z16], out=z16[:], in0=bk[6][:, 0:16], in1=bfb[:], op=ALU.add)
            P.I('act', 'activation', reads=[z16], writes=[z16], out=z16[:], in_=z16[:], func=AF.Exp, scale=-1.0)
            P.I('act', 'activation', reads=[z16], writes=[z16], out=z16[:], in_=z16[:], func=AF.Ln, bias=1.0)
            P.I('dve', 'tensor_scalar', reads=[z16], writes=[z16], out=z16[:], in0=z16[:], scalar1=-1.0, scalar2=0.0, op0=ALU.mult, op1=ALU.add)
            P.dma('sp', lf_d[g * 128:(g + 1) * 128, :], z16[:], reads=[z16], writes=[lf_d])


def phase_fox_a(P, cx, l, x_d, modv_d, nmix, wq_d, KT_d, V_d, lf_d, o_d, sg_d):
    bk = cx.bank
    bq = l - 2
    off = l * 12288
    A = P.sbuf([128, D], F32, name="A"); B = P.sbuf([128, D], F32, name="B")
    xt = P.sbuf([128, D], F32, name="xt"); h = P.sbuf([128, D], F32, name="h"); junk = P.sbuf([128, D], BF16, name="junk")
    make_AB(P, A, B, nmix, nmix[l:l + 1, :], modv_d, off, off + D, h)
    hT = P.sbuf([128, 16, 512], BF16, name="hT")
    ss = P.sbuf([128, 4], F32, name="ss"); rstd = P.sbuf([128, 4], F32, name="rstd")
    wblk = [P.sbuf([128, 16, 512], BF16, name="wb%d" % i) for i in range(2)]
    qTall = P.sbuf([128, 16, TOK], BF16, name="qTall")
    ev = [P.sbuf([128, 512], F32, name="ev0")] * 2
    lf = P.sbuf([128, 512], F32, name="lf"); Fw = P.sbuf([128, 512], F32, name="Fw"); Tot = P.sbuf([128, 512], F32, name="Tot")
    Pre = P.sbuf([128, 512], F32, name="Pre")
    P.dma('sp', lf[:].rearrange("s (b h) -> s b h", h=16), lf_d[:, :].rearrange("(b s) h -> s b h", s=128), reads=[lf_d], writes=[lf])
    P.I('pe', 'matmul', reads=[lf, cx.cst], writes=[bk[6]], out=bk[6][:], lhsT=cx.c(C_M1), rhs=lf[:], start=True, stop=True)
    P.I('pe', 'matmul', reads=[lf, cx.cst], writes=[bk[7]], out=bk[7][:], lhsT=cx.c(C_ONES), rhs=lf[:], start=True, stop=True)
    P.I('dve', 'tensor_copy', reads=[bk[7]], writes=[Tot], out=Tot[:], in_=bk[7][:])
    P.I('pool', 'memset', writes=[Pre], ap=Pre[:], constant=0.0)
    for b_i in range(1, 32):
        P.I('dve', 'tensor_tensor', reads=[Pre, Tot], writes=[Pre], out=Pre[:, b_i * 16:(b_i + 1) * 16], in0=Pre[:, (b_i - 1) * 16:b_i * 16],
            in1=Tot[:, (b_i - 1) * 16:b_i * 16], op=ALU.add)
    P.I('dve', 'tensor_tensor', reads=[bk[6], Pre], writes=[Fw], out=Fw[:], in0=bk[6][:], in1=Pre[:], op=ALU.add)
    Fv = Fw[:].rearrange("s (b h) -> s b h", h=16)
    Pv = Pre[:].rearrange("s (b h) -> s b h", h=16)
    ne = 0
    for ps_ in range(4):
        cols = slice(ps_ * 512, (ps_ + 1) * 512)
        for t in range(4):
            g = ps_ * 4 + t
            P.dma('sp', xt[:], x_d[g * 128:(g + 1) * 128, :], reads=[x_d], writes=[xt])
            normmod_T(P, cx, xt, A, B, h, junk, ss, rstd, hT, t * 128, [bk[0], bk[1]])
        for cb in range(8):
            w = wblk[cb % 2]
            P.dma('pool', w[:], wq_d[bq, :, cb * 512:(cb + 1) * 512].rearrange("(c p) n -> p c n", p=128), reads=[wq_d], writes=[w])
            if cb < 4:
                for m in range(4):
                    b_ = bk[2 + m % 2]
                    for c in range(16):
                        P.I('pe', 'matmul', reads=[w, hT], writes=[b_], out=b_[:], lhsT=w[:, c, m * 128:(m + 1) * 128], rhs=hT[:, c, :],
                            start=(c == 0), stop=(c == 15))
                    P.I('act', 'activation', reads=[b_], writes=[qTall], out=qTall[:, cb * 4 + m, cols], in_=b_[:], func=AF.Copy,
                        scale=float(128 ** -0.5))
            else:
                for t in range(4):
                    g = ps_ * 4 + t
                    b_ = bk[4 + t % 2]
                    for c in range(16):
                        P.I('pe', 'matmul', reads=[w, hT], writes=[b_], out=b_[:], lhsT=hT[:, c, t * 128:(t + 1) * 128], rhs=w[:, c, :],
                            start=(c == 0), stop=(c == 15))
                    e_ = ev[ne % 2]; ne += 1
                    P.I('act', 'activation', reads=[b_], writes=[e_], out=e_[:], in_=b_[:], func=AF.Sigmoid)
                    P.dma('sp', sg_d[g * 128:(g + 1) * 128, (cb - 4) * 512:(cb - 3) * 512], e_[:], reads=[e_], writes=[sg_d])
    KTh = [P.sbuf([128, 4096], BF16, name="KTh%d" % i) for i in range(2)]
    Vh = [P.sbuf([128, 32, 129], BF16, name="Vh%d" % i) for i in range(2)]
    for i in range(2):
        P.I('pool', 'memset', writes=[Vh[i]], ap=Vh[i][:], constant=1.0)
    mA = P.sbuf([128, 128], BF16, name="mA"); mB = P.sbuf([128, 128], BF16, name="mB")
    P.I('dve', 'tensor_copy', reads=[cx.cst], writes=[mA], out=mA[:], in_=cx.c(C_MA))
    P.I('dve', 'tensor_copy', reads=[cx.cst], writes=[mB], out=mB[:], in_=cx.c(C_MB))
    bias = [P.sbuf([128, 32], F32, name="bias%d" % i) for i in range(2)]
    PTb = [P.sbuf([128, 128], BF16, name="PT%d" % i) for i in range(4)]
    oh = [P.sbuf([128, 16, 128], F32, name="oh0")] * 2
    rZ = P.sbuf([128, 2], F32, name="rZ")
    npt = 0; nbi = 0; nst = 0
    for hh in range(16):
        Kt = KTh[hh % 2]; Vt = Vh[hh % 2]; o_h = oh[hh % 2]
        P.dma('pool', Kt[:], KT_d[hh * 128:(hh + 1) * 128, :], reads=[KT_d], writes=[Kt])
        P.dma('pool', Vt[:, :, 0:128], V_d[:, hh * 128:(hh + 1) * 128].rearrange("(b s) d -> s b d", s=128), reads=[V_d], writes=[Vt])
        for m in range(16):
            nblk = 2 * m + 2
            bi = bias[nbi % 2]; nbi += 1
            P.I('dve', 'scalar_tensor_tensor', reads=[Fw, Pre], writes=[bi], out=bi[:, 0:nblk], in0=Fv[:, 0:nblk, hh], scalar=-1.0,
                in1=Pv[:, 2 * m + 1, hh:hh + 1].to_broadcast([128, nblk]), op0=ALU.mult, op1=ALU.add)
            ob = bk[4 + (hh * 16 + m) % 2]
            for j in range(nblk):
                sb_ = bk[nst % 4]; nst += 1
                P.I('pe', 'matmul', reads=[Kt, qTall], writes=[sb_], out=sb_[:, 0:128], lhsT=Kt[:, j * 128:(j + 1) * 128],
                    rhs=qTall[:, hh, m * 128:(m + 1) * 128], start=True, stop=True)
                pt = PTb[npt % 4]; npt += 1
                P.I('act', 'activation', reads=[sb_, bi], writes=[pt], out=pt[:], in_=sb_[:, 0:128], func=AF.Exp, bias=bi[:, j:j + 1], scale=1.0)
                if j == nblk - 2:
                    P.I('dve', 'tensor_tensor', reads=[pt, mA], writes=[pt], out=pt[:], in0=pt[:], in1=mA[:], op=ALU.mult)
                if j == nblk - 1:
                    P.I('dve', 'tensor_tensor', reads=[pt, mB], writes=[pt], out=pt[:], in0=pt[:], in1=mB[:], op=ALU.mult)
                P.I('pe', 'matmul', reads=[pt, Vt], writes=[ob], out=ob[:, 0:129], lhsT=pt[:], rhs=Vt[:, j, :], start=(j == 0), stop=(j == nblk - 1))
            P.I('dve', 'reciprocal', reads=[ob], writes=[rZ], out=rZ[:, 0:1], in_=ob[:, 128:129])
            P.I('dve', 'tensor_scalar', reads=[ob, rZ], writes=[o_h], out=o_h[:, m, :], in0=ob[:, 0:128], scalar1=rZ[:, 0:1], scalar2=0.0,
                op0=ALU.mult, op1=ALU.add)
        P.dma('sp', o_d[:, hh * 128:(hh + 1) * 128].rearrange("(m t) d -> t m d", t=128), o_h[:], reads=[o_h], writes=[o_d])


def phase_fox_b(P, cx, l, x_d, o_d, sg_d, wout_d, xo_d, modv_d):
    bk = cx.bank
    off = l * 12288
    gate = P.sbuf([128, D], F32, name="gate")
    bcast_row(P, gate, gate[:], modv_d, modv_d[0:1, off + 2 * D:off + 3 * D])
    wout = P.sbuf([128, 16, D], BF16, name="wout")
    for n in range(4):
        P.dma('pool', wout[:, :, n * 512:(n + 1) * 512], wout_d[l - 2, :, n * 512:(n + 1) * 512].rearrange("(c p) n -> p c n", p=128),
              reads=[wout_d], writes=[wout])
    o = P.sbuf([128, D], F32, name="o"); sg = P.sbuf([128, D], F32, name="sg"); xt = P.sbuf([128, D], F32, name="xt")
    onT = P.sbuf([128, 16, 128], BF16, name="onT")
    for g in range(NT):
        rows = slice(g * 128, (g + 1) * 128)
        P.dma('sp', o[:], o_d[rows, :], reads=[o_d], writes=[o])
        P.dma('act', sg[:], sg_d[rows, :], reads=[sg_d], writes=[sg])
        P.dma('sp', xt[:], x_d[rows, :], reads=[x_d], writes=[xt])
        P.I('pool', 'tensor_tensor', reads=[o, sg], writes=[o], out=o[:], in0=o[:], in1=sg[:], op=ALU.mult)
        transpose_T(P, cx, o, onT, 0, [bk[4], bk[5]])
        for n in range(4):
            b_ = bk[n]
            for c in range(16):
                P.I('pe', 'matmul', reads=[onT, wout], writes=[b_], out=b_[:], lhsT=onT[:, c, :], rhs=wout[:, c, n * 512:(n + 1) * 512],
                    start=(c == 0), stop=(c == 15))
            P.I('dve', 'tensor_tensor', reads=[b_, gate], writes=[sg], out=sg[:, n * 512:(n + 1) * 512], in0=b_[:],
                in1=gate[:, n * 512:(n + 1) * 512], op=ALU.mult)
        P.I('pool', 'tensor_tensor', reads=[sg, xt], writes=[xt], out=xt[:], in0=sg[:], in1=xt[:], op=ALU.add)
        P.dma('sp', xo_d[rows, :], xt[:], reads=[xt], writes=[xo_d])


def phase_final(P, x_d, fn_d, o_d):
    A = P.sbuf([128, D], F32, name="A")
    bcast_row(P, A, A[:], fn_d, fn_d[0:1, :])
    xt = [P.sbuf([128, D], F32, name="xt%d" % i) for i in range(2)]
    h = [P.sbuf([128, D], F32, name="h%d" % i) for i in range(2)]
    junk = P.sbuf([128, D], BF16, name="junk")
    ss = P.sbuf([128, 4], F32, name="ss"); rstd = P.sbuf([128, 4], F32, name="rstd")
    for g in range(NT):
        x_, h_ = xt[g % 2], h[g % 2]
        P.dma('sp', x_[:], x_d[g * 128:(g + 1) * 128, :], reads=[x_d], writes=[x_])
        P.I('act', 'activation', reads=[x_], writes=[junk, ss], out=junk[:], in_=x_[:], func=AF.Square, accum_out=ss[:, 0:1])
        rstd_from_ss(P, ss, rstd, 1, 1.0 / D)
        P.I('dve', 'scalar_tensor_tensor', reads=[x_, rstd, A], writes=[h_], out=h_[:], in0=x_[:], scalar=rstd[:, 0:1], in1=A[:],
            op0=ALU.mult, op1=ALU.mult)
        P.dma('sp', o_d[g * 128:(g + 1) * 128, :], h_[:], reads=[h_], writes=[o_d])


def phase_xsel(P, cx, xfull_d, xB_d):
    omp = P.sbuf([128, 1], F32, name="omp")
    P.I('dve', 'tensor_scalar', reads=[cx.cst], writes=[omp], out=omp[:], in0=cx.cst[:, C_P:C_P + 1], scalar1=-1.0, scalar2=1.0,
        op0=ALU.mult, op1=ALU.add)
    xe = [P.sbuf([128, D], F32, name="xe%d" % i) for i in range(2)]
    xo = [P.sbuf([128, D], F32, name="xo%d" % i) for i in range(2)]
    for m in range(16):
        a, b_ = xe[m % 2], xo[m % 2]
        P.dma('sp', a[:], xfull_d[(2 * m) * 128:(2 * m + 1) * 128, :], reads=[xfull_d], writes=[a])
        P.dma('act', b_[:], xfull_d[(2 * m + 1) * 128:(2 * m + 2) * 128, :], reads=[xfull_d], writes=[b_])
        P.I('dve', 'tensor_scalar', reads=[a, omp], writes=[a], out=a[:], in0=a[:], scalar1=omp[:, 0:1], scalar2=0.0,
            op0=ALU.mult, op1=ALU.add)
        P.I('dve', 'scalar_tensor_tensor', reads=[b_, a, cx.cst], writes=[a], out=a[:], in0=b_[:], scalar=cx.cst[:, C_P:C_P + 1],
            in1=a[:], op0=ALU.mult, op1=ALU.add)
        P.dma('sp', xB_d[m * 128:(m + 1) * 128, :], a[:], reads=[a], writes=[xB_d])


def build_fused():
    nc = bass.Bass("TRN2", target_bir_lowering=False)
    P = Prog(nc)
    EI = "ExternalInput"
    S = 4096
    cst = P.dram("cst", [128, NCST], F32, kind=EI)
    x_in = P.dram("x", [S, D], F32, kind=EI)
    c_d = P.dram("c", [1, D], F32, kind=EI)
    Wm = P.dram("Wm", [D, MODN], F32, kind=EI)
    Bm = P.dram("Bm", [1, MODN], F32, kind=EI)
    nmix = P.dram("norm_mix", [4, D], F32, kind=EI)
    nffn = P.dram("norm_ffn", [4, D], F32, kind=EI)
    win = P.dram("gla_w_in", [2, D, 6160], F32, kind=EI)
    wg2 = P.dram("gla_w_gate2", [2, 16, 1024], F32, kind=EI)
    bg2 = P.dram("gla_b_gate2", [2, 1024], F32, kind=EI)
    gnorm = P.dram("gla_norm", [2, 512], F32, kind=EI)
    gwout = P.dram("gla_w_out", [2, D, D], F32, kind=EI)
    kvn = P.dram("kv_norm", [1, D], F32, kind=EI)
    wkv = P.dram("fox_w_kv", [D, 4112], F32, kind=EI)
    bf_d = P.dram("fox_b_f", [1, 16], F32, kind=EI)
    fwq = P.dram("fox_w_q", [2, D, 2 * D], F32, kind=EI)
    fwo = P.dram("fox_w_out", [2, D, D], F32, kind=EI)
    pwq = P.dram("peer_w_q", [4, D, D], F32, kind=EI)
    subk = P.dram("peer_subkeys", [4, 8, 2, 128, 128], F32, kind=EI)
    uT = P.dram("uT", [4, D, 16384], F32, kind=EI)
    pv = P.dram("pv", [4, 16384, D], F32, kind=EI)
    fn_d = P.dram("final_norm", [1, D], F32, kind=EI)
    out_d = P.dram("out", [TOK, D], F32, kind="ExternalOutput")
    modv = P.dram("modv", [1, MODN], F32)
    XA = P.dram("XA", [S, D], F32); XB = P.dram("XB", [S, D], F32)
    oloc = P.dram("oloc", [S, D], F32); gs = P.dram("gs", [S, D], F32)
    KT = P.dram("KT", [D, S], F32); V = P.dram("V", [S, D], F32); lf = P.dram("lf", [S, 16], F32)
    xs0 = P.dram("xs0", [TOK, D], F32); xs1 = P.dram("xs1", [TOK, D], F32)
    o_s = P.dram("o_s", [TOK, D], F32); sg_s = P.dram("sg_s", [TOK, D], F32)
    cx = Ctx(P, cst)
    P.cx = cx
    with P.phase():
        phase_mod(P, c_d, Wm, Bm, modv)
    xin = x_in
    for l in range(2):
        with P.phase():
            gla_a(P, cx, l, xin, modv, nmix, win, wg2, bg2, oloc, gs, nt=32)
        with P.phase():
            gla_b(P, cx, l, xin, XA, modv, gnorm, gwout, oloc, gs, nt=32)
        with P.phase():
            peer(P, cx, l, XA, XB, modv, nffn, pwq, subk, uT, pv, nt=32)
        xin = XB
    with P.phase():
        phase_kv(P, cx, XB, modv, kvn, wkv, bf_d, KT, V, lf, 32)
    with P.phase():
        phase_xsel(P, cx, XB, xs0)
    for l in (2, 3):
        with P.phase():
            phase_fox_a(P, cx, l, xs0, modv, nmix, fwq, KT, V, lf, o_s, sg_s)
        with P.phase():
            phase_fox_b(P, cx, l, xs0, o_s, sg_s, fwo, xs1, modv)
        with P.phase():
            peer(P, cx, l, xs1, xs0, modv, nffn, pwq, subk, uT, pv, nt=16)
    with P.phase():
        phase_final(P, xs0, fn_d, out_d)
    P.finish()
    return nc


def kernel(x, c, ada_w, ada_b, norm_mix, norm_ffn, gla_w_in, gla_w_gate2, gla_b_gate2, gla_norm, gla_w_out, kv_norm,
           kv_ada_w, kv_ada_b, fox_w_kv, fox_b_f, fox_w_q, fox_w_out, peer_w_q, peer_subkeys, peer_u, peer_v, final_norm):
    f = lambda a: np.ascontiguousarray(np.asarray(a, dtype=np.float32))
    x = f(x); c = f(c)
    Wm = np.ascontiguousarray(np.concatenate([f(ada_w[l]) for l in range(4)] + [f(kv_ada_w)], axis=1))
    Bm = np.ascontiguousarray(np.concatenate([f(ada_b[l]) for l in range(4)] + [f(kv_ada_b)])[None, :])
    uT = np.ascontiguousarray(np.transpose(f(peer_u), (0, 2, 1)))
    shared = dict(Wm=Wm, Bm=Bm, norm_mix=f(norm_mix), norm_ffn=f(norm_ffn), gla_w_in=f(gla_w_in), gla_w_gate2=f(gla_w_gate2),
                  gla_b_gate2=f(gla_b_gate2), gla_norm=f(gla_norm), gla_w_out=f(gla_w_out), kv_norm=f(kv_norm)[None, :],
                  fox_w_kv=f(fox_w_kv), fox_b_f=f(fox_b_f)[None, :], fox_w_q=f(fox_w_q), fox_w_out=f(fox_w_out),
                  peer_w_q=f(peer_w_q), peer_subkeys=f(peer_subkeys), uT=uT, pv=f(peer_v), final_norm=f(final_norm)[None, :])
    ins = [dict(shared, cst=make_consts(r % 2), x=x[r // 2], c=c[r // 2:r // 2 + 1]) for r in range(8)]
    res = run_bass_kernel_spmd(build_fused(), ins, core_ids=list(range(8))).results
    out = np.empty((4, 32, 128, D), np.float32)
    for r in range(8):
        out[r // 2, (r % 2)::2] = res[r]["out"].reshape(16, 128, D)
    return out.reshape(4, 4096, D)
```

```python
import contextlib
import numpy as np
import concourse.bass as bass
import concourse.mybir as mybir

F32 = mybir.dt.float32
BF16 = mybir.dt.bfloat16
AF = mybir.ActivationFunctionType
ALU = mybir.AluOpType
AX = mybir.AxisListType

ENGS = ['pe', 'act', 'dve', 'pool', 'sp']
DMA_SLOTS = {'sp': 12, 'act': 4, 'pool': 6}


class Buf:
    def __init__(self, ap_full, name=""):
        self.t = ap_full
        self.name = name
        self.last_w = None
        self.readers = []

    def __getitem__(self, k):
        return self.t[k]


class Prog:
    def __init__(self, nc):
        self.nc = nc
        self.es = contextlib.ExitStack()
        self.ops = {e: [] for e in ENGS}
        self.cnt = {e: 0 for e in ENGS}
        self.seen = {e: {f: 0 for f in ENGS} for e in ENGS}
        self.sem = {}
        for e in ENGS:
            self.sem[e] = self.es.enter_context(nc.semaphore("s_" + e))
        self.dsem = {}
        self.dval = {}
        self.dseen = {e: {} for e in ENGS}
        self.drr = {q: 0 for q in DMA_SLOTS}
        for q, n in DMA_SLOTS.items():
            for i in range(n):
                key = (q, i)
                self.dsem[key] = self.es.enter_context(nc.semaphore("d_%s%d" % (q, i)))
                self.dval[key] = 0
        self.n_alloc = 0

    def sbuf(self, shape, dtype=F32, name=None, stack=None):
        self.n_alloc += 1
        name = name or ("sb%d" % self.n_alloc)
        t = (stack or getattr(self, 'cur', None) or self.es).enter_context(self.nc.sbuf_tensor(name + "_%d" % self.n_alloc, list(shape), dtype))
        return Buf(t, name)

    def psum(self, shape, dtype=F32, name=None, stack=None):
        self.n_alloc += 1
        name = name or ("ps%d" % self.n_alloc)
        t = (stack or self.es).enter_context(self.nc.psum_tensor(name + "_%d" % self.n_alloc, list(shape), dtype))
        return Buf(t, name)

    def dram(self, name, shape, dtype=F32, kind="Internal"):
        t = self.nc.dram_tensor(name, list(shape), dtype, kind=kind)
        return Buf(t.ap(), name)

    def _collect(self, eng, reads, writes):
        deps = []
        for b in reads:
            if b.last_w is not None:
                deps.append(b.last_w)
        for b in writes:
            if b.last_w is not None:
                deps.append(b.last_w)
            deps.extend(b.readers)
        waits = []
        for d in deps:
            if d[0] == 'eng':
                _, f, k = d
                if f == eng and eng == 'pe':
                    continue
                if self.seen[eng][f] < k:
                    self.seen[eng][f] = k
                    waits.append((self.sem[f], k))
            else:
                _, key, val = d
                if self.dseen[eng].get(key, 0) < val:
                    self.dseen[eng][key] = val
                    waits.append((self.dsem[key], val))
        best = {}
        for s, v in waits:
            if id(s) not in best or best[id(s)][1] < v:
                best[id(s)] = (s, v)
        return list(best.values())

    def _mark(self, tok, reads, writes):
        for b in writes:
            b.last_w = tok
            b.readers = []
        for b in reads:
            if b not in writes:
                b.readers.append(tok)
                if len(b.readers) > 64:
                    b.readers = b.readers[-64:]

    def op(self, eng, fn, reads=(), writes=()):
        waits = self._collect(eng, reads, writes)
        self.cnt[eng] += 1
        k = self.cnt[eng]
        sem = self.sem[eng]

        def run(e, waits=waits, fn=fn, sem=sem):
            for s, v in waits:
                e.wait_ge(s, v)
            fn(e).then_inc(sem, 1)
        self.ops[eng].append(run)
        self._mark(('eng', eng, k), reads, writes)

    def I(self, eng, meth, reads=(), writes=(), **kw):
        self.op(eng, lambda e, meth=meth, kw=kw: getattr(e, meth)(**kw), reads, writes)

    def dma(self, q, out, in_, reads=(), writes=(), **kw):
        waits = self._collect(q, reads, writes)
        n = DMA_SLOTS[q]
        i = self.drr[q]
        self.drr[q] = (i + 1) % n
        key = (q, i)
        prev = self.dval[key]
        if prev > 0 and self.dseen[q].get(key, 0) < prev:
            self.dseen[q][key] = prev
            waits.append((self.dsem[key], prev))
        self.dval[key] = prev + 16
        val = prev + 16
        sem = self.dsem[key]

        def run(e, waits=waits, out=out, in_=in_, sem=sem, kw=kw):
            for s, v in waits:
                e.wait_ge(s, v)
            e.dma_start(out=out, in_=in_, **kw).then_inc(sem, 16)
        self.ops[q].append(run)
        self._mark(('dma', key, val), reads, writes)

    def barrier(self):
        for e in ENGS:
            waits = []
            for f in ENGS:
                if f != e and self.seen[e][f] < self.cnt[f]:
                    self.seen[e][f] = self.cnt[f]
                    waits.append((self.sem[f], self.cnt[f]))
            for key, val in self.dval.items():
                if val > 0 and self.dseen[e].get(key, 0) < val:
                    self.dseen[e][key] = val
                    waits.append((self.dsem[key], val))

            def run(en, waits=waits):
                for s, v in waits:
                    en.wait_ge(s, v)
            self.ops[e].append(run)

    def collective(self, kind, groups, in_buf, in_ap, out_buf, out_ap):
        q = 'pool'
        waits = self._collect(q, [in_buf], [out_buf])
        n = DMA_SLOTS[q]
        i = self.drr[q]
        self.drr[q] = (i + 1) % n
        key = (q, i)
        prev = self.dval[key]
        if prev > 0 and self.dseen[q].get(key, 0) < prev:
            self.dseen[q][key] = prev
            waits.append((self.dsem[key], prev))
        self.dval[key] = prev + 16
        val = prev + 16
        sem = self.dsem[key]

        def run(e, waits=waits, sem=sem):
            for s_, v in waits:
                e.wait_ge(s_, v)
            e.collective_compute(kind, ALU.bypass, replica_groups=groups, ins=[in_ap], outs=[out_ap]).then_inc(sem, 16)
        self.ops[q].append(run)
        self._mark(('dma', key, val), [in_buf], [out_buf])

    @contextlib.contextmanager
    def phase(self):
        st = contextlib.ExitStack()
        self.cur = st
        try:
            yield
            self.flush()
        finally:
            self.cur = None
            st.close()

    def flush(self):
        self.barrier()
        self._emit()
        self.ops = {e: [] for e in ENGS}
        for b in ():
            pass

    def finish(self):
        self.barrier()
        self._emit()
        self.es.close()

    def _emit(self):
        nc = self.nc
        with nc.Block() as block:
            @block.tensor
            def _(e):
                for f in self.ops['pe']:
                    f(e)

            @block.scalar
            def _(e):
                for f in self.ops['act']:
                    f(e)

            @block.vector
            def _(e):
                for f in self.ops['dve']:
                    f(e)

            @block.gpsimd
            def _(e):
                for f in self.ops['pool']:
                    f(e)

            @block.sync
            def _(e):
                for f in self.ops['sp']:
                    f(e)

from concourse.bass_utils import run_bass_kernel_spmd

D = 2048
NT = 16
TOK = 2048
EPS = 1e-6
MODN = 4 * 12288 + 4096

C_ID, C_M1, C_M2, C_LINC, C_USUF, C_ONES, C_MA, C_MB, C_P = 0, 128, 256, 384, 512, 640, 768, 896, 1024
NCST = 1025


def make_consts(p):
    c = np.zeros((128, NCST), np.float32)
    s = np.arange(128)[:, None]
    t = np.arange(128)[None, :]
    c[:, C_ID:C_ID + 128] = (s == t)
    m1 = (s <= t).astype(np.float32)
    c[:, C_M1:C_M1 + 128] = m1
    c[:, C_M2:C_M2 + 128] = ((s > t) & ((s // 64) == (t // 64)))
    c[:, C_LINC:C_LINC + 128] = m1 * (-1.0 / 16.0)
    c[:, C_USUF:C_USUF + 128] = (s > t) * (-1.0 / 16.0)
    c[:, C_ONES:C_ONES + 128] = 1.0
    if p == 0:
        c[:, C_MA:C_MA + 128] = m1
        c[:, C_MB:C_MB + 128] = 0.0
    else:
        c[:, C_MA:C_MA + 128] = 1.0
        c[:, C_MB:C_MB + 128] = m1
    c[:, C_P] = float(p)
    return c


class Ctx:
    def __init__(self, P, cst_d):
        self.P = P
        self.bank = [P.psum([128, 512], F32, name="bank%d" % i) for i in range(8)]
        self.cst = P.sbuf([128, NCST], F32, name="cst")
        P.dma('sp', self.cst[:], cst_d[:, :], reads=[cst_d], writes=[self.cst])
        self.small = {}

    def c(self, off, n=128):
        return self.cst[:, off:off + n]


def bcast_row(P, dst, dst_ap, src_buf, src_ap_row, q='sp'):
    P.dma(q, dst_ap, src_ap_row.partition_broadcast(128), reads=[src_buf], writes=[dst])


def rstd_from_ss(P, ss, rstd, n, inv_n):
    P.I('dve', 'tensor_scalar', reads=[ss], writes=[rstd], out=rstd[:, 0:n], in0=ss[:, 0:n],
        scalar1=inv_n, scalar2=EPS, op0=ALU.mult, op1=ALU.add)
    P.I('act', 'activation', reads=[rstd], writes=[rstd], out=rstd[:, 0:n], in_=rstd[:, 0:n], func=AF.Sqrt)
    P.I('dve', 'reciprocal', reads=[rstd], writes=[rstd], out=rstd[:, 0:n], in_=rstd[:, 0:n])


def normmod_T(P, cx, xt, A, B, h, junk, ss, rstd, hT, tcol, banks):
    P.I('act', 'activation', reads=[xt], writes=[junk, ss], out=junk[:], in_=xt[:], func=AF.Square,
        accum_out=ss[:, 0:1])
    rstd_from_ss(P, ss, rstd, 1, 1.0 / D)
    P.I('dve', 'scalar_tensor_tensor', reads=[xt, rstd, A], writes=[h], out=h[:], in0=xt[:],
        scalar=rstd[:, 0:1], in1=A[:], op0=ALU.mult, op1=ALU.mult)
    if B is not None:
        P.I('pool', 'tensor_tensor', reads=[h, B], writes=[h], out=h[:], in0=h[:], in1=B[:], op=ALU.add)
    transpose_T(P, cx, h, hT, tcol, banks)


def transpose_T(P, cx, h, hT, tcol, banks, nch=16):
    for g in range(nch // 4):
        bk = banks[g % len(banks)]
        for k in range(4):
            c = g * 4 + k
            P.I('pe', 'transpose', reads=[h, cx.cst], writes=[bk], out=bk[:, k * 128:(k + 1) * 128],
                in_=h[:, c * 128:(c + 1) * 128], identity=cx.c(C_ID))
        eng = 'act' if g % 2 == 0 else 'dve'
        src = bk[:].rearrange("p (k t) -> p k t", k=4)
        dst = hT[:, g * 4:(g + 1) * 4, tcol:tcol + 128]
        if eng == 'act':
            P.I('act', 'activation', reads=[bk], writes=[hT], out=dst, in_=src, func=AF.Copy)
        else:
            P.I('dve', 'tensor_copy', reads=[bk], writes=[hT], out=dst, in_=src)


def make_AB(P, A, B, gain_d, gain_row, modv_d, off_shift, off_scale, tmp):
    bcast_row(P, A, A[:], gain_d, gain_row)
    if modv_d is not None:
        bcast_row(P, tmp, tmp[:], modv_d, modv_d[0:1, off_scale:off_scale + D])
        bcast_row(P, B, B[:], modv_d, modv_d[0:1, off_shift:off_shift + D])
        P.I('dve', 'scalar_tensor_tensor', reads=[tmp, A], writes=[A], out=A[:], in0=tmp[:], scalar=1.0,
            in1=A[:], op0=ALU.add, op1=ALU.mult)


def phase_mod(P, c_d, w_d, b_d, o_d):
    ca = P.sbuf([128, 16], F32, name="ca")
    P.dma('sp', ca[:], c_d[0:1, :].rearrange("o (c p) -> p (o c)", p=128), reads=[c_d], writes=[ca],
          allow_slow_non_contiguous=True)
    P.I('act', 'activation', reads=[ca], writes=[ca], out=ca[:], in_=ca[:], func=AF.Silu)
    wt = [P.sbuf([128, 16, 512], F32, name="wt%d" % i) for i in range(2)]
    bt = [P.sbuf([1, 512], F32, name="bt%d" % i) for i in range(2)]
    ot = [P.sbuf([1, 512], F32, name="ot%d" % i) for i in range(2)]
    ps = P.cx.bank
    for n in range(MODN // 512):
        w, b_, o, p_ = wt[n % 2], bt[n % 2], ot[n % 2], ps[n % 2]
        P.dma('sp' if n % 2 == 0 else 'act', w[:], w_d[:, n * 512:(n + 1) * 512].rearrange("(c p) n -> p c n", p=128),
              reads=[w_d], writes=[w])
        P.dma('pool', b_[:], b_d[0:1, n * 512:(n + 1) * 512], reads=[b_d], writes=[b_])
        for c in range(16):
            P.I('pe', 'matmul', reads=[ca, w], writes=[p_], out=p_[0:1, :], lhsT=ca[:, c:c + 1], rhs=w[:, c, :],
                start=(c == 0), stop=(c == 15))
        P.I('dve', 'tensor_tensor', reads=[p_, b_], writes=[o], out=o[:], in0=p_[0:1, :], in1=b_[:], op=ALU.add)
        P.dma('sp', o_d[0:1, n * 512:(n + 1) * 512], o[:], reads=[o], writes=[o_d])


def gla_a(P, cx, l, x_d, modv_d, nmix_d, win_d, wg2_d, bg2_d, oloc_d, gs_d, qc_d=None, send_d=None, nt=NT):
    bk = cx.bank
    A = P.sbuf([128, D], F32, name="A"); B = P.sbuf([128, D], F32, name="B");
    off = l * 12288
    PW = 512
    TP = 4
    hT = P.sbuf([128, 16, PW], BF16, name="hT")
    xt = [P.sbuf([128, D], F32, name="xt0")] * 2
    h = [P.sbuf([128, D], F32, name="h0")] * 2
    make_AB(P, A, B, nmix_d, nmix_d[l:l + 1, :], modv_d, off, off + D, h[0])
    junk = P.sbuf([128, D], BF16, name="junk")
    ss = P.sbuf([128, 4], F32, name="ss"); rstd = P.sbuf([128, 4], F32, name="rstd")
    wblk = [P.sbuf([128, 16, 256], BF16, name="wblk%d" % i) for i in range(2)]
    wgl = P.sbuf([128, 16, 16], BF16, name="wgl")
    P.dma('pool', wgl[:], win_d[l, :, 6144:6160].rearrange("(c p) n -> p c n", p=128), reads=[win_d], writes=[wgl])
    wg2 = P.sbuf([17, 1024], F32, name="wg2")
    P.dma('sp', wg2[0:16, :], wg2_d[l, :, :], reads=[wg2_d], writes=[wg2])
    P.dma('sp', wg2[16:17, :], bg2_d[l:l + 1, :], reads=[bg2_d], writes=[wg2])
    glT = P.sbuf([17, PW], F32, name="glT")
    P.I('pool', 'memset', writes=[glT], ap=glT[:], constant=1.0)
    qT = P.sbuf([128, 8, PW], BF16, name="qT"); kT = P.sbuf([128, 8, PW], BF16, name="kT")
    ktm = [P.sbuf([128, 1024], F32, name="ktm%d" % i) for i in range(TP)]
    vbf = [P.sbuf([128, 2048], BF16, name="vbf%d" % i) for i in range(TP)]
    gst = [P.sbuf([128, 512], F32, name="gst%d" % i) for i in range(2)]
    ez = P.sbuf([128, 1024], F32, name="ez"); la = P.sbuf([128, 1024], F32, name="la")
    EA = P.sbuf([128, 8, 128], F32, name="EA"); EB = P.sbuf([128, 8, 128], F32, name="EB")
    ER = P.sbuf([128, 1024], F32, name="ER")
    QA = P.sbuf([128, 8, 128], BF16, name="QA"); QB = P.sbuf([128, 8, 128], BF16, name="QB")
    KA = P.sbuf([128, 8, 128], BF16, name="KA"); KB = P.sbuf([128, 8, 128], BF16, name="KB")
    KE = P.sbuf([128, 1024], BF16, name="KE")
    QC = P.sbuf([128, 8, 128], F32, name="QC")
    cumP = P.sbuf([128, 8], F32, name="cumP"); expP = P.sbuf([128, 8], F32, name="expP")
    P.I('pool', 'memset', writes=[cumP], ap=cumP[:], constant=0.0)
    t1 = P.sbuf([128, 4, 128], F32, name="t1"); t2 = P.sbuf([128, 4, 128], F32, name="t2")
    PT = P.sbuf([128, 4, 128], BF16, name="PT")
    S32 = [P.sbuf([128, 512], F32, name="S32_%d" % i) for i in range(8)]
    Sbf = [P.sbuf([128, 512], BF16, name="Sbf_%d" % i) for i in range(8)]
    for i in range(8):
        P.I('pool', 'memset', writes=[S32[i]], ap=S32[i][:], constant=0.0)
        P.I('pool', 'memset', writes=[Sbf[i]], ap=Sbf[i][:], constant=0.0)
    ot = [P.sbuf([128, 2048], F32, name="ot0")] * 2
    m1b = cx.c(C_M1).unsqueeze(1).to_broadcast([128, 4, 128])
    m2b = cx.c(C_M2).unsqueeze(1).to_broadcast([128, 4, 128])
    nb = 0
    for ps_ in range(nt // TP):
        for t in range(TP):
            g = ps_ * TP + t
            P.dma('sp', xt[t % 2][:], x_d[g * 128:(g + 1) * 128, :], reads=[x_d], writes=[xt[t % 2]])
            normmod_T(P, cx, xt[t % 2], A, B, h[t % 2], junk, ss, rstd, hT, t * 128, [bk[0], bk[1]])
        for cb in range(24):
            w = wblk[nb % 2]; nb += 1
            P.dma('pool', w[:], win_d[l, :, cb * 256:(cb + 1) * 256].rearrange("(c p) n -> p c n", p=128),
                  reads=[win_d], writes=[w])
            if cb < 8:
                dstT = qT if cb < 4 else kT
                for m in range(2):
                    b_ = bk[(cb * 2 + m) % 2]
                    for c in range(16):
                        P.I('pe', 'matmul', reads=[w, hT], writes=[b_], out=b_[:, 0:PW], lhsT=w[:, c, m * 128:(m + 1) * 128],
                            rhs=hT[:, c, :], start=(c == 0), stop=(c == 15))
                    P.I('act', 'activation', reads=[b_], writes=[dstT], out=dstT[:, (cb % 4) * 2 + m, :], in_=b_[:, 0:PW],
                        func=AF.Copy)
            if cb >= 4:
                for t in range(TP):
                    g = ps_ * TP + t
                    b_ = bk[2 + (t % 2)]
                    for c in range(16):
                        P.I('pe', 'matmul', reads=[w, hT], writes=[b_], out=b_[:, 0:256], lhsT=hT[:, c, t * 128:(t + 1) * 128],
                            rhs=w[:, c, :], start=(c == 0), stop=(c == 15))
                    if cb < 8:
                        P.I('dve', 'tensor_copy', reads=[b_], writes=[ktm[t]], out=ktm[t][:, (cb - 4) * 256:(cb - 3) * 256],
                            in_=b_[:, 0:256])
                    elif cb < 16:
                        P.I('dve', 'tensor_copy', reads=[b_], writes=[vbf[t]], out=vbf[t][:, (cb - 8) * 256:(cb - 7) * 256],
                            in_=b_[:, 0:256])
                    else:
                        go = gst[(cb * TP + t) % 2]
                        P.I('act', 'activation', reads=[b_], writes=[go], out=go[:, 0:256], in_=b_[:, 0:256], func=AF.Silu)
                        P.dma('sp', gs_d[g * 128:(g + 1) * 128, (cb - 16) * 256:(cb - 15) * 256], go[:, 0:256], reads=[go],
                              writes=[gs_d])
        for c in range(16):
            P.I('pe', 'matmul', reads=[wgl, hT], writes=[bk[0]], out=bk[0][0:16, 0:PW], lhsT=wgl[:, c, :], rhs=hT[:, c, :],
                start=(c == 0), stop=(c == 15))
        P.I('act', 'activation', reads=[bk[0]], writes=[glT], out=glT[0:16, :], in_=bk[0][0:16, 0:PW], func=AF.Copy)
        for t in range(TP):
            g = ps_ * TP + t
            for n in range(2):
                P.I('pe', 'matmul', reads=[glT, wg2], writes=[bk[4 + n]], out=bk[4 + n][:], lhsT=glT[:, t * 128:(t + 1) * 128],
                    rhs=wg2[:, n * 512:(n + 1) * 512], start=True, stop=True)
                P.I('act', 'activation', reads=[bk[4 + n]], writes=[ez], out=ez[:, n * 512:(n + 1) * 512], in_=bk[4 + n][:],
                    func=AF.Exp, scale=-1.0)
            P.I('act', 'activation', reads=[ez], writes=[la], out=la[:], in_=ez[:], func=AF.Ln, bias=1.0)
            for m in range(8):
                b_ = bk[4 + m // 4]
                P.I('pe', 'matmul', reads=[la, cx.cst], writes=[b_], out=b_[:, (m % 4) * 128:(m % 4 + 1) * 128],
                    lhsT=la[:, m * 128:(m + 1) * 128], rhs=cx.c(C_LINC), start=True, stop=True)
            for n in range(2):
                P.I('pe', 'matmul', reads=[la, cx.cst], writes=[bk[6 + n]], out=bk[6 + n][:], lhsT=cx.c(C_USUF),
                    rhs=la[:, n * 512:(n + 1) * 512], start=True, stop=True)
            for n in range(2):
                src = bk[4 + n][:].rearrange("p (k t) -> p k t", k=4)
                P.I('act', 'activation', reads=[bk[4 + n]], writes=[EA], out=EA[:, n * 4:(n + 1) * 4, :], in_=src, func=AF.Exp)
                P.I('act', 'activation', reads=[bk[4 + n]], writes=[EB], out=EB[:, n * 4:(n + 1) * 4, :], in_=src, func=AF.Exp,
                    scale=-1.0)
                P.I('act', 'activation', reads=[bk[6 + n]], writes=[ER], out=ER[:, n * 512:(n + 1) * 512], in_=bk[6 + n][:],
                    func=AF.Exp)
            if qc_d is not None:
                P.I('act', 'activation', reads=[cumP], writes=[expP], out=expP[:], in_=cumP[:], func=AF.Exp)
            for n in range(2 if qc_d is not None else 0):
                P.I('dve', 'tensor_tensor', reads=[cumP, bk[4 + n]], writes=[cumP], out=cumP[:, n * 4:(n + 1) * 4],
                    in0=cumP[:, n * 4:(n + 1) * 4],
                    in1=bk[4 + n][:].rearrange("p (k t) -> p k t", k=4)[:, :, 127], op=ALU.add)
            qs = qT[:, :, t * 128:(t + 1) * 128]
            ks = kT[:, :, t * 128:(t + 1) * 128]
            P.I('dve', 'scalar_tensor_tensor', reads=[qT, EA], writes=[QA], out=QA[:], in0=qs, scalar=0.0625, in1=EA[:],
                op0=ALU.mult, op1=ALU.mult)
            P.I('dve', 'scalar_tensor_tensor', reads=[qT, EB], writes=[QB], out=QB[:], in0=qs, scalar=0.0625, in1=EB[:],
                op0=ALU.mult, op1=ALU.mult)
            P.I('pool', 'tensor_tensor', reads=[kT, EA], writes=[KA], out=KA[:], in0=ks, in1=EA[:], op=ALU.mult)
            P.I('pool', 'tensor_tensor', reads=[kT, EB], writes=[KB], out=KB[:], in0=ks, in1=EB[:], op=ALU.mult)
            P.I('pool', 'tensor_tensor', reads=[ktm[t], ER], writes=[KE], out=KE[:], in0=ktm[t][:], in1=ER[:], op=ALU.mult)
            if qc_d is not None:
                P.I('dve', 'scalar_tensor_tensor', reads=[qT, EA, expP], writes=[QC], out=QC[:], in0=qs, scalar=0.0625, in1=EA[:],
                    op0=ALU.mult, op1=ALU.mult)
                P.I('dve', 'tensor_tensor', reads=[QC, expP], writes=[QC], out=QC[:], in0=QC[:],
                    in1=expP[:].unsqueeze(2).to_broadcast([128, 8, 128]), op=ALU.mult)
                P.dma('sp', qc_d[:, g * 128:(g + 1) * 128].rearrange("(m p) t -> p m t", p=128), QC[:], reads=[QC], writes=[qc_d])
            for hd in range(4):
                for j in range(2):
                    m = 2 * hd + j
                    P.I('pe', 'matmul', reads=[KB, QA], writes=[bk[0]], out=bk[0][:, hd * 128:(hd + 1) * 128], lhsT=KB[:, m, :],
                        rhs=QA[:, m, :], start=(j == 0), stop=(j == 1))
                for j in range(2):
                    m = 2 * hd + j
                    P.I('pe', 'matmul', reads=[KA, QB], writes=[bk[1]], out=bk[1][:, hd * 128:(hd + 1) * 128], lhsT=KA[:, m, :],
                        rhs=QB[:, m, :], start=(j == 0), stop=(j == 1))
            P.I('dve', 'tensor_tensor', reads=[bk[0], cx.cst], writes=[t1], out=t1[:],
                in0=bk[0][:].rearrange("p (k t) -> p k t", k=4), in1=m1b, op=ALU.mult)
            P.I('dve', 'tensor_tensor', reads=[bk[1], cx.cst], writes=[t2], out=t2[:],
                in0=bk[1][:].rearrange("p (k t) -> p k t", k=4), in1=m2b, op=ALU.mult)
            P.I('pool', 'tensor_tensor', reads=[t1, t2], writes=[PT], out=PT[:], in0=t1[:], in1=t2[:], op=ALU.add)
            o_ = ot[t % 2]
            for hd in range(4):
                b_ = bk[4 + hd]
                for j in range(2):
                    m = 2 * hd + j
                    P.I('pe', 'matmul', reads=[QA, Sbf[m]], writes=[b_], out=b_[:], lhsT=QA[:, m, :], rhs=Sbf[m][:],
                        start=(j == 0), stop=False)
                P.I('pe', 'matmul', reads=[PT, vbf[t]], writes=[b_], out=b_[:], lhsT=PT[:, hd, :],
                    rhs=vbf[t][:, hd * 512:(hd + 1) * 512], start=False, stop=True)
                P.I('act', 'activation', reads=[b_], writes=[o_], out=o_[:, hd * 512:(hd + 1) * 512], in_=b_[:], func=AF.Copy)
            P.dma('sp', oloc_d[g * 128:(g + 1) * 128, :], o_[:], reads=[o_], writes=[oloc_d])
            for hd in range(4):
                for j in range(2):
                    m = 2 * hd + j
                    b_ = bk[2 + (m % 2)]
                    P.I('pe', 'matmul', reads=[KE, vbf[t]], writes=[b_], out=b_[:], lhsT=KE[:, m * 128:(m + 1) * 128],
                        rhs=vbf[t][:, hd * 512:(hd + 1) * 512], start=True, stop=True)
                    P.I('dve', 'scalar_tensor_tensor', reads=[S32[m], EA, b_], writes=[S32[m]], out=S32[m][:], in0=S32[m][:],
                        scalar=EA[:, m, 127:128], in1=b_[:], op0=ALU.mult, op1=ALU.add)
                    P.I('act', 'activation', reads=[S32[m]], writes=[Sbf[m]], out=Sbf[m][:], in_=S32[m][:], func=AF.Copy)
    if send_d is not None:
        for m in range(8):
            P.dma('sp', send_d[m * 128:(m + 1) * 128, :], S32[m][:], reads=[S32[m]], writes=[send_d])


def gla_b(P, cx, l, x_d, xo_d, modv_d, gnorm_d, wout_d, oloc_d, gs_d, qc_d=None, sprev_d=None, nt=NT):
    bk = cx.bank
    off = l * 12288
    gate = P.sbuf([128, D], F32, name="gate")
    bcast_row(P, gate, gate[:], modv_d, modv_d[0:1, off + 2 * D:off + 3 * D])
    gn = P.sbuf([128, 512], F32, name="gn")
    bcast_row(P, gn, gn[:], gnorm_d, gnorm_d[l:l + 1, :])
    wout = P.sbuf([128, 16, D], BF16, name="wout")
    for n in range(4):
        P.dma('pool', wout[:, :, n * 512:(n + 1) * 512], wout_d[l, :, n * 512:(n + 1) * 512].rearrange("(c p) n -> p c n", p=128),
              reads=[wout_d], writes=[wout])
    corr = qc_d is not None
    if corr:
        sp = P.sbuf([128, 8, 512], BF16, name="sprev")
        P.dma('pool', sp[:], sprev_d[:, :].rearrange("(m p) n -> p m n", p=128), reads=[sprev_d], writes=[sp])
        qc = P.sbuf([128, 8, 128], BF16, name="qcb")
    o = P.sbuf([128, D], F32, name="o"); gs = P.sbuf([128, D], F32, name="gs"); xt = P.sbuf([128, D], F32, name="xtb")
    on = P.sbuf([128, D], F32, name="on"); junk = P.sbuf([128, 512], BF16, name="junkb")
    onT = P.sbuf([128, 16, 128], BF16, name="onT")
    ss = P.sbuf([128, 4], F32, name="ssb"); rstd = P.sbuf([128, 4], F32, name="rstdb")
    for g in range(nt):
        rows = slice(g * 128, (g + 1) * 128)
        P.dma('sp', o[:], oloc_d[rows, :], reads=[oloc_d], writes=[o])
        P.dma('act', gs[:], gs_d[rows, :], reads=[gs_d], writes=[gs])
        P.dma('sp', xt[:], x_d[rows, :], reads=[x_d], writes=[xt])
        if corr:
            P.dma('pool', qc[:], qc_d[:, rows].rearrange("(m p) t -> p m t", p=128), reads=[qc_d], writes=[qc])
        for hd in range(4):
            b_ = bk[hd]
            osl = o[:, hd * 512:(hd + 1) * 512]
            if corr:
                for j in range(2):
                    m = 2 * hd + j
                    P.I('pe', 'matmul', reads=[qc, sp], writes=[b_], out=b_[:], lhsT=qc[:, m, :], rhs=sp[:, m, :],
                        start=(j == 0), stop=(j == 1))
                P.I('dve', 'tensor_tensor', reads=[o, b_], writes=[o], out=osl, in0=osl, in1=b_[:], op=ALU.add)
            P.I('act', 'activation', reads=[o], writes=[junk, ss], out=junk[:], in_=osl, func=AF.Square,
                accum_out=ss[:, hd:hd + 1])
        rstd_from_ss(P, ss, rstd, 4, 1.0 / 512)
        for hd in range(4):
            P.I('dve', 'scalar_tensor_tensor', reads=[o, rstd, gn], writes=[on], out=on[:, hd * 512:(hd + 1) * 512],
                in0=o[:, hd * 512:(hd + 1) * 512], scalar=rstd[:, hd:hd + 1], in1=gn[:], op0=ALU.mult, op1=ALU.mult)
        P.I('pool', 'tensor_tensor', reads=[on, gs], writes=[on], out=on[:], in0=on[:], in1=gs[:], op=ALU.mult)
        transpose_T(P, cx, on, onT, 0, [bk[4], bk[5]])
        for n in range(4):
            b_ = bk[n]
            for c in range(16):
                P.I('pe', 'matmul', reads=[onT, wout], writes=[b_], out=b_[:], lhsT=onT[:, c, :], rhs=wout[:, c, n * 512:(n + 1) * 512],
                    start=(c == 0), stop=(c == 15))
            ysl = on[:, n * 512:(n + 1) * 512]
            P.I('dve', 'tensor_tensor', reads=[b_, gate], writes=[on], out=ysl, in0=b_[:], in1=gate[:, n * 512:(n + 1) * 512],
                op=ALU.mult)
        P.I('pool', 'tensor_tensor', reads=[on, xt], writes=[xt], out=xt[:], in0=on[:], in1=xt[:], op=ALU.add)
        P.dma('sp', xo_d[rows, :], xt[:], reads=[xt], writes=[xo_d])


def peer(P, cx, l, x_d, xo_d, modv_d, nffn_d, wq_d, subk_d, uT_d, v_d, nt=NT):
    bk = cx.bank
    off = l * 12288 + 3 * D
    RG = P.sbuf([128, 25600], F32, name="RG")

    def carve(a, b, name, dt=F32):
        ap = RG[:, a:b]
        if dt == BF16:
            ap = ap.bitcast(BF16)
        return Buf(ap, name)
    hT = P.sbuf([128, 16, 512], BF16, name="hTp")
    acc = [P.sbuf([128, D], F32, name="acc%d" % i) for i in range(4)]
    ssb = [P.sbuf([128, 16, 128], F32, name="ssb%d" % i) for i in range(4)]
    tau = [P.sbuf([128, 8], F32, name="tau%d" % i) for i in range(4)]
    negC = [P.sbuf([128, 8], F32, name="negC%d" % i) for i in range(4)]
    xi = [P.sbuf([128, 8], F32, name="xi%d" % i) for i in range(4)]
    halfC = P.sbuf([128, 8], F32, name="halfC")
    XD = P.sbuf([128, 2048], F32, name="XD")
    subkT = P.sbuf([128, 16, 128], F32, name="subkT")
    idb = P.sbuf([128, 128], BF16, name="idb")
    P.I('dve', 'tensor_copy', reads=[cx.cst], writes=[idb], out=idb[:], in_=cx.c(C_ID))
    m16 = P.sbuf([128, 16, 16], F32, name="m16"); top = P.sbuf([128, 8, 16], F32, name="top")
    negm = P.sbuf([128, 8], F32, name="negm"); Z = P.sbuf([128, 8], F32, name="Z"); j16 = P.sbuf([128, 16], F32, name="j16")
    ss = P.sbuf([128, 4], F32, name="ssp"); rstd = P.sbuf([128, 4], F32, name="rstdp")
    stg = acc[0]
    P.dma('sp', stg[:].rearrange("p (a n) -> p a n", a=16), subk_d[l].rearrange("h q n d -> n (h q) d"), reads=[subk_d], writes=[stg])
    for g4 in range(4):
        b_ = bk[g4 % 2]
        for k in range(4):
            hp = g4 * 4 + k
            P.I('pe', 'transpose', reads=[stg, cx.cst], writes=[b_], out=b_[:, k * 128:(k + 1) * 128],
                in_=stg[:, hp * 128:(hp + 1) * 128], identity=cx.c(C_ID))
        P.I('act', 'activation', reads=[b_], writes=[subkT], out=subkT[:, g4 * 4:(g4 + 1) * 4, :],
            in_=b_[:].rearrange("p (k t) -> p k t", k=4), func=AF.Copy)
    P.barrier()
    for ps_ in range(nt // 4):
        xt = carve(0, 2048, "xt"); h = carve(2048, 4096, "h"); A = carve(4096, 6144, "A"); B = carve(6144, 8192, "B")
        qT = carve(8192, 16384, "qT"); cand = carve(16384, 18432, "cand"); candw = carve(18432, 20480, "candw")
        wqb = [carve(20480, 22528, "wq0", BF16), carve(22528, 24576, "wq1", BF16)]
        junk = carve(24576, 25600, "junk", BF16)
        make_AB(P, A, B, nffn_d, nffn_d[l:l + 1, :], modv_d, off, off + D, h)
        for t in range(4):
            g = ps_ * 4 + t
            P.dma('sp', xt[:], x_d[g * 128:(g + 1) * 128, :], reads=[x_d], writes=[xt])
            normmod_T(P, cx, xt, A, B, h, junk, ss, rstd, hT, t * 128, [bk[0], bk[1]])
        qTv = qT[:].rearrange("p (a n) -> p a n", a=16)
        for cb in range(8):
            w = wqb[cb % 2]
            wv = w[:].rearrange("p (c n) -> p c n", c=16)
            P.dma('pool', wv, wq_d[l, :, cb * 256:(cb + 1) * 256].rearrange("(c p) n -> p c n", p=128), reads=[wq_d], writes=[w])
            for m in range(2):
                b_ = bk[2 + (cb * 2 + m) % 2]
                for c in range(16):
                    P.I('pe', 'matmul', reads=[w, hT], writes=[b_], out=b_[:], lhsT=wv[:, c, m * 128:(m + 1) * 128], rhs=hT[:, c, :],
                        start=(c == 0), stop=(c == 15))
                P.I('act', 'activation', reads=[b_], writes=[qT], out=qTv[:, cb * 2 + m, :], in_=b_[:], func=AF.Copy)
        hv = h[:].rearrange("p (a n) -> p a n", a=16)
        cv4 = cand[:].rearrange("p (h a b) -> p h a b", h=8, a=16)
        cv = cand[:].rearrange("p (h n) -> p h n", h=8)
        cwv = candw[:].rearrange("p (h n) -> p h n", h=8)
        for t in range(4):
            s_ = ssb[t]
            for hp in range(16):
                b_ = bk[4 + hp // 4]
                P.I('pe', 'matmul', reads=[qT, subkT], writes=[b_], out=b_[:, (hp % 4) * 128:(hp % 4 + 1) * 128],
                    lhsT=qTv[:, hp, t * 128:(t + 1) * 128], rhs=subkT[:, hp, :], start=True, stop=True)
            for g4 in range(4):
                P.I('act', 'activation', reads=[bk[4 + g4]], writes=[s_], out=s_[:, g4 * 4:(g4 + 1) * 4, :],
                    in_=bk[4 + g4][:].rearrange("p (k t) -> p k t", k=4), func=AF.Copy)
            for hp in range(16):
                P.I('dve', 'max', reads=[s_], writes=[m16], out=m16[:, hp, 0:8], in_=s_[:, hp, :])
                P.I('dve', 'match_replace', reads=[s_, m16], writes=[h], out=hv[:, hp, :], in_to_replace=m16[:, hp, 0:8],
                    in_values=s_[:, hp, :], imm_value=-1e30)
                P.I('dve', 'max', reads=[h], writes=[m16], out=m16[:, hp, 8:16], in_=hv[:, hp, :])
            m16v = m16[:].rearrange("p (h q) k -> p h q k", q=2)
            P.I('dve', 'tensor_tensor', reads=[m16], writes=[cand], out=cv4,
                in0=m16v[:, :, 0, :].unsqueeze(3).to_broadcast([128, 8, 16, 16]),
                in1=m16v[:, :, 1, :].unsqueeze(2).to_broadcast([128, 8, 16, 16]), op=ALU.add)
            for hh in range(8):
                P.I('dve', 'max', reads=[cand], writes=[top], out=top[:, hh, 0:8], in_=cv[:, hh, :])
                P.I('dve', 'match_replace', reads=[cand, top], writes=[candw], out=cwv[:, hh, :], in_to_replace=top[:, hh, 0:8],
                    in_values=cv[:, hh, :], imm_value=-1e30)
                P.I('dve', 'max', reads=[candw], writes=[top], out=top[:, hh, 8:16], in_=cwv[:, hh, :])
            P.I('dve', 'tensor_scalar', reads=[top], writes=[negm], out=negm[:], in0=top[:, :, 0], scalar1=-1.0, scalar2=0.0,
                op0=ALU.mult, op1=ALU.add)
            for hh in range(8):
                P.I('act', 'activation', reads=[top, negm], writes=[j16, Z], out=j16[:], in_=top[:, hh, :], func=AF.Exp,
                    bias=negm[:, hh:hh + 1], scale=1.0, accum_out=Z[:, hh:hh + 1])
            P.I('act', 'activation', reads=[Z], writes=[Z], out=Z[:], in_=Z[:], func=AF.Ln)
            P.I('dve', 'tensor_tensor', reads=[negm, Z], writes=[negC[t]], out=negC[t][:], in0=negm[:], in1=Z[:], op=ALU.subtract)
            P.I('dve', 'tensor_scalar', reads=[top], writes=[tau[t]], out=tau[t][:], in0=top[:, :, 15], scalar1=1.0, scalar2=-1e-5,
                op0=ALU.mult, op1=ALU.add)
            P.I('dve', 'tensor_tensor', reads=[tau[t], negC[t]], writes=[xi[t]], out=xi[t][:], in0=tau[t][:], in1=negC[t][:], op=ALU.add)
            P.I('act', 'activation', reads=[xi[t]], writes=[xi[t]], out=xi[t][:], in_=xi[t][:], func=AF.Exp)
            P.I('dve', 'tensor_scalar', reads=[negC[t]], writes=[halfC], out=halfC[:], in0=negC[t][:], scalar1=0.5, scalar2=0.0,
                op0=ALU.mult, op1=ALU.add)
            for hh in range(8):
                P.I('act', 'activation', reads=[s_, halfC], writes=[s_], out=s_[:, 2 * hh:2 * hh + 2, :], in_=s_[:, 2 * hh:2 * hh + 2, :],
                    func=AF.Exp, bias=halfC[:, hh:hh + 1], scale=1.0)
        P.barrier()
        uTb = [carve(0, 4096, "uT0", BF16), carve(4096, 8192, "uT1", BF16)]
        vb = [carve(8192, 12288, "v0", BF16), carve(12288, 16384, "v1", BF16)]
        gAb = [[carve(16384 + k * 1024 + i * 256, 16384 + k * 1024 + (i + 1) * 256, "gA%d_%d" % (k, i), BF16) for i in range(4)]
               for k in range(2)]
        Xb = [carve(18432 + k * 512, 18432 + (k + 1) * 512, "X%d" % k) for k in range(4)]
        Tb = [[carve(20992 + k * 2048 + hh * 256, 20992 + k * 2048 + (hh + 1) * 256, "T%d_%d" % (k, hh), BF16) for hh in range(8)]
              for k in range(2)]
        GTb = [carve(20480 + k * 64, 20480 + (k + 1) * 64, "GT%d" % k, BF16) for k in range(8)]
        wbb = [[Buf(bk[6 + k][:, i * 128:(i + 1) * 128], "wb%d_%d" % (k, i)) for i in range(4)] for k in range(2)]
        accb = [[Buf(acc[t][:, n * 512:(n + 1) * 512], "acc%d_%d" % (t, n)) for n in range(4)] for t in range(4)]
        cnt = {'x': 0, 'gt': 0}
        units = [(g, t) for g in range(32) for t in range(4)]
        NU = len(units)
        HA = 4

        def load_group(g):
            uT = uTb[g % 2]; v = vb[g % 2]
            uTv = uT[:].rearrange("p (c e) -> p c e", c=16)
            vv = v[:].rearrange("p (i n) -> p i n", i=4)
            P.dma('pool', uTv, uT_d[l, :, g * 512:(g + 1) * 512].rearrange("(c p) e -> p c e", p=128), reads=[uT_d], writes=[uT])
            P.dma('pool', vv, v_d[l, g * 512:(g + 1) * 512, :].rearrange("(i j) n -> j i n", j=128), reads=[v_d], writes=[v])

        def group_A(g):
            uT = uTb[g % 2]
            uTv = uT[:].rearrange("p (c e) -> p c e", c=16)
            for i in range(4):
                b_ = bk[4 + i % 2]
                gA = gAb[g % 2][i]
                for c in range(16):
                    P.I('pe', 'matmul', reads=[uT, hT], writes=[b_], out=b_[:], lhsT=uTv[:, c, i * 128:(i + 1) * 128], rhs=hT[:, c, :],
                        start=(c == 0), stop=(c == 15))
                P.I('act', 'activation', reads=[b_], writes=[gA], out=gA[:], in_=b_[:], func=AF.Gelu_apprx_tanh)

        def stage_A_act(u):
            g, t = units[u]
            s_ = ssb[t]
            for hh in range(HA):
                X = Xb[hh]
                Xv = X[:].rearrange("p (i j) -> p i j", i=4)
                for i in range(4):
                    P.I('act', 'activation', reads=[s_], writes=[X], out=Xv[:, i, :], in_=s_[:, 2 * hh + 1, :], func=AF.Copy,
                        scale=s_[:, 2 * hh, g * 4 + i:g * 4 + i + 1])

        def stage_A_dve_indep(u):
            g, t = units[u]
            s_ = ssb[t]
            sv = s_[:].rearrange("p (h q) n -> p h q n", q=2)
            nd = 8 - HA
            P.I('dve', 'tensor_tensor', reads=[s_], writes=[XD], out=XD[:].rearrange("p (h i j) -> p h i j", h=nd, i=4),
                in0=sv[:, HA:8, 0, g * 4:(g + 1) * 4].unsqueeze(3).to_broadcast([128, nd, 4, 128]),
                in1=sv[:, HA:8, 1, :].unsqueeze(2).to_broadcast([128, nd, 4, 128]), op=ALU.mult)
            for k in range(nd):
                hh = HA + k
                T = Tb[u % 2][hh]
                P.I('dve', 'scalar_tensor_tensor', reads=[XD, xi[t]], writes=[T], out=T[:], in0=XD[:, k * 512:(k + 1) * 512],
                    scalar=xi[t][:, hh:hh + 1], in1=XD[:, k * 512:(k + 1) * 512], op0=ALU.is_ge, op1=ALU.mult)

        def stage_A_dve_dep(u):
            g, t = units[u]
            for hh in range(HA):
                X = Xb[hh]
                T = Tb[u % 2][hh]
                P.I('dve', 'scalar_tensor_tensor', reads=[X, xi[t]], writes=[T], out=T[:], in0=X[:],
                    scalar=xi[t][:, hh:hh + 1], in1=X[:], op0=ALU.is_ge, op1=ALU.mult)

        def stage_B1(u):
            for i in range(4):
                wb = wbb[u % 2][i]
                for hh in range(8):
                    T = Tb[u % 2][hh]
                    P.I('pe', 'matmul', reads=[T, idb], writes=[wb], out=wb[:], lhsT=T[:, i * 128:(i + 1) * 128],
                        rhs=idb[:], start=(hh == 0), stop=(hh == 7))

        def stage_B2(u):
            g, t = units[u]
            v = vb[g % 2]
            vv = v[:].rearrange("p (i n) -> p i n", i=4)
            gts = []
            for i in range(4):
                GT = GTb[cnt['gt'] % 8]; cnt['gt'] += 1
                gts.append(GT)
                P.I('dve', 'tensor_tensor', reads=[wbb[u % 2][i], gAb[g % 2][i]], writes=[GT], out=GT[:], in0=wbb[u % 2][i][:],
                    in1=gAb[g % 2][i][:, t * 128:(t + 1) * 128], op=ALU.mult)
            for i in range(4):
                for n in range(4):
                    P.I('pe', 'matmul', reads=[gts[i], v], writes=[bk[n]], out=bk[n][:], lhsT=gts[i][:], rhs=vv[:, i, n * 512:(n + 1) * 512],
                        start=(i == 0), stop=(i == 3))

        def stage_B3(u):
            g, t = units[u]
            for n in range(4):
                a_ = accb[t][n]
                if g == 0:
                    P.I('dve', 'tensor_copy', reads=[bk[n]], writes=[a_], out=a_[:], in_=bk[n][:])
                else:
                    P.I('dve', 'tensor_tensor', reads=[bk[n], a_], writes=[a_], out=a_[:], in0=a_[:], in1=bk[n][:], op=ALU.add)

        load_group(0)
        group_A(0)
        for u0 in (0, 1):
            stage_A_act(u0)
            stage_A_dve_indep(u0)
            stage_A_dve_dep(u0)
        stage_B1(0)
        for k in range(NU):
            g, t = units[k]
            if t == 0 and g + 1 < 32:
                load_group(g + 1)
            if k + 2 < NU:
                stage_A_act(k + 2)
            if t == 1 and g + 1 < 32:
                group_A(g + 1)
            if k + 1 < NU:
                stage_B1(k + 1)
            if k + 2 < NU:
                stage_A_dve_indep(k + 2)
            if k >= 1:
                stage_B3(k - 1)
            stage_B2(k)
            if k + 2 < NU:
                stage_A_dve_dep(k + 2)
        stage_B3(NU - 1)
        P.barrier()
        gate = carve(0, 2048, "gate"); xt5 = carve(2048, 4096, "xt5")
        bcast_row(P, gate, gate[:], modv_d, modv_d[0:1, off + 2 * D:off + 3 * D])
        for t in range(4):
            g = ps_ * 4 + t
            P.dma('sp', xt5[:], x_d[g * 128:(g + 1) * 128, :], reads=[x_d], writes=[xt5])
            P.I('dve', 'tensor_tensor', reads=[acc[t], gate], writes=[acc[t]], out=acc[t][:], in0=acc[t][:], in1=gate[:], op=ALU.mult)
            P.I('pool', 'tensor_tensor', reads=[acc[t], xt5], writes=[xt5], out=xt5[:], in0=acc[t][:], in1=xt5[:], op=ALU.add)
            P.dma('sp', xo_d[g * 128:(g + 1) * 128, :], xt5[:], reads=[xt5], writes=[xo_d])
        P.barrier()


def phase_kv(P, cx, x_d, modv_d, kvn, wkv, bf_d, KT_d, V_d, lf_d, nt):
    bk = cx.bank
    A = P.sbuf([128, D], F32, name="A"); B = P.sbuf([128, D], F32, name="B")
    xt = P.sbuf([128, D], F32, name="xt"); h = P.sbuf([128, D], F32, name="h"); junk = P.sbuf([128, D], BF16, name="junk")
    make_AB(P, A, B, kvn, kvn[0:1, :], modv_d, 49152, 51200, h)
    hT = P.sbuf([128, 16, 512], BF16, name="hT")
    ss = P.sbuf([128, 4], F32, name="ss"); rstd = P.sbuf([128, 4], F32, name="rstd")
    wblk = [P.sbuf([128, 16, 512], BF16, name="wb%d" % i) for i in range(2)]
    wfl = P.sbuf([128, 16, 16], BF16, name="wfl")
    P.dma('pool', wfl[:], wkv[:, 4096:4112].rearrange("(c p) n -> p c n", p=128), reads=[wkv], writes=[wfl])
    bfb = P.sbuf([128, 16], F32, name="bfb")
    bcast_row(P, bfb, bfb[:], bf_d, bf_d[0:1, :])
    ev = [P.sbuf([128, 512], F32, name="ev%d" % i) for i in range(2)]
    z16 = P.sbuf([128, 16], F32, name="z16")
    ne = 0
    for ps_ in range(nt // 4):
        cols = slice(ps_ * 512, (ps_ + 1) * 512)
        for t in range(4):
            g = ps_ * 4 + t
            P.dma('sp', xt[:], x_d[g * 128:(g + 1) * 128, :], reads=[x_d], writes=[xt])
            normmod_T(P, cx, xt, A, B, h, junk, ss, rstd, hT, t * 128, [bk[0], bk[1]])
        for cb in range(8):
            w = wblk[cb % 2]
            P.dma('pool', w[:], wkv[:, cb * 512:(cb + 1) * 512].rearrange("(c p) n -> p c n", p=128), reads=[wkv], writes=[w])
            if cb < 4:
                for m in range(4):
                    b_ = bk[2 + m % 2]
                    for c in range(16):
                        P.I('pe', 'matmul', reads=[w, hT], writes=[b_], out=b_[:], lhsT=w[:, c, m * 128:(m + 1) * 128], rhs=hT[:, c, :],
                            start=(c == 0), stop=(c == 15))
                    e_ = ev[ne % 2]; ne += 1
                    P.I('act', 'activation', reads=[b_], writes=[e_], out=e_[:], in_=b_[:], func=AF.Copy)
                    P.dma('sp', KT_d[(cb * 4 + m) * 128:(cb * 4 + m + 1) * 128, cols], e_[:], reads=[e_], writes=[KT_d])
            else:
                for t in range(4):
                    g = ps_ * 4 + t
                    b_ = bk[4 + t % 2]
                    for c in range(16):
                        P.I('pe', 'matmul', reads=[w, hT], writes=[b_], out=b_[:], lhsT=hT[:, c, t * 128:(t + 1) * 128], rhs=w[:, c, :],
                            start=(c == 0), stop=(c == 15))
                    e_ = ev[ne % 2]; ne += 1
                    P.I('dve', 'tensor_copy', reads=[b_], writes=[e_], out=e_[:], in_=b_[:])
                    P.dma('sp', V_d[g * 128:(g + 1) * 128, (cb - 4) * 512:(cb - 3) * 512], e_[:], reads=[e_], writes=[V_d])
        for t in range(4):
            g = ps_ * 4 + t
            for c in range(16):
                P.I('pe', 'matmul', reads=[wfl, hT], writes=[bk[6]], out=bk[6][:, 0:16], lhsT=hT[:, c, t * 128:(t + 1) * 128], rhs=wfl[:, c, :],
                    start=(c == 0), stop=(c == 15))
            P.I('dve', 'tensor_tensor', reads=[bk[6], bfb], writes=[z16], out=z16[:], in0=bk[6][:, 0:16], in1=bfb[:], op=ALU.add)
            P.I('act', 'activation', reads=[z16], writes=[z16], out=z16[:], in_=z16[:], func=AF.Exp, scale=-1.0)
            P.I('act', 'activation', reads=[z16], writes=[z16], out=z16[:], in_=z16[:], func=AF.Ln, bias=1.0)
            P.I('dve', 'tensor_scalar', reads=[z16], writes=[z16], out=z16[:], in0=z16[:], scalar1=-1.0, scalar2=0.0, op0=ALU.mult, op1=ALU.add)
            P.dma('sp', lf_d[g * 128:(g + 1) * 128, :], z16[:], reads=[z16], writes=[lf_d])


def phase_fox_a(P, cx, l, x_d, modv_d, nmix, wq_d, KT_d, V_d, lf_d, o_d, sg_d):
    bk = cx.bank
    bq = l - 2
    off = l * 12288
    A = P.sbuf([128, D], F32, name="A"); B = P.sbuf([128, D], F32, name="B")
    xt = P.sbuf([128, D], F32, name="xt"); h = P.sbuf([128, D], F32, name="h"); junk = P.sbuf([128, D], BF16, name="junk")
    make_AB(P, A, B, nmix, nmix[l:l + 1, :], modv_d, off, off + D, h)
    hT = P.sbuf([128, 16, 512], BF16, name="hT")
    ss = P.sbuf([128, 4], F32, name="ss"); rstd = P.sbuf([128, 4], F32, name="rstd")
    wblk = [P.sbuf([128, 16, 512], BF16, name="wb%d" % i) for i in range(2)]
    qTall = P.sbuf([128, 16, TOK], BF16, name="qTall")
    ev = [P.sbuf([128, 512], F32, name="ev0")] * 2
    lf = P.sbuf([128, 512], F32, name="lf"); Fw = P.sbuf([128, 512], F32, name="Fw"); Tot = P.sbuf([128, 512], F32, name="Tot")
    Pre = P.sbuf([128, 512], F32, name="Pre")
    P.dma('sp', lf[:].rearrange("s (b h) -> s b h", h=16), lf_d[:, :].rearrange("(b s) h -> s b h", s=128), reads=[lf_d], writes=[lf])
    P.I('pe', 'matmul', reads=[lf, cx.cst], writes=[bk[6]], out=bk[6][:], lhsT=cx.c(C_M1), rhs=lf[:], start=True, stop=True)
    P.I('pe', 'matmul', reads=[lf, cx.cst], writes=[bk[7]], out=bk[7][:], lhsT=cx.c(C_ONES), rhs=lf[:], start=True, stop=True)
    P.I('dve', 'tensor_copy', reads=[bk[7]], writes=[Tot], out=Tot[:], in_=bk[7][:])
    P.I('pool', 'memset', writes=[Pre], ap=Pre[:], constant=0.0)
    for b_i in range(1, 32):
        P.I('dve', 'tensor_tensor', reads=[Pre, Tot], writes=[Pre], out=Pre[:, b_i * 16:(b_i + 1) * 16], in0=Pre[:, (b_i - 1) * 16:b_i * 16],
            in1=Tot[:, (b_i - 1) * 16:b_i * 16], op=ALU.add)
    P.I('dve', 'tensor_tensor', reads=[bk[6], Pre], writes=[Fw], out=Fw[:], in0=bk[6][:], in1=Pre[:], op=ALU.add)
    Fv = Fw[:].rearrange("s (b h) -> s b h", h=16)
    Pv = Pre[:].rearrange("s (b h) -> s b h", h=16)
    ne = 0
    for ps_ in range(4):
        cols = slice(ps_ * 512, (ps_ + 1) * 512)
        for t in range(4):
            g = ps_ * 4 + t
            P.dma('sp', xt[:], x_d[g * 128:(g + 1) * 128, :], reads=[x_d], writes=[xt])
            normmod_T(P, cx, xt, A, B, h, junk, ss, rstd, hT, t * 128, [bk[0], bk[1]])
        for cb in range(8):
            w = wblk[cb % 2]
            P.dma('pool', w[:], wq_d[bq, :, cb * 512:(cb + 1) * 512].rearrange("(c p) n -> p c n", p=128), reads=[wq_d], writes=[w])
            if cb < 4:
                for m in range(4):
                    b_ = bk[2 + m % 2]
                    for c in range(16):
                        P.I('pe', 'matmul', reads=[w, hT], writes=[b_], out=b_[:], lhsT=w[:, c, m * 128:(m + 1) * 128], rhs=hT[:, c, :],
                            start=(c == 0), stop=(c == 15))
                    P.I('act', 'activation', reads=[b_], writes=[qTall], out=qTall[:, cb * 4 + m, cols], in_=b_[:], func=AF.Copy,
                        scale=float(128 ** -0.5))
            else:
                for t in range(4):
                    g = ps_ * 4 + t
                    b_ = bk[4 + t % 2]
                    for c in range(16):
                        P.I('pe', 'matmul', reads=[w, hT], writes=[b_], out=b_[:], lhsT=hT[:, c, t * 128:(t + 1) * 128], rhs=w[:, c, :],
                            start=(c == 0), stop=(c == 15))
                    e_ = ev[ne % 2]; ne += 1
                    P.I('act', 'activation', reads=[b_], writes=[e_], out=e_[:], in_=b_[:], func=AF.Sigmoid)
                    P.dma('sp', sg_d[g * 128:(g + 1) * 128, (cb - 4) * 512:(cb - 3) * 512], e_[:], reads=[e_], writes=[sg_d])
    KTh = [P.sbuf([128, 4096], BF16, name="KTh%d" % i) for i in range(2)]
    Vh = [P.sbuf([128, 32, 129], BF16, name="Vh%d" % i) for i in range(2)]
    for i in range(2):
        P.I('pool', 'memset', writes=[Vh[i]], ap=Vh[i][:], constant=1.0)
    mA = P.sbuf([128, 128], BF16, name="mA"); mB = P.sbuf([128, 128], BF16, name="mB")
    P.I('dve', 'tensor_copy', reads=[cx.cst], writes=[mA], out=mA[:], in_=cx.c(C_MA))
    P.I('dve', 'tensor_copy', reads=[cx.cst], writes=[mB], out=mB[:], in_=cx.c(C_MB))
    bias = [P.sbuf([128, 32], F32, name="bias%d" % i) for i in range(2)]
    PTb = [P.sbuf([128, 128], BF16, name="PT%d" % i) for i in range(4)]
    oh = [P.sbuf([128, 16, 128], F32, name="oh0")] * 2
    rZ = P.sbuf([128, 2], F32, name="rZ")
    npt = 0; nbi = 0; nst = 0
    for hh in range(16):
        Kt = KTh[hh % 2]; Vt = Vh[hh % 2]; o_h = oh[hh % 2]
        P.dma('pool', Kt[:], KT_d[hh * 128:(hh + 1) * 128, :], reads=[KT_d], writes=[Kt])
        P.dma('pool', Vt[:, :, 0:128], V_d[:, hh * 128:(hh + 1) * 128].rearrange("(b s) d -> s b d", s=128), reads=[V_d], writes=[Vt])
        for m in range(16):
            nblk = 2 * m + 2
            bi = bias[nbi % 2]; nbi += 1
            P.I('dve', 'scalar_tensor_tensor', reads=[Fw, Pre], writes=[bi], out=bi[:, 0:nblk], in0=Fv[:, 0:nblk, hh], scalar=-1.0,
                in1=Pv[:, 2 * m + 1, hh:hh + 1].to_broadcast([128, nblk]), op0=ALU.mult, op1=ALU.add)
            ob = bk[4 + (hh * 16 + m) % 2]
            sts = []
            LA = 2
            for j in range(nblk + LA):
                if j < nblk:
                    sbn = bk[nst % 4]; nst += 1
                    P.I('pe', 'matmul', reads=[Kt, qTall], writes=[sbn], out=sbn[:, 0:128], lhsT=Kt[:, j * 128:(j + 1) * 128],
                        rhs=qTall[:, hh, m * 128:(m + 1) * 128], start=True, stop=True)
                    sts.append(sbn)
                if j < LA:
                    continue
                j = j - LA
                sb_ = sts[j]
                pt = PTb[npt % 4]; npt += 1
                P.I('act', 'activation', reads=[sb_, bi], writes=[pt], out=pt[:], in_=sb_[:, 0:128], func=AF.Exp, bias=bi[:, j:j + 1], scale=1.0)
                if j == nblk - 2:
                    P.I('dve', 'tensor_tensor', reads=[pt, mA], writes=[pt], out=pt[:], in0=pt[:], in1=mA[:], op=ALU.mult)
                if j == nblk - 1:
                    P.I('dve', 'tensor_tensor', reads=[pt, mB], writes=[pt], out=pt[:], in0=pt[:], in1=mB[:], op=ALU.mult)
                P.I('pe', 'matmul', reads=[pt, Vt], writes=[ob], out=ob[:, 0:129], lhsT=pt[:], rhs=Vt[:, j, :], start=(j == 0), stop=(j == nblk - 1))
            P.I('dve', 'reciprocal', reads=[ob], writes=[rZ], out=rZ[:, 0:1], in_=ob[:, 128:129])
            P.I('dve', 'tensor_scalar', reads=[ob, rZ], writes=[o_h], out=o_h[:, m, :], in0=ob[:, 0:128], scalar1=rZ[:, 0:1], scalar2=0.0,
                op0=ALU.mult, op1=ALU.add)
        P.dma('sp', o_d[:, hh * 128:(hh + 1) * 128].rearrange("(m t) d -> t m d", t=128), o_h[:], reads=[o_h], writes=[o_d])


def phase_fox_b(P, cx, l, x_d, o_d, sg_d, wout_d, xo_d, modv_d):
    bk = cx.bank
    off = l * 12288
    gate = P.sbuf([128, D], F32, name="gate")
    bcast_row(P, gate, gate[:], modv_d, modv_d[0:1, off + 2 * D:off + 3 * D])
    wout = P.sbuf([128, 16, D], BF16, name="wout")
    for n in range(4):
        P.dma('pool', wout[:, :, n * 512:(n + 1) * 512], wout_d[l - 2, :, n * 512:(n + 1) * 512].rearrange("(c p) n -> p c n", p=128),
              reads=[wout_d], writes=[wout])
    o = P.sbuf([128, D], F32, name="o"); sg = P.sbuf([128, D], F32, name="sg"); xt = P.sbuf([128, D], F32, name="xt")
    onT = P.sbuf([128, 16, 128], BF16, name="onT")
    for g in range(NT):
        rows = slice(g * 128, (g + 1) * 128)
        P.dma('sp', o[:], o_d[rows, :], reads=[o_d], writes=[o])
        P.dma('act', sg[:], sg_d[rows, :], reads=[sg_d], writes=[sg])
        P.dma('sp', xt[:], x_d[rows, :], reads=[x_d], writes=[xt])
        P.I('pool', 'tensor_tensor', reads=[o, sg], writes=[o], out=o[:], in0=o[:], in1=sg[:], op=ALU.mult)
        transpose_T(P, cx, o, onT, 0, [bk[4], bk[5]])
        for n in range(4):
            b_ = bk[n]
            for c in range(16):
                P.I('pe', 'matmul', reads=[onT, wout], writes=[b_], out=b_[:], lhsT=onT[:, c, :], rhs=wout[:, c, n * 512:(n + 1) * 512],
                    start=(c == 0), stop=(c == 15))
            P.I('dve', 'tensor_tensor', reads=[b_, gate], writes=[sg], out=sg[:, n * 512:(n + 1) * 512], in0=b_[:],
                in1=gate[:, n * 512:(n + 1) * 512], op=ALU.mult)
        P.I('pool', 'tensor_tensor', reads=[sg, xt], writes=[xt], out=xt[:], in0=sg[:], in1=xt[:], op=ALU.add)
        P.dma('sp', xo_d[rows, :], xt[:], reads=[xt], writes=[xo_d])


def phase_final(P, x_d, fn_d, o_d):
    A = P.sbuf([128, D], F32, name="A")
    bcast_row(P, A, A[:], fn_d, fn_d[0:1, :])
    xt = [P.sbuf([128, D], F32, name="xt%d" % i) for i in range(2)]
    h = [P.sbuf([128, D], F32, name="h%d" % i) for i in range(2)]
    junk = P.sbuf([128, D], BF16, name="junk")
    ss = P.sbuf([128, 4], F32, name="ss"); rstd = P.sbuf([128, 4], F32, name="rstd")
    for g in range(NT):
        x_, h_ = xt[g % 2], h[g % 2]
        P.dma('sp', x_[:], x_d[g * 128:(g + 1) * 128, :], reads=[x_d], writes=[x_])
        P.I('act', 'activation', reads=[x_], writes=[junk, ss], out=junk[:], in_=x_[:], func=AF.Square, accum_out=ss[:, 0:1])
        rstd_from_ss(P, ss, rstd, 1, 1.0 / D)
        P.I('dve', 'scalar_tensor_tensor', reads=[x_, rstd, A], writes=[h_], out=h_[:], in0=x_[:], scalar=rstd[:, 0:1], in1=A[:],
            op0=ALU.mult, op1=ALU.mult)
        P.dma('sp', o_d[g * 128:(g + 1) * 128, :], h_[:], reads=[h_], writes=[o_d])


def phase_xsel(P, cx, xfull_d, xB_d):
    omp = P.sbuf([128, 1], F32, name="omp")
    P.I('dve', 'tensor_scalar', reads=[cx.cst], writes=[omp], out=omp[:], in0=cx.cst[:, C_P:C_P + 1], scalar1=-1.0, scalar2=1.0,
        op0=ALU.mult, op1=ALU.add)
    xe = [P.sbuf([128, D], F32, name="xe%d" % i) for i in range(2)]
    xo = [P.sbuf([128, D], F32, name="xo%d" % i) for i in range(2)]
    for m in range(16):
        a, b_ = xe[m % 2], xo[m % 2]
        P.dma('sp', a[:], xfull_d[(2 * m) * 128:(2 * m + 1) * 128, :], reads=[xfull_d], writes=[a])
        P.dma('act', b_[:], xfull_d[(2 * m + 1) * 128:(2 * m + 2) * 128, :], reads=[xfull_d], writes=[b_])
        P.I('dve', 'tensor_scalar', reads=[a, omp], writes=[a], out=a[:], in0=a[:], scalar1=omp[:, 0:1], scalar2=0.0,
            op0=ALU.mult, op1=ALU.add)
        P.I('dve', 'scalar_tensor_tensor', reads=[b_, a, cx.cst], writes=[a], out=a[:], in0=b_[:], scalar=cx.cst[:, C_P:C_P + 1],
            in1=a[:], op0=ALU.mult, op1=ALU.add)
        P.dma('sp', xB_d[m * 128:(m + 1) * 128, :], a[:], reads=[a], writes=[xB_d])


def build_fused():
    nc = bass.Bass("TRN2", target_bir_lowering=False)
    P = Prog(nc)
    EI = "ExternalInput"
    S = 4096
    cst = P.dram("cst", [128, NCST], F32, kind=EI)
    x_in = P.dram("x", [S, D], F32, kind=EI)
    c_d = P.dram("c", [1, D], F32, kind=EI)
    Wm = P.dram("Wm", [D, MODN], F32, kind=EI)
    Bm = P.dram("Bm", [1, MODN], F32, kind=EI)
    nmix = P.dram("norm_mix", [4, D], F32, kind=EI)
    nffn = P.dram("norm_ffn", [4, D], F32, kind=EI)
    win = P.dram("gla_w_in", [2, D, 6160], F32, kind=EI)
    wg2 = P.dram("gla_w_gate2", [2, 16, 1024], F32, kind=EI)
    bg2 = P.dram("gla_b_gate2", [2, 1024], F32, kind=EI)
    gnorm = P.dram("gla_norm", [2, 512], F32, kind=EI)
    gwout = P.dram("gla_w_out", [2, D, D], F32, kind=EI)
    kvn = P.dram("kv_norm", [1, D], F32, kind=EI)
    wkv = P.dram("fox_w_kv", [D, 4112], F32, kind=EI)
    bf_d = P.dram("fox_b_f", [1, 16], F32, kind=EI)
    fwq = P.dram("fox_w_q", [2, D, 2 * D], F32, kind=EI)
    fwo = P.dram("fox_w_out", [2, D, D], F32, kind=EI)
    pwq = P.dram("peer_w_q", [4, D, D], F32, kind=EI)
    subk = P.dram("peer_subkeys", [4, 8, 2, 128, 128], F32, kind=EI)
    uT = P.dram("uT", [4, D, 16384], F32, kind=EI)
    pv = P.dram("pv", [4, 16384, D], F32, kind=EI)
    fn_d = P.dram("final_norm", [1, D], F32, kind=EI)
    out_d = P.dram("out", [TOK, D], F32, kind="ExternalOutput")
    modv = P.dram("modv", [1, MODN], F32)
    XA = P.dram("XA", [S, D], F32); XB = P.dram("XB", [S, D], F32)
    oloc = P.dram("oloc", [S, D], F32); gs = P.dram("gs", [S, D], F32)
    KT = P.dram("KT", [D, S], F32); V = P.dram("V", [S, D], F32); lf = P.dram("lf", [S, 16], F32)
    xs0 = P.dram("xs0", [TOK, D], F32); xs1 = P.dram("xs1", [TOK, D], F32)
    o_s = P.dram("o_s", [TOK, D], F32); sg_s = P.dram("sg_s", [TOK, D], F32)
    cx = Ctx(P, cst)
    P.cx = cx
    with P.phase():
        phase_mod(P, c_d, Wm, Bm, modv)
    xin = x_in
    for l in range(2):
        with P.phase():
            gla_a(P, cx, l, xin, modv, nmix, win, wg2, bg2, oloc, gs, nt=32)
        with P.phase():
            gla_b(P, cx, l, xin, XA, modv, gnorm, gwout, oloc, gs, nt=32)
        with P.phase():
            peer(P, cx, l, XA, XB, modv, nffn, pwq, subk, uT, pv, nt=32)
        xin = XB
    with P.phase():
        phase_kv(P, cx, XB, modv, kvn, wkv, bf_d, KT, V, lf, 32)
    with P.phase():
        phase_xsel(P, cx, XB, xs0)
    for l in (2, 3):
        with P.phase():
            phase_fox_a(P, cx, l, xs0, modv, nmix, fwq, KT, V, lf, o_s, sg_s)
        with P.phase():
            phase_fox_b(P, cx, l, xs0, o_s, sg_s, fwo, xs1, modv)
        with P.phase():
            peer(P, cx, l, xs1, xs0, modv, nffn, pwq, subk, uT, pv, nt=16)
    with P.phase():
        phase_final(P, xs0, fn_d, out_d)
    P.finish()
    return nc


def kernel(x, c, ada_w, ada_b, norm_mix, norm_ffn, gla_w_in, gla_w_gate2, gla_b_gate2, gla_norm, gla_w_out, kv_norm,
           kv_ada_w, kv_ada_b, fox_w_kv, fox_b_f, fox_w_q, fox_w_out, peer_w_q, peer_subkeys, peer_u, peer_v, final_norm):
    f = lambda a: np.ascontiguousarray(np.asarray(a, dtype=np.float32))
    x = f(x); c = f(c)
    Wm = np.ascontiguousarray(np.concatenate([f(ada_w[l]) for l in range(4)] + [f(kv_ada_w)], axis=1))
    Bm = np.ascontiguousarray(np.concatenate([f(ada_b[l]) for l in range(4)] + [f(kv_ada_b)])[None, :])
    uT = np.ascontiguousarray(np.transpose(f(peer_u), (0, 2, 1)))
    shared = dict(Wm=Wm, Bm=Bm, norm_mix=f(norm_mix), norm_ffn=f(norm_ffn), gla_w_in=f(gla_w_in), gla_w_gate2=f(gla_w_gate2),
                  gla_b_gate2=f(gla_b_gate2), gla_norm=f(gla_norm), gla_w_out=f(gla_w_out), kv_norm=f(kv_norm)[None, :],
                  fox_w_kv=f(fox_w_kv), fox_b_f=f(fox_b_f)[None, :], fox_w_q=f(fox_w_q), fox_w_out=f(fox_w_out),
                  peer_w_q=f(peer_w_q), peer_subkeys=f(peer_subkeys), uT=uT, pv=f(peer_v), final_norm=f(final_norm)[None, :])
    ins = [dict(shared, cst=make_consts(r % 2), x=x[r // 2], c=c[r // 2:r // 2 + 1]) for r in range(8)]
    res = run_bass_kernel_spmd(build_fused(), ins, core_ids=list(range(8))).results
    out = np.empty((4, 32, 128, D), np.float32)
    for r in range(8):
        out[r // 2, (r % 2)::2] = res[r]["out"].reshape(16, 128, D)
    return out.reshape(4, 4096, D)
```
